# Optimizing a Trainium2 kernel written in Bass

```python
import math
import jax, jax.numpy as jnp
from jax import lax
import numpy as np

D_MODEL = 1024
BATCH = 32
SEQ = 2048
DEPTH = 1

HEAD_DIM = 64
MIX_WIDTH = D_MODEL
N_HEADS_A = (MIX_WIDTH // 2) // HEAD_DIM
N_HEADS_B = (MIX_WIDTH // 2) // HEAD_DIM
D_NOPE = HEAD_DIM
D_ROPE = HEAD_DIM // 2
KV_RANK = 2 * HEAD_DIM
V_DIM = HEAD_DIM
IDX_HEADS = 8
IDX_DIM = HEAD_DIM
TOPK_MAX = 256
Q_BLOCK = 128
DILATED_PATTERNS = ((128, 1), (512, 4), (2048, 16))
CROSS_HEADS = 4
CROSS_HEAD_DIM = D_MODEL // CROSS_HEADS
MEM_TOKENS = 256
D_FF = 4 * D_MODEL
ROPE_THETA = 10000.0
NORM_EPS = 1e-6

IN_SIZES = (N_HEADS_A * D_NOPE,
            N_HEADS_A * D_ROPE,
            KV_RANK,
            D_ROPE,
            IDX_HEADS * IDX_DIM,
            IDX_DIM,
            IDX_HEADS,
            3 * N_HEADS_B * HEAD_DIM)
IN_COLS = 512 + 256 + 128 + 32 + 512 + 64 + 8 + 1536 if D_MODEL == 1024 else int(np.sum(IN_SIZES))

kernel_name = "hybrid_dsa_dilated_xattn_block"


def rmsnorm(x, g):
    xf = x.astype(jnp.float32)
    y = xf * lax.rsqrt(jnp.mean(xf * xf, axis=-1, keepdims=True) + NORM_EPS)
    return (y * g.astype(jnp.float32)).astype(x.dtype)


def rope(x):
    S, d = x.shape[1], x.shape[-1]
    inv = ROPE_THETA ** (-jnp.arange(0, d, 2, dtype=jnp.float32) / d)
    ang = jnp.arange(S, dtype=jnp.float32)[:, None] * inv[None, :]
    cos = jnp.cos(ang)[None, :, None, :].astype(x.dtype)
    sin = jnp.sin(ang)[None, :, None, :].astype(x.dtype)
    x1, x2 = jnp.split(x, 2, axis=-1)
    return jnp.concatenate([x1 * cos - x2 * sin, x2 * cos + x1 * sin], axis=-1)


def dsa_sparse_attention(q_nope, q_rope, c_kv, k_rope, q_idx, k_idx, w_idx, w_uk, w_uv):
    B, S = q_nope.shape[0], q_nope.shape[1]
    topk = min(TOPK_MAX, S // 4)
    nb = S // Q_BLOCK
    scale = (D_NOPE + D_ROPE) ** -0.5
    q_lat = jnp.einsum('bshn,hnc->bshc', q_nope, w_uk)
    w_eff = w_idx.astype(jnp.float32) * (IDX_HEADS ** -0.5) * (IDX_DIM ** -0.5)
    key_pos = jnp.arange(S)
    gather = jax.vmap(lambda table, ix: table[ix])

    def to_blocks(a):
        return jnp.moveaxis(a.reshape((B, nb, Q_BLOCK) + a.shape[2:]), 1, 0)

    def block(args):
        ql, qr, qi, wi, blk = args
        q_pos = blk * Q_BLOCK + jnp.arange(Q_BLOCK)
        causal = key_pos[None, :] <= q_pos[:, None]
        logits = jnp.einsum('bqhd,bsd->bhqs', qi, k_idx).astype(jnp.float32)
        index = jnp.einsum('bhqs,bqh->bqs', jax.nn.relu(logits), wi)
        index = jnp.where(causal[None], index, -jnp.inf)
        _, sel = lax.top_k(index, topk)
        valid = sel <= q_pos[None, :, None]
        c_sel = gather(c_kv, sel)
        r_sel = gather(k_rope, sel)
        s = (jnp.einsum('bqhc,bqkc->bhqk', ql, c_sel)
             + jnp.einsum('bqhr,bqkr->bhqk', qr, r_sel)).astype(jnp.float32) * scale
        s = jnp.where(valid[:, None], s, -jnp.inf)
        p = jax.nn.softmax(s, axis=-1).astype(c_sel.dtype)
        return jnp.einsum('bhqk,bqkc->bqhc', p, c_sel)

    o_lat = lax.map(block, (to_blocks(q_lat), to_blocks(q_rope), to_blocks(q_idx),
                            to_blocks(w_eff), jnp.arange(nb)))
    o_lat = jnp.moveaxis(o_lat, 0, 1).reshape(B, S, N_HEADS_A, KV_RANK)
    return jnp.einsum('bshc,hcv->bshv', o_lat, w_uv)


def dilated_pattern(q, k, v, window, dilation):
    B, H, S, d = q.shape
    n = S // dilation
    w_sub = window // dilation
    nb = -(-n // Q_BLOCK)
    pad = nb * Q_BLOCK - n

    def split(a):
        a = a.reshape(B, H, n, dilation, d).transpose(0, 1, 3, 2, 4)
        a = jnp.pad(a, ((0, 0), (0, 0), (0, 0), (0, pad), (0, 0)))
        return a.reshape(B, H, dilation, nb, Q_BLOCK, d)

    def with_prev(a):
        prev = jnp.concatenate([jnp.zeros_like(a[:, :, :, :1]), a[:, :, :, :-1]], axis=3)
        return jnp.concatenate([prev, a], axis=4)

    qb = split(q)
    kk = with_prev(split(k))
    vv = with_prev(split(v))
    s = jnp.einsum('bhrnqd,bhrnkd->bhrnqk', qb, kk).astype(jnp.float32) * (d ** -0.5)
    rel = (jnp.arange(Q_BLOCK)[:, None] + Q_BLOCK) - jnp.arange(2 * Q_BLOCK)[None, :]
    band = (rel >= 0) & (rel <= w_sub)
    has_prev = (jnp.arange(nb)[:, None, None] > 0) | (jnp.arange(2 * Q_BLOCK)[None, None, :] >= Q_BLOCK)
    mask = band[None] & has_prev
    s = jnp.where(mask, s, -jnp.inf)
    lse = jax.nn.logsumexp(s, axis=-1)
    p = jnp.exp(s - lse[..., None]).astype(v.dtype)
    o = jnp.einsum('bhrnqk,bhrnkd->bhrnqd', p, vv)

    def merge(a, tail):
        a = a.reshape((B, H, dilation, nb * Q_BLOCK) + tail)[:, :, :, :n]
        return jnp.swapaxes(a, 2, 3).reshape((B, H, S) + tail)

    return merge(o, (d,)), merge(lse, ())


def dilated_attention(q, k, v):
    outs, lses = [], []
    for window, dilation in DILATED_PATTERNS:
        o, l = dilated_pattern(q, k, v, window, dilation)
        outs.append(o)
        lses.append(l)
    alpha = jax.nn.softmax(jnp.stack(lses, axis=0), axis=0).astype(q.dtype)
    return jnp.sum(alpha[..., None] * jnp.stack(outs, axis=0), axis=0)


def setup_inputs(seed: int = 0) -> dict:
    key = jax.random.key(seed)
    ks = jax.random.split(key, 20)
    f32 = jnp.float32

    def w(k, shape, fan_in):
        return jax.random.normal(k, shape, f32) * (fan_in ** -0.5)

    def gain(k, shape):
        return 1.0 + 0.02 * jax.random.normal(k, shape, f32)

    L = DEPTH
    return {
        "x": jax.random.normal(ks[0], (BATCH, SEQ, D_MODEL), f32),
        "mem": jax.random.normal(ks[1], (BATCH, MEM_TOKENS, D_MODEL), f32),
        "norm_mix_g": gain(ks[2], (L, D_MODEL)),
        "w_in": w(ks[3], (L, D_MODEL, IN_COLS), D_MODEL),
        "kv_norm_g": gain(ks[4], (L, KV_RANK)),
        "w_uk": w(ks[5], (L, N_HEADS_A, D_NOPE, KV_RANK), D_NOPE),
        "w_uv": w(ks[6], (L, N_HEADS_A, KV_RANK, V_DIM), KV_RANK),
        "w_out": w(ks[7], (L, N_HEADS_A * V_DIM + N_HEADS_B * HEAD_DIM, D_MODEL), MIX_WIDTH),
        "norm_cross_g": gain(ks[8], (L, D_MODEL)),
        "norm_mem_g": gain(ks[9], (L, D_MODEL)),
        "w_q_cross": w(ks[10], (L, D_MODEL, CROSS_HEADS * CROSS_HEAD_DIM), D_MODEL),
        "w_kv_cross": w(ks[11], (L, D_MODEL, 2 * CROSS_HEADS * CROSS_HEAD_DIM), D_MODEL),
        "w_o_cross": w(ks[12], (L, CROSS_HEADS * CROSS_HEAD_DIM, D_MODEL), CROSS_HEADS * CROSS_HEAD_DIM),
        "norm_mlp_g": gain(ks[13], (L, D_MODEL)),
        "w_up": w(ks[14], (L, D_MODEL, D_FF), D_MODEL),
        "w_down": w(ks[15], (L, D_FF, D_MODEL), D_FF),
        "norm_final_g": gain(ks[16], (D_MODEL,)),
    }


def reference(x, mem, norm_mix_g, w_in, kv_norm_g, w_uk, w_uv, w_out, norm_cross_g, norm_mem_g,
              w_q_cross, w_kv_cross, w_o_cross, norm_mlp_g, w_up, w_down, norm_final_g):
    B, S, _ = x.shape
    M = mem.shape[1]
    split_at = [int(c) for c in np.cumsum(IN_SIZES)[:-1]]
    for l in range(DEPTH):
        h = rmsnorm(x, norm_mix_g[l])
        proj = h @ w_in[l]
        p_qn, p_qr, p_ckv, p_kr, p_qi, p_ki, p_wi, p_qkv = jnp.split(proj, split_at, axis=-1)
        q_nope = p_qn.reshape(B, S, N_HEADS_A, D_NOPE)
        q_rope = rope(p_qr.reshape(B, S, N_HEADS_A, D_ROPE))
        c_kv = rmsnorm(p_ckv, kv_norm_g[l])
        k_rope = rope(p_kr[:, :, None, :])[:, :, 0]
        q_idx = rope(p_qi.reshape(B, S, IDX_HEADS, IDX_DIM))
        k_idx = rope(p_ki[:, :, None, :])[:, :, 0]
        o_a = dsa_sparse_attention(q_nope, q_rope, c_kv, k_rope, q_idx, k_idx, p_wi, w_uk[l], w_uv[l])
        qkv = p_qkv.reshape(B, S, 3, N_HEADS_B, HEAD_DIM)
        q_b = rope(qkv[:, :, 0]).transpose(0, 2, 1, 3)
        k_b = rope(qkv[:, :, 1]).transpose(0, 2, 1, 3)
        v_b = qkv[:, :, 2].transpose(0, 2, 1, 3)
        o_b = dilated_attention(q_b, k_b, v_b).transpose(0, 2, 1, 3)
        heads = jnp.concatenate([o_a.reshape(B, S, N_HEADS_A * V_DIM),
                                 o_b.reshape(B, S, N_HEADS_B * HEAD_DIM)], axis=-1)
        x = x + heads @ w_out[l]
        hc = rmsnorm(x, norm_cross_g[l])
        m = rmsnorm(mem, norm_mem_g[l])
        qc = (hc @ w_q_cross[l]).reshape(B, S, CROSS_HEADS, CROSS_HEAD_DIM)
        kvc = (m @ w_kv_cross[l]).reshape(B, M, 2, CROSS_HEADS, CROSS_HEAD_DIM)
        sc = jnp.einsum('bshd,bmhd->bhsm', qc, kvc[:, :, 0]).astype(jnp.float32) * (CROSS_HEAD_DIM ** -0.5)
        pc = jax.nn.softmax(sc, axis=-1).astype(x.dtype)
        oc = jnp.einsum('bhsm,bmhd->bshd', pc, kvc[:, :, 1]).reshape(B, S, CROSS_HEADS * CROSS_HEAD_DIM)
        x = x + oc @ w_o_cross[l]
        hm = rmsnorm(x, norm_mlp_g[l])
        x = x + jnp.square(jax.nn.relu(hm @ w_up[l])) @ w_down[l]
    return rmsnorm(x, norm_final_g)
```

```python
import math
from contextlib import ExitStack
import numpy as np
import concourse.bass as bass
import concourse.mybir as mybir
from concourse.bass_utils import run_bass_kernel_spmd

F32 = mybir.dt.float32
BF16 = mybir.dt.bfloat16
AF = mybir.ActivationFunctionType
ALU = mybir.AluOpType
AX = mybir.AxisListType

NCORES = 8
NB = 4
S = 2048
D = 1024
BIS_ITERS = 18
NEG = -1.0e30
STRIP_W = 2816


class Res:
    __slots__ = ("name", "writer", "readers", "excl")

    def __init__(self, name):
        self.name = name
        self.writer = None
        self.readers = []
        self.excl = (name[0] == "psum")


class ResReg:
    def __init__(self):
        self.d = {}

    def __call__(self, *key):
        r = self.d.get(key)
        if r is None:
            r = self.d[key] = Res(key)
        return r


class Eng:
    def __init__(self, name, sem, is_pe=False):
        self.name, self.sem, self.is_pe = name, sem, is_pe
        self.count = 0
        self.ops = []
        self.waited = {}


class Prog:
    def __init__(self, nc, sems, dma_sem_pool):
        self.nc = nc
        self.E = {n: Eng(n, sems[n], is_pe=(n == "pe")) for n in ("pe", "act", "dve", "pool", "sp")}
        self.dma_sems = {}
        self.sem_pool = dma_sem_pool
        self.nwaits = 0
        self.nops = 0

    def _deps(self, reads, writes):
        deps = []
        for r in reads:
            if r.writer is not None:
                deps.append(r.writer)
            if r.excl:
                deps.extend(r.readers)
        for w in writes:
            if w.writer is not None:
                deps.append(w.writer)
            deps.extend(w.readers)
        return deps

    def _waits(self, eng, deps):
        need = {}
        for d in deps:
            key = (d[0], d[1])
            if need.get(key, 0) < d[2]:
                need[key] = d[2]
        waits = []
        for key, v in need.items():
            if key[0] == "e" and key[1] == eng.name and eng.is_pe:
                continue
            if eng.waited.get(key, 0) >= v:
                continue
            eng.waited[key] = v
            if key[0] == "e":
                waits.append((self.E[key[1]].sem, v))
            else:
                waits.append((self.dma_sems[key[1]][0], v * 16))
        self.nwaits += len(waits)
        return waits

    def _commit(self, tok, reads, writes):
        for r in reads:
            if r.excl:
                r.writer = tok
                r.readers = []
            else:
                r.readers.append(tok)
        for w in writes:
            w.writer = tok
            w.readers = []

    def op(self, engname, fns, reads=(), writes=()):
        eng = self.E[engname]
        if isinstance(fns, tuple):
            fns = [fns]
        waits = self._waits(eng, self._deps(reads, writes))
        eng.count += 1
        tok = ("e", engname, eng.count)
        eng.ops.append((waits, fns, ("e", eng.sem)))
        self._commit(tok, reads, writes)
        self.nops += len(fns)
        return tok

    def dma(self, queue, fn, semkey, reads=(), writes=()):
        eng = self.E[queue]
        if semkey not in self.dma_sems:
            self.dma_sems[semkey] = [self.sem_pool.pop(), 0]
        ent = self.dma_sems[semkey]
        deps = self._deps(reads, writes)
        if ent[1] > 0:
            deps.append(("d", semkey, ent[1]))
        waits = self._waits(eng, deps)
        ent[1] += 1
        tok = ("d", semkey, ent[1])
        eng.ops.append((waits, [fn], ("d", ent[0])))
        self._commit(tok, reads, writes)
        self.nops += 1
        return tok

    def wait_tokens(self, engname, toks):
        eng = self.E[engname]
        waits = self._waits(eng, list(toks))
        if waits:
            eng.ops.append((waits, [], None))

    def barrier(self):
        toks = []
        for e in self.E.values():
            if e.count > 0:
                toks.append(("e", e.name, e.count))
        for k, (s, c) in self.dma_sems.items():
            if c > 0:
                toks.append(("d", k, c))
        for e in self.E.values():
            self.wait_tokens(e.name, [t for t in toks if not (t[0] == "e" and t[1] == e.name and e.is_pe)])

    def new_epoch(self, sems, reg):
        for e in self.E.values():
            e.sem = sems[e.name]
            e.count = 0
            e.waited = {k: v for k, v in e.waited.items() if k[0] == "d"}
        for r in reg.d.values():
            if r.writer is not None and r.writer[0] == "e":
                r.writer = None
            r.readers = [t for t in r.readers if t[0] != "e"]

    def emit(self, block):
        def run(eng, h):
            for waits, fns, inc in eng.ops:
                for sem, v in waits:
                    h.wait_ge(sem, v)
                ins = None
                for f in fns:
                    ins = getattr(h, f[0])(*f[1], **f[2])
                if inc is not None and ins is not None:
                    ins.then_inc(inc[1], 1 if inc[0] == "e" else 16)

        block.tensor(lambda h: run(self.E["pe"], h))
        block.scalar(lambda h: run(self.E["act"], h))
        block.vector(lambda h: run(self.E["dve"], h))
        block.gpsimd(lambda h: run(self.E["pool"], h))
        block.sync(lambda h: run(self.E["sp"], h))


def I(name, *a, **k):
    return (name, a, k)


class Rot:
    def __init__(self, items):
        self.items, self.i = items, 0

    def next(self):
        it = self.items[self.i % len(self.items)]
        self.i += 1
        return it


O_QN, O_QR, O_CKV, O_KR, O_QI, O_KI, O_WI, O_QKV = 0, 512, 768, 896, 928, 1440, 1504, 1512


def _win_plan():
    cols = []
    slabs = []

    def slab(chunks_cols):
        off = len(cols)
        chunks = []
        for kind, idx, cc in chunks_cols:
            chunks.append(dict(kind=kind, idx=idx, coff=len(cols) - off, M=len(cc)))
            cols.extend(cc)
        slabs.append((off, len(cols) - off, chunks))

    def qa(h):
        return list(range(O_QN + 64 * h, O_QN + 64 * h + 64)) + list(range(O_QR + 32 * h, O_QR + 32 * h + 32))

    r = lambda a, n: list(range(a, a + n))
    slab([("QA", h, qa(h)) for h in range(0, 4)])
    slab([("QA", h, qa(h)) for h in range(4, 8)] + [("KR", 0, r(O_KI, 64) + r(O_KR, 32))])
    slab([("QI", j, r(O_QI + 128 * j, 128)) for j in range(4)])
    slab([("QB", j, r(O_QKV + 128 * j, 128)) for j in range(4)])
    slab([("KB", j, r(O_QKV + 512 + 128 * j, 128)) for j in range(4)])
    slab([("KI", 0, r(O_KI, 64) + r(O_KI, 64)), ("CKV", 0, r(O_CKV, 128))])
    slab([("VB", 0, r(O_QKV + 1024, 512))])
    slab([("WI", 0, r(O_WI, 8))])
    return np.array(cols, dtype=np.int64), slabs


def _consts():
    c = {}
    c["ident"] = np.eye(128, dtype=np.float32)
    tri = np.zeros((128, 128), np.float32)
    tri[np.triu_indices(128, 1)] = NEG
    c["tri"] = tri
    i = np.arange(128)[:, None]
    cc = np.arange(STRIP_W)[None, :]
    dl = cc - i - 384
    m = ((dl >= 0) & (dl <= 128)).astype(np.float32) + ((dl >= 0) & (dl % 4 == 0) & (dl <= 512)) + \
        ((dl >= 0) & (dl % 16 == 0) & (dl <= 2048))
    c["strip"] = m.astype(np.float32)
    p32 = np.zeros((128, 128), np.float32)
    for k in range(16):
        p32[64 + k, 80 + k] = 1
        p32[80 + k, 64 + k] = 1
    p64 = np.zeros((128, 128), np.float32)
    for hb in (0, 64):
        for k in range(32):
            p64[hb + k, hb + 32 + k] = 1
            p64[hb + 32 + k, hb + k] = 1
    c["p32"], c["p64"] = p32, p64
    c["cvec"] = np.tile((2.0 ** -(np.arange(BIS_ITERS) + 1.0))[None, :], (128, 1)).astype(np.float32)
    pos = np.arange(S, dtype=np.float32)[None, :]
    inv32 = (10000.0 ** (-np.arange(0, 32, 2, dtype=np.float32) / 32)).astype(np.float32)
    inv64 = (10000.0 ** (-np.arange(0, 64, 2, dtype=np.float32) / 64)).astype(np.float32)
    cos32 = np.ones((128, S), np.float32)
    sin32 = np.zeros((128, S), np.float32)
    a32 = inv32[:, None] * pos
    cos32[64:80], cos32[80:96] = np.cos(a32), np.cos(a32)
    sin32[64:80], sin32[80:96] = -np.sin(a32), np.sin(a32)
    a64 = inv64[:, None] * pos
    cos64 = np.zeros((128, S), np.float32)
    sin64 = np.zeros((128, S), np.float32)
    for hb in (0, 64):
        cos64[hb:hb + 32], cos64[hb + 32:hb + 64] = np.cos(a64), np.cos(a64)
        sin64[hb:hb + 32], sin64[hb + 32:hb + 64] = -np.sin(a64), np.sin(a64)
    c["cos32"], c["sin32"], c["cos64"], c["sin64"] = cos32, sin32, cos64, sin64
    return c


class _Stop(Exception):
    pass


def build_nc(nb=NB, stop=None, dbg_step=0):
    nc = bass.Bass("TRN2", target_bir_lowering=False)
    win_cols, slabs = _win_plan()
    NCOL = len(win_cols)

    def din(name, shape, dt=F32):
        return nc.dram_tensor(name, list(shape), dt, kind="ExternalInput").ap()

    x_d = din("x", [nb, S, D])
    mem_d = din("mem", [nb, 256, D])
    win_d = din("win", [D, NCOL])
    wuk_d = din("wukT", [128, 512])
    wuv_d = din("wuv", [128, 512])
    wout_d = din("wout", [D, D])
    wq_d = din("wq", [D, D])
    wkv_d = din("wkv", [D, 2 * D])
    wo_d = din("wo", [D, D])
    wup_d = din("wup", [D, 4 * D])
    wdn_d = din("wdn", [4 * D, D])
    gall_d = din("gall", [128, 33])
    gF_d = din("gF", [128, D])
    c_ident = din("ident", [128, 128])
    c_tri = din("tri", [128, 128])
    c_strip = din("strip", [128, STRIP_W])
    c_p32 = din("p32", [128, 128])
    c_p64 = din("p64", [128, 128])
    c_cvec = din("cvec", [128, BIS_ITERS])
    c_tab = {k: din(k, [128, S]) for k in ("cos32", "sin32", "cos64", "sin64")}
    out_d = nc.dram_tensor("out", [nb, S, D], F32, kind="ExternalOutput").ap()
    qa_s = nc.dram_tensor("qa_s", [8, 96, S], BF16, kind="ExternalOutput").ap()
    qi_s = nc.dram_tensor("qi_s", [4, 128, S], BF16, kind="ExternalOutput").ap()
    qb_s = nc.dram_tensor("qb_s", [4, 128, S], BF16, kind="ExternalOutput").ap()

    with ExitStack() as es:
        def sb(name, shape, dt):
            return es.enter_context(nc.sbuf_tensor("sb_" + name, list(shape), dt))

        def sem(name):
            return es.enter_context(nc.semaphore(name))

        sems = {n: sem("s_" + n) for n in ("pe", "act", "dve", "pool", "sp")}
        P = Prog(nc, sems, [sem(f"dq{i}") for i in range(40)])
        R = ResReg()

        HT = sb("HT", [128, 8, S], BF16)
        identb = sb("identb", [128, 128], BF16)
        onesb = sb("onesb", [128, 128], BF16)
        tri = sb("tri", [128, 128], F32)
        strip = sb("strip", [128, STRIP_W], BF16)
        p32 = sb("p32", [128, 128], BF16)
        p64 = sb("p64", [128, 128], BF16)
        cvec = sb("cvec", [128, BIS_ITERS], F32)
        gall = sb("gall", [128, 33], F32)
        wukT = sb("wukT", [128, 512], BF16)
        wuv = sb("wuv", [128, 512], BF16)
        weff = sb("weff", [128, 16, 8], F32)
        stat = sb("stat", [128, 64], F32)
        ARENA_B = 156 * 1024
        arena = sb("arena", [128, ARENA_B // 2], BF16)
        psum = [es.enter_context(nc.psum_tensor(f"ps{i}", [128, 512], F32)) for i in range(8)]
        PB = [(psum[i], R("psum", i)) for i in range(8)]

        class Carver:
            def __init__(self):
                self.off = 0

            def take(self, shape, dt):
                esz = 4 if dt == F32 else 2
                n = int(np.prod(shape[1:]))
                nbytes = n * esz
                nbytes_al = (nbytes + 63) // 64 * 64
                a = arena[:, self.off // 2: (self.off + nbytes) // 2]
                self.off += nbytes_al
                assert self.off <= ARENA_B, ("arena overflow", self.off)
                if dt == F32:
                    a = a.bitcast(F32)
                if len(shape) == 3:
                    a = a.rearrange("p (a b) -> p a b", a=shape[1])
                elif len(shape) == 4:
                    a = a.rearrange("p (a b c) -> p a b c", a=shape[1], b=shape[2])
                return a

        def bfv(ps_ap):
            return ps_ap.bitcast(BF16)

        def ld(dst, src, key, queue="sp"):
            P.dma(queue, I("dma_start", out=dst, in_=src), key, writes=[R(key)])

        ld(identb[:], c_ident[:, :], "identb", "pool")
        P.dma("pool", I("dma_start", out=strip[:, 0:1408], in_=c_strip[:, 0:1408]), "strip", writes=[R("strip")])
        P.dma("pool", I("dma_start", out=strip[:, 1408:STRIP_W], in_=c_strip[:, 1408:STRIP_W]), "strip2", writes=[R("strip2")])
        ld(p32[:], c_p32[:, :], "p32", "pool")
        ld(p64[:], c_p64[:, :], "p64", "pool")
        ld(wukT[:], wuk_d[:, :], "wukT", "pool")
        ld(wuv[:], wuv_d[:, :], "wuv", "pool")
        ld(tri[:], c_tri[:, :], "tri")
        ld(cvec[:], c_cvec[:, :], "cvec")
        ld(gall[:], gall_d[:, :], "gall")
        P.op("dve", I("memset", onesb[:], 1.0), writes=[R("onesb")])
        CONST_R = [R(k) for k in ("identb", "strip", "strip2", "p32", "p64", "wukT", "wuv", "tri", "cvec", "gall", "onesb")]
        P.barrier()

        out_toks = []

        def evac(i, dst, src, reads, writes):
            if i % 2 == 0:
                P.op("act", I("activation", out=dst, in_=src, func=AF.Copy), reads=reads, writes=writes)
            else:
                P.op("dve", I("tensor_copy", out=dst, in_=src), reads=reads, writes=writes)

        def norm_tile(src, src_res, gcol, i, junk, junk_r, xn, xn_r, tbank, dstT, dst_res, ssc):
            ss = stat[:, ssc:ssc + 1]
            rs = stat[:, ssc + 1:ssc + 2]
            ss_r, rs_r = R("stat", ssc), R("stat", ssc + 1)
            P.op("act", I("activation", out=junk, in_=src, func=AF.Square, accum_out=ss),
                 reads=[src_res], writes=[junk_r, ss_r])
            P.op("act", I("activation", out=rs, in_=ss, func=AF.Sqrt, scale=1.0 / D, bias=1e-6),
                 reads=[ss_r], writes=[rs_r])
            P.op("dve", I("reciprocal", out=rs, in_=rs), reads=[rs_r], writes=[rs_r])
            P.op("act", I("activation", out=xn, in_=src, func=AF.Copy, scale=rs),
                 reads=[src_res, rs_r], writes=[xn_r])
            tb_ap, tb_r = tbank
            tv = bfv(tb_ap[:, :]).rearrange("p (a b) -> p a b", a=8)
            P.op("pe", [(I("transpose", out=tv[:, c, :], in_=xn[:, c * 128:(c + 1) * 128], identity=identb[:]))
                        for c in range(8)], reads=[xn_r, R("identb")], writes=[tb_r])
            P.op("dve", I("tensor_tensor", out=dstT, in0=tv,
                                                   in1=gall[:, gcol:gcol + 8].unsqueeze(2).to_broadcast([128, 8, 128]),
                                                   op=ALU.mult),
                 reads=[tb_r, R("gall")], writes=[dst_res])

        def load_w(dst, src_rows_cols, key, res):
            P.dma("pool", I("dma_start", out=dst, in_=src_rows_cols), key, writes=[res])

        def chk(tag):
            if stop == tag:
                raise _Stop()

        try:
          for b in range(nb):
              P.barrier()
              P.new_epoch({n: sem(f"s{b}_" + n) for n in ("pe", "act", "dve", "pool", "sp")}, R)
              cv = Carver()
              Kp = cv.take([128, 8, S], BF16)
              Vp = cv.take([128, 16, 768], BF16)
              kidx = cv.take([128, S], BF16)
              kbT = cv.take([128, 4, S], BF16)
              Vb = cv.take([128, 16, 768], BF16)
              keys_end = cv.off
              xs = [cv.take([128, D], F32) for _ in range(2)]
              junk = cv.take([128, D], BF16)
              xn = [cv.take([128, D], BF16) for _ in range(2)]
              cv.off = keys_end
              tabc = cv.take([128, S], F32)
              tabs_ = cv.take([128, S], F32)
              wsl = [cv.take([128, 8, 512], BF16) for _ in range(2)]
              qbuf = [cv.take([128, 512], BF16) for _ in range(2)]
              t1b = [cv.take([128, 512], F32) for _ in range(2)]
              t2b = [cv.take([128, 512], F32) for _ in range(2)]
              stg = [cv.take([128, 512], BF16) for _ in range(2)]
              ckf = cv.take([128, 512], F32)
              sqb = cv.take([128, 512], BF16)
              rtb = cv.take([128, 512], F32)
              ckvT = cv.take([128, S], BF16)

              for (T, nm) in ((Vp, "Vp"), (Vb, "Vb")):
                  tv4 = T.rearrange("p k (a c) -> p k a c", a=4)
                  P.op("pool", I("memset", tv4[:, :, :, 64:128], 1.0),
                       writes=[R(nm, kb) for kb in range(16)])
              for i in range(16):
                  P.dma("sp", I("dma_start", out=xs[i % 2][:, :], in_=x_d[b, i * 128:(i + 1) * 128, :]),
                        f"xs{i % 2}", writes=[R("xs", i % 2)])
                  norm_tile(xs[i % 2][:, :], R("xs", i % 2), 0, i, junk[:, :], R("junk"), xn[i % 2][:, :], R("xn", i % 2),
                            PB[7], HT[:, :, i * 128:(i + 1) * 128], R("HT", i // 4), 2 * (i % 2))
              P.barrier()
              chk("norm1")
              cur_tab = [None]
              mmb = Rot([PB[0], PB[1], PB[2]])
              ppb = Rot([PB[3], PB[4]])
              msb = Rot([PB[5], PB[6]])
              ecnt = [0]
              for si, (soff, sw, chunks) in enumerate(slabs):
                  if si > 0:
                      chk(f"slab{si - 1}")
                  ws, ws_r = wsl[si % 2], R("wsl", si % 2)
                  load_w(ws[:, :, 0:sw], win_d[:, soff:soff + sw].rearrange("(c p) n -> p c n", p=128), f"wsl{si % 2}", ws_r)
                  for ch in chunks:
                      kind, idx, coff, M = ch["kind"], ch["idx"], ch["coff"], ch["M"]
                      if kind in ("VB", "WI"):
                          for tb in range(16):
                              bk, bk_r = mmb.next()
                              P.op("pe", [(I("matmul",
                                  bk[:, 0:M], lhsT=HT[:, c, tb * 128:(tb + 1) * 128], rhs=ws[:, c, coff:coff + M],
                                  start=(c == 0), stop=(c == 7))) for c in range(8)],
                                  reads=[ws_r, R("HT", tb // 4)], writes=[bk_r])
                              if kind == "WI":
                                  P.op("act", I("activation",
                                      out=weff[:, tb, :], in_=bk[:, 0:8], func=AF.Copy, scale=float(8 ** -0.5 * 64 ** -0.5)),
                                      reads=[bk_r], writes=[R("weff", tb)])
                              else:
                                  src4 = bk[:, :].rearrange("p (a c) -> p a c", a=4)
                                  dst4 = Vb[:, tb, :].rearrange("p (a c) -> p a c", a=4)
                                  P.op("act", I("activation", out=dst4[:, :, 0:64], in_=src4[:, :, 0:64], func=AF.Copy),
                                       reads=[bk_r], writes=[R("Vb", tb)])
                                  P.op("dve", I("tensor_copy", out=dst4[:, :, 128:192], in_=src4[:, :, 64:128]),
                                       reads=[bk_r], writes=[R("Vb", tb)])
                          continue
                      for tt in range(4):
                          tsl = slice(tt * 512, (tt + 1) * 512)
                          bk, bk_r = mmb.next()
                          P.op("pe", [(I("matmul",
                              bk[0:M, :], lhsT=ws[:, c, coff:coff + M], rhs=HT[:, c, tsl],
                              start=(c == 0), stop=(c == 7))) for c in range(8)],
                              reads=[ws_r, R("HT", tt)], writes=[bk_r])
                          if dbg_step == 1:
                              raise _Stop()
                          if kind == "CKV":
                              P.op("act", I("activation", out=ckf[:, :], in_=bk[:, :], func=AF.Copy),
                                   reads=[bk_r], writes=[R("ckf")])
                              P.op("act", I("activation", out=sqb[:, :], in_=bk[:, :], func=AF.Square),
                                   reads=[bk_r], writes=[R("sqb")])
                              sm, sm_r = msb.next()
                              P.op("pe", I("matmul", sm[:, :], lhsT=onesb[:, :], rhs=sqb[:, :], start=True, stop=True),
                                   reads=[R("sqb"), R("onesb")], writes=[sm_r])
                              P.op("act", I("activation", out=rtb[:, :], in_=sm[:, :], func=AF.Sqrt,
                                                                          scale=1.0 / 128, bias=1e-6),
                                   reads=[sm_r], writes=[R("rtb")])
                              P.op("dve", I("reciprocal", out=rtb[:, :], in_=rtb[:, :]), reads=[R("rtb")], writes=[R("rtb")])
                              P.op("dve", I("scalar_tensor_tensor",
                                  out=ckvT[:, tsl], in0=ckf[:, :], scalar=gall[:, 32:33], in1=rtb[:, :],
                                  op0=ALU.mult, op1=ALU.mult),
                                  reads=[R("ckf"), R("rtb"), R("gall")], writes=[R("ckvT", tt)])
                              for hh in range(8):
                                  kb_, kb_r = msb.next()
                                  P.op("pe", I("matmul",
                                      kb_[0:64, :], lhsT=wukT[:, hh * 64:(hh + 1) * 64], rhs=ckvT[:, tsl], start=True, stop=True),
                                      reads=[R("ckvT", tt), R("wukT")], writes=[kb_r])
                                  ecnt[0] += 1
                                  evac(ecnt[0], Kp[0:64, hh, tsl], kb_[0:64, :], [kb_r], [R("Kp", hh, tt)])
                              for j in range(4):
                                  kb = tt * 4 + j
                                  vb_, vb_r = msb.next()
                                  P.op("pe", I("matmul",
                                      vb_[:, :], lhsT=ckvT[:, kb * 128:(kb + 1) * 128], rhs=wuv[:, :], start=True, stop=True),
                                      reads=[R("ckvT", tt), R("wuv")], writes=[vb_r])
                                  src4 = vb_[:, :].rearrange("p (a c) -> p a c", a=4)
                                  dst4 = Vp[:, kb, :].rearrange("p (a c) -> p a c", a=4)
                                  P.op("act", I("activation", out=dst4[:, :, 0:64], in_=src4[:, :, 0:64], func=AF.Copy),
                                       reads=[vb_r], writes=[R("Vp", kb)])
                                  P.op("dve", I("tensor_copy", out=dst4[:, :, 128:192], in_=src4[:, :, 64:128]),
                                       reads=[vb_r], writes=[R("Vp", kb)])
                              continue
                          t32 = kind in ("QA", "KR")
                          tk = "32" if t32 else "64"
                          if cur_tab[0] != tk:
                              cur_tab[0] = tk
                              P.dma("sp", I("dma_start", out=tabc[:, :], in_=c_tab["cos" + tk][:, :]), "tabc", writes=[R("tabc")])
                              P.dma("sp", I("dma_start", out=tabs_[:, :], in_=c_tab["sin" + tk][:, :]), "tabs", writes=[R("tabs")])
                          cosT, sinT = tabc, tabs_
                          cos_r, sin_r = R("tabc"), R("tabs")
                          pm, pm_r = (p32, R("p32")) if t32 else (p64, R("p64"))
                          u = ecnt[0] = ecnt[0] + 1
                          qb_, qb_r = qbuf[u % 2], R("qbuf", u % 2)
                          t1, t1_r = t1b[u % 2], R("t1b", u % 2)
                          t2, t2_r = t2b[u % 2], R("t2b", u % 2)
                          P.op("act", I("activation", out=qb_[0:M, :], in_=bk[0:M, :], func=AF.Copy),
                               reads=[bk_r], writes=[qb_r])
                          if dbg_step == 2:
                              raise _Stop()
                          pb_, pb_r = ppb.next()
                          P.op("pe", I("matmul",
                              pb_[0:M, :], lhsT=pm[0:M, 0:M], rhs=qb_[0:M, :], start=True, stop=True),
                              reads=[qb_r, pm_r], writes=[pb_r])
                          if dbg_step == 3:
                              raise _Stop()
                          import os as _os
                          _v = _os.environ.get("DBG_VAR", "")
                          if _v == "v1":
                              P.op("dve", I("tensor_copy", out=t1[0:M, :], in_=bk[0:M, :]), reads=[bk_r], writes=[t1_r])
                          elif _v == "v5":
                              P.op("dve", I("tensor_copy", out=t1[0:M, :], in_=bk[0:M, :]), reads=[bk_r, qb_r, pb_r], writes=[t1_r])
                          elif _v == "v2":
                              P.op("dve", I("tensor_tensor", out=t1[0:M, :], in0=bk[0:M, :], in1=t2[0:M, :], op=ALU.mult),
                                   reads=[bk_r], writes=[t1_r])
                          elif _v == "v3":
                              P.op("dve", I("tensor_copy", out=t1[0:M, :], in_=cosT[0:M, tsl]), reads=[cos_r], writes=[t1_r])
                          elif _v == "v4":
                              P.op("dve", I("tensor_tensor", out=t1[0:M, :], in0=bk[0:M, :], in1=cosT[0:M, tsl], op=ALU.mult),
                                   reads=[bk_r], writes=[t1_r])
                          else:
                              P.op("dve", I("tensor_tensor",
                                  out=t1[0:M, :], in0=bk[0:M, :], in1=cosT[0:M, tsl], op=ALU.mult),
                                  reads=[bk_r, cos_r], writes=[t1_r])
                          if dbg_step == 4:
                              raise _Stop()
                          P.op("dve", I("tensor_tensor",
                              out=t2[0:M, :], in0=pb_[0:M, :], in1=sinT[0:M, tsl], op=ALU.mult),
                              reads=[pb_r, sin_r], writes=[t2_r])
                          if dbg_step == 5:
                              raise _Stop()
                          if kind == "KR":
                              for hh in range(8):
                                  P.op("pool", I("tensor_tensor",
                                      out=Kp[64:96, hh, tsl], in0=t1[64:96, :], in1=t2[64:96, :], op=ALU.add),
                                      reads=[t1_r, t2_r], writes=[R("Kp2", hh, tt)])
                          elif kind == "KI":
                              P.op("pool", I("tensor_tensor",
                                  out=kidx[:, tsl], in0=t1[:, :], in1=t2[:, :], op=ALU.add),
                                  reads=[t1_r, t2_r], writes=[R("kidx", tt)])
                          elif kind == "KB":
                              P.op("pool", I("tensor_tensor",
                                  out=kbT[:, idx, tsl], in0=t1[:, :], in1=t2[:, :], op=ALU.add),
                                  reads=[t1_r, t2_r], writes=[R("kbT", idx, tt)])
                          else:
                              sg, sg_r = stg[u % 2], R("stg", u % 2)
                              P.op("pool", I("tensor_tensor",
                                  out=sg[0:M, :], in0=t1[0:M, :], in1=t2[0:M, :], op=ALU.add),
                                  reads=[t1_r, t2_r], writes=[sg_r])
                              if dbg_step == 6:
                                  raise _Stop()
                              dst = {"QA": qa_s, "QI": qi_s, "QB": qb_s}[kind]
                              P.dma("sp", I("dma_start",
                                  out=dst[idx, 0:M, tsl], in_=sg[0:M, :]), f"stg{u % 2}",
                                  reads=[sg_r], writes=[R("scr", kind, idx, tt)])
              P.barrier()

              chk("proj")
              cv = Carver()
              cv.off = keys_end
              qaq = cv.take([128, 4, 512], BF16)
              qiq = cv.take([128, 4, 512], BF16)
              qbq = qiq
              idx_sb = cv.take([128, S], F32)
              Rb = [cv.take([128, 512], BF16) for _ in range(2)]
              diag = cv.take([128, 8, 128], BF16)
              Mk = cv.take([128, S], BF16)
              maskT = cv.take([128, 16, 512], BF16)
              Eb = [cv.take([128, 512], BF16) for _ in range(2)]
              Pmb = [cv.take([128, 512], BF16) for _ in range(2)]
              dsh = [cv.take([128, 512], F32)] * 2
              bis = cv.take([128, 32], F32)
              steps = cv.take([128, BIS_ITERS], F32)

              Lb = Rot([PB[0], PB[1]])
              accb = PB[2]
              Tb = PB[3]
              Sb = Rot([PB[4], PB[5]])
              Ob = Rot([PB[6], PB[7]])
              sc_a = float((64 + 32) ** -0.5)
              sc_b = 0.125
              ucount = [0]

              for qt in range(4):
                  qsl = slice(qt * 512, (qt + 1) * 512)
                  def load_qaq(hf, qsl=qsl, qt=qt):
                      P.dma("sp", I("dma_start", out=qaq[0:96, :, :], in_=qa_s[4 * hf:4 * hf + 4, :, qsl].rearrange("a r t -> r a t")),
                            "qaq", reads=[R("scr", "QA", i, qt) for i in range(8)], writes=[R("qaq")])
                  load_qaq(0)
                  P.dma("sp", I("dma_start", out=qiq[:, :, :], in_=qi_s[:, :, qsl].rearrange("a r t -> r a t")),
                        "qiq", reads=[R("scr", "QI", i, qt) for i in range(4)], writes=[R("qiq")])
                  P.op("pool", I("memset", maskT[:, :, :], 0.0), writes=[R("maskT")])
                  for tbl in range(4):
                      tb = 4 * qt + tbl
                      lo = tbl * 128
                      n = 128 * (tb + 1)
                      for hh in range(8):
                          P.op("pool", I("tensor_scalar",
                              out=diag[:, hh, :], in0=identb[:, :], scalar1=weff[:, tb, hh:hh + 1], scalar2=None, op0=ALU.mult),
                              reads=[R("identb"), R("weff", tb)], writes=[R("diag", hh)])
                      for st in range(qt + 1):
                          wd = min(512, n - 512 * st)
                          acc, acc_r = accb

                          def emitL(hh, st=st, wd=wd):
                              lb, lb_r = Lb.next()
                              pr = (hh % 2) * 64
                              P.op("pe", I("matmul",
                                  lb[:, 0:wd], lhsT=qiq[pr:pr + 64, hh // 2, lo:lo + 128],
                                  rhs=kidx[pr:pr + 64, st * 512:st * 512 + wd], start=True, stop=True),
                                  reads=[R("qiq"), R("kidx", st)], writes=[lb_r])
                              rb, rb_r = Rb[hh % 2], R("Rb", hh % 2)
                              P.op("act", I("activation", out=rb[:, 0:wd], in_=lb[:, 0:wd], func=AF.Relu),
                                   reads=[lb_r], writes=[rb_r])

                          def emitA(hh, wd=wd):
                              rb, rb_r = Rb[hh % 2], R("Rb", hh % 2)
                              P.op("pe", I("matmul",
                                  acc[:, 0:wd], lhsT=diag[:, hh, :], rhs=rb[:, 0:wd], start=(hh == 0), stop=(hh == 7)),
                                  reads=[rb_r, R("diag", hh)], writes=[acc_r])

                          emitL(0)
                          for hh in range(8):
                              if hh + 1 < 8:
                                  emitL(hh + 1)
                              emitA(hh)
                          c0 = st * 512
                          if st == qt:
                              if wd > 128:
                                  P.op("act", I("activation",
                                      out=idx_sb[:, c0:c0 + wd - 128], in_=acc[:, 0:wd - 128], func=AF.Copy),
                                      reads=[acc_r], writes=[R("idx")])
                              P.op("dve", I("tensor_tensor",
                                  out=idx_sb[:, c0 + wd - 128:c0 + wd], in0=acc[:, wd - 128:wd], in1=tri[:, :], op=ALU.add),
                                  reads=[acc_r, R("tri")], writes=[R("idx")])
                          else:
                              P.op("act", I("activation", out=idx_sb[:, c0:c0 + 512], in_=acc[:, :], func=AF.Copy),
                                   reads=[acc_r], writes=[R("idx")])
                      thr = bis[:, 0:1]
                      if tb >= 2:
                          mx, mn, W, cnt, a_ = bis[:, 1:2], bis[:, 2:3], bis[:, 3:4], bis[:, 4:5], bis[:, 5:6]
                          RB = R("bis")
                          P.op("dve", I("tensor_reduce", out=mx, in_=idx_sb[:, 0:n], op=ALU.max, axis=AX.X),
                               reads=[R("idx")], writes=[RB])
                          P.op("dve", I("tensor_reduce", out=mn, in_=idx_sb[:, 0:n - 128], op=ALU.min, axis=AX.X),
                               reads=[R("idx")], writes=[RB])
                          P.op("dve", I("tensor_tensor", out=W, in0=mx, in1=mn, op=ALU.subtract), reads=[RB], writes=[RB])
                          P.op("dve", I("tensor_tensor", out=thr, in0=mx, in1=mn, op=ALU.add), reads=[RB], writes=[RB])
                          P.op("dve", I("tensor_scalar", out=thr, in0=thr, scalar1=0.5, scalar2=None, op0=ALU.mult),
                               reads=[RB], writes=[RB])
                          P.op("dve", I("tensor_scalar", out=steps[:, :], in0=cvec[:, :], scalar1=W, scalar2=None, op0=ALU.mult),
                               reads=[RB, R("cvec")], writes=[RB])
                          for it in range(BIS_ITERS):
                              P.op("dve", I("tensor_scalar",
                                  out=Mk[:, 0:n], in0=idx_sb[:, 0:n], scalar1=thr, scalar2=None,
                                  op0=ALU.is_ge, op1=ALU.add, accum_out=cnt),
                                  reads=[R("idx"), RB], writes=[R("Mk"), RB])
                              P.op("dve", I("tensor_scalar", out=a_, in0=cnt, scalar1=255.5, scalar2=0.5,
                                                                     op0=ALU.is_ge, op1=ALU.subtract),
                                   reads=[RB], writes=[RB])
                              P.op("dve", I("scalar_tensor_tensor",
                                  out=thr, in0=a_, scalar=steps[:, it:it + 1], in1=thr, op0=ALU.mult, op1=ALU.add),
                                  reads=[RB], writes=[RB])
                          P.op("dve", I("tensor_tensor", out=thr, in0=thr, in1=steps[:, BIS_ITERS - 1:BIS_ITERS], op=ALU.subtract),
                               reads=[RB], writes=[RB])
                          P.op("dve", I("tensor_scalar", out=Mk[:, 0:n], in0=idx_sb[:, 0:n], scalar1=thr, scalar2=None,
                                                                       op0=ALU.is_ge),
                               reads=[R("idx"), RB], writes=[R("Mk")])
                      else:
                          P.op("dve", I("tensor_scalar", out=Mk[:, 0:n], in0=idx_sb[:, 0:n], scalar1=-1.0e29, scalar2=None,
                                                                       op0=ALU.is_ge),
                               reads=[R("idx")], writes=[R("Mk")])
                      tbk, tbk_r = Tb
                      tv = bfv(tbk[:, :]).rearrange("p (a b) -> p a b", a=8)
                      for g0 in range(0, tb + 1, 8):
                          g1 = min(tb + 1, g0 + 8)
                          P.op("pe", [(I("transpose", out=tv[:, kb - g0, :], in_=Mk[:, kb * 128:(kb + 1) * 128],
                                                                            identity=identb[:, :])) for kb in range(g0, g1)],
                               reads=[R("Mk"), R("identb")], writes=[tbk_r])
                          ucount[0] += 1
                          evac(ucount[0], maskT[:, g0:g1, lo:lo + 128], tv[:, 0:g1 - g0, :], [tbk_r], [R("maskT")])

                  if qt == 0:
                      chk("idx0")
                  P.dma("sp", I("dma_start", out=qbq[:, :, :], in_=qb_s[:, :, qsl].rearrange("a r t -> r a t")),
                        "qiq", reads=[R("scr", "QB", i, qt) for i in range(4)], writes=[R("qiq")])
                  NK = 4 * qt + 4
                  units = [("A", hh, kb) for hh in range(8) for kb in range(NK)] + \
                          [("B", hh, kb) for hh in range(8) for kb in range(NK)]
                  obank = {}

                  def emitQK(u):
                      g, hh, kb = units[u]
                      c0 = max(0, kb - 4 * qt) * 128
                      sbk, sbk_r = Sb.next()
                      pr = (hh % 2) * 64
                      if g == "A":
                          if hh == 4 and kb == 0:
                              load_qaq(1)
                          P.op("pe", I("matmul", sbk[:, c0:512], lhsT=Kp[0:96, hh, kb * 128:(kb + 1) * 128],
                                                        rhs=qaq[0:96, hh % 4, c0:512], start=True, stop=True),
                               reads=[R("Kp", hh, kb // 4), R("Kp2", hh, kb // 4), R("qaq")], writes=[sbk_r])
                      else:
                          P.op("pe", I("matmul", sbk[:, c0:512], lhsT=kbT[pr:pr + 64, hh // 2, kb * 128:(kb + 1) * 128],
                                                        rhs=qbq[pr:pr + 64, hh // 2, c0:512], start=True, stop=True),
                               reads=[R("kbT", hh // 2, kb // 4), R("qiq")], writes=[sbk_r])
                      e, e_r = Eb[u % 2], R("Eb", u % 2)
                      P.op("act", I("activation", out=e[:, c0:512], in_=sbk[:, c0:512], func=AF.Exp,
                                                         scale=(sc_a if g == "A" else sc_b)),
                           reads=[sbk_r], writes=[e_r])
                      pmt, pmt_r = Pmb[u % 2], R("Pmb", u % 2)
                      if g == "A":
                          msk, msk_r = maskT[:, kb, c0:512], R("maskT")
                      else:
                          o_ = 128 * (4 * qt - kb) + 384
                          msk, msk_r = strip[:, o_ + c0:o_ + 512], R("strip")
                      eng = "pool" if u % 2 == 0 else "dve"
                      P.op(eng, I("tensor_tensor", out=pmt[:, c0:512], in0=e[:, c0:512], in1=msk, op=ALU.mult),
                           reads=[e_r, msk_r], writes=[pmt_r])

                  def emitPV(u):
                      g, hh, kb = units[u]
                      c0 = max(0, kb - 4 * qt) * 128
                      if kb == 0:
                          obank[(g, hh)] = Ob.next()
                      ob, ob_r = obank[(g, hh)]
                      pmt, pmt_r = Pmb[u % 2], R("Pmb", u % 2)
                      V = Vp if g == "A" else Vb
                      vo = (hh // 2) * 192 + (hh % 2) * 64
                      P.op("pe", I("matmul", ob[:, c0:512], lhsT=V[:, kb, vo:vo + 128], rhs=pmt[:, c0:512],
                                                    start=(kb == 0), stop=(kb == NK - 1)),
                           reads=[pmt_r, R("Vp" if g == "A" else "Vb", kb)], writes=[ob_r])
                      if kb == NK - 1:
                          ev = (hh % 2 == 0)
                          np_, dp_ = (slice(0, 64), slice(64, 128)) if ev else (slice(64, 128), slice(0, 64))
                          d, d_r = dsh[0], R("dsh", 0)
                          P.op("act", I("activation", out=d[np_, :], in_=ob[dp_, :], func=AF.Copy),
                               reads=[ob_r], writes=[d_r])
                          P.op("dve", I("reciprocal", out=d[np_, :], in_=d[np_, :]), reads=[d_r], writes=[d_r])
                          chunk = (hh // 2) + (0 if g == "A" else 4)
                          P.op("dve", I("tensor_tensor", out=HT[np_, chunk, qsl], in0=ob[np_, :], in1=d[np_, :], op=ALU.mult),
                               reads=[ob_r, d_r], writes=[R("HT", qt)])

                  emitQK(0)
                  for u in range(len(units)):
                      if u + 1 < len(units):
                          emitQK(u + 1)
                      emitPV(u)
              P.barrier()

              chk("attn")
              cv = Carver()
              XR = cv.take([128, 16, D], F32)
              x_end = cv.off
              wA = [cv.take([128, 8, 512], BF16) for _ in range(2)]
              mmb = Rot([PB[0], PB[1], PB[2], PB[3]])
              for q4 in range(4):
                  P.dma("sp", I("dma_start",
                      out=XR[:, q4 * 4:(q4 + 1) * 4, :], in_=x_d[b, q4 * 512:(q4 + 1) * 512, :].rearrange("(n p) d -> p n d", p=128)),
                      f"xr{q4}", writes=[R("XR", t) for t in range(q4 * 4, q4 * 4 + 4)])

              def out_proj(w_d, srcT, src_res_fn, keyp):
                  for half in range(2):
                      ws, ws_r = wA[half], R("wA", half)
                      load_w(ws[:, :, :], w_d[:, half * 512:(half + 1) * 512].rearrange("(c p) n -> p c n", p=128), f"wA{half}", ws_r)
                      for tb in range(16):
                          bk, bk_r = mmb.next()
                          P.op("pe", [(I("matmul",
                              bk[:, :], lhsT=srcT[:, c, tb * 128:(tb + 1) * 128], rhs=ws[:, c, :],
                              start=(c == 0), stop=(c == 7))) for c in range(8)],
                              reads=[ws_r, src_res_fn(tb)], writes=[bk_r])
                          P.op("dve", I("tensor_tensor",
                              out=XR[:, tb, half * 512:(half + 1) * 512], in0=bk[:, :], in1=XR[:, tb, half * 512:(half + 1) * 512],
                              op=ALU.add), reads=[bk_r, R("XR", tb)], writes=[R("XR", tb)])

              out_proj(wout_d, HT, lambda tb: R("HT", tb // 4), "wout")
              P.barrier()

              chk("wout")
              cv = Carver()
              cv.off = x_end
              OC = cv.take([128, 8, S], BF16)
              wA = [cv.take([128, 8, 512], BF16) for _ in range(2)]
              junk = cv.take([128, D], BF16)
              xn = [cv.take([128, D], BF16)] * 2
              ms = [cv.take([128, D], F32)] * 2
              mT = cv.take([128, 8, 256], BF16)
              KcT = cv.take([128, 8, 256], BF16)
              Vc = cv.take([128, 2, D], BF16)
              qcT = cv.take([128, 8, 512], BF16)
              Ec = [cv.take([128, 512], BF16) for _ in range(4)]
              rdn = [cv.take([128, 512], F32)] * 2
              for i in range(16):
                  norm_tile(XR[:, i, :], R("XR", i), 8, i, junk[:, :], R("junk"), xn[0][:, :], R("xn", 0),
                            PB[7], HT[:, :, i * 128:(i + 1) * 128], R("HT", i // 4), 2 * (i % 2))
              for i in range(2):
                  P.dma("sp", I("dma_start", out=ms[i][:, :], in_=mem_d[b, i * 128:(i + 1) * 128, :]), "ms0",
                        writes=[R("ms", 0)])
                  norm_tile(ms[i][:, :], R("ms", 0), 16, i, junk[:, :], R("junk"), xn[0][:, :], R("xn", 0),
                            PB[7], mT[:, :, i * 128:(i + 1) * 128], R("mT"), 2 * (i % 2))
              mmb = Rot([PB[0], PB[1], PB[2]])
              ec = 0
              for half in range(2):
                  ws, ws_r = wA[half], R("wA", half)
                  load_w(ws[:, :, :], wkv_d[:, half * 512:(half + 1) * 512].rearrange("(c p) n -> p c n", p=128), f"wA{half}", ws_r)
                  for j4 in range(4):
                      j = half * 4 + j4
                      bk, bk_r = mmb.next()
                      P.op("pe", [(I("matmul",
                          bk[:, 0:256], lhsT=ws[:, c, j4 * 128:(j4 + 1) * 128], rhs=mT[:, c, :], start=(c == 0), stop=(c == 7)))
                          for c in range(8)], reads=[ws_r, R("mT")], writes=[bk_r])
                      ec += 1
                      evac(ec, KcT[:, j, :], bk[:, 0:256], [bk_r], [R("KcT", j)])
              for half in range(2):
                  ws, ws_r = wA[half], R("wA", half)
                  load_w(ws[:, :, :], wkv_d[:, D + half * 512:D + (half + 1) * 512].rearrange("(c p) n -> p c n", p=128),
                         f"wA{half}", ws_r)
                  for mc in range(2):
                      bk, bk_r = mmb.next()
                      P.op("pe", [(I("matmul",
                          bk[:, :], lhsT=mT[:, c, mc * 128:(mc + 1) * 128], rhs=ws[:, c, :], start=(c == 0), stop=(c == 7)))
                          for c in range(8)], reads=[ws_r, R("mT")], writes=[bk_r])
                      ec += 1
                      evac(ec, Vc[:, mc, half * 512:(half + 1) * 512], bk[:, :], [bk_r], [R("Vc", mc)])
              for half in range(2):
                  load_w(wA[half][:, :, :], wq_d[:, half * 512:(half + 1) * 512].rearrange("(c p) n -> p c n", p=128),
                         f"wA{half}", R("wA", half))
              sbk2 = Rot([PB[3], PB[4]])
              obk = Rot([PB[5], PB[6]])
              dbk = PB[7]
              for tt in range(4):
                  tsl = slice(tt * 512, (tt + 1) * 512)
                  for j in range(8):
                      bk, bk_r = mmb.next()
                      P.op("pe", [(I("matmul",
                          bk[:, :], lhsT=wA[j // 4][:, c, (j % 4) * 128:(j % 4 + 1) * 128], rhs=HT[:, c, tsl],
                          start=(c == 0), stop=(c == 7))) for c in range(8)],
                          reads=[R("wA", j // 4), R("HT", tt)], writes=[bk_r])
                      ec += 1
                      evac(ec, qcT[:, j, :], bk[:, :], [bk_r], [R("qcT", j)])
                  for hh in range(4):
                      for mc in range(2):
                          sk, sk_r = sbk2.next()
                          P.op("pe", [(I("matmul",
                              sk[:, :], lhsT=KcT[:, 2 * hh + jj, mc * 128:(mc + 1) * 128], rhs=qcT[:, 2 * hh + jj, :],
                              start=(jj == 0), stop=(jj == 1))) for jj in range(2)],
                              reads=[R("KcT", 2 * hh), R("KcT", 2 * hh + 1), R("qcT", 2 * hh), R("qcT", 2 * hh + 1)], writes=[sk_r])
                          e, e_r = Ec[(hh % 2) * 2 + mc], R("Ec", (hh % 2) * 2 + mc)
                          P.op("act", I("activation", out=e[:, :], in_=sk[:, :], func=AF.Exp, scale=1.0 / 16),
                               reads=[sk_r], writes=[e_r])
                      e0, e1 = Ec[(hh % 2) * 2], Ec[(hh % 2) * 2 + 1]
                      er = [R("Ec", (hh % 2) * 2), R("Ec", (hh % 2) * 2 + 1)]
                      db, db_r = dbk
                      P.op("pe", [(I("matmul", db[:, :], lhsT=onesb[:, :], rhs=e[:, :],
                                                                  start=(mc == 0), stop=(mc == 1))) for mc, e in ((0, e0), (1, e1))],
                           reads=er + [R("onesb")], writes=[db_r])
                      rd, rd_r = rdn[0], R("rdn", 0)
                      P.op("dve", I("reciprocal", out=rd[:, :], in_=db[:, :]), reads=[db_r], writes=[rd_r])
                      for jj in range(2):
                          ob, ob_r = obk.next()
                          P.op("pe", [(I("matmul",
                              ob[:, :], lhsT=Vc[:, mc, (2 * hh + jj) * 128:(2 * hh + jj + 1) * 128], rhs=e[:, :],
                              start=(mc == 0), stop=(mc == 1))) for mc, e in ((0, e0), (1, e1))],
                              reads=er + [R("Vc", 0), R("Vc", 1)], writes=[ob_r])
                          P.op("dve", I("tensor_tensor",
                              out=OC[:, 2 * hh + jj, tsl], in0=ob[:, :], in1=rd[:, :], op=ALU.mult),
                              reads=[ob_r, rd_r], writes=[R("OC", tt)])
              mmb = Rot([PB[0], PB[1], PB[2], PB[3]])
              P.barrier()

              def out_proj2(w_d, srcT, src_res_fn):
                  for half in range(2):
                      ws, ws_r = wA[half], R("wA", half)
                      load_w(ws[:, :, :], w_d[:, half * 512:(half + 1) * 512].rearrange("(c p) n -> p c n", p=128), f"wA{half}", ws_r)
                      for tb in range(16):
                          bk, bk_r = mmb.next()
                          P.op("pe", [(I("matmul",
                              bk[:, :], lhsT=srcT[:, c, tb * 128:(tb + 1) * 128], rhs=ws[:, c, :],
                              start=(c == 0), stop=(c == 7))) for c in range(8)],
                              reads=[ws_r, src_res_fn(tb)], writes=[bk_r])
                          P.op("dve", I("tensor_tensor",
                              out=XR[:, tb, half * 512:(half + 1) * 512], in0=bk[:, :], in1=XR[:, tb, half * 512:(half + 1) * 512],
                              op=ALU.add), reads=[bk_r, R("XR", tb)], writes=[R("XR", tb)])

              out_proj2(wo_d, OC, lambda tb: R("OC", tb // 4))
              P.barrier()

              chk("cross")
              cv = Carver()
              cv.off = x_end
              junk = cv.take([128, D], BF16)
              xn = [cv.take([128, D], BF16) for _ in range(2)]
              wU = [cv.take([128, 8, 512], BF16) for _ in range(2)]
              wD = [cv.take([128, 4, D], BF16) for _ in range(2)]
              hid = [cv.take([128, 4, 512], BF16) for _ in range(2)]
              rl = [cv.take([128, 512], BF16) for _ in range(2)]
              gF = cv.take([128, D], F32)
              ot = [cv.take([128, D], F32) for _ in range(2)]
              P.dma("sp", I("dma_start", out=gF[:, :], in_=gF_d[:, :]), "gF", writes=[R("gF")])
              for i in range(16):
                  norm_tile(XR[:, i, :], R("XR", i), 24, i, junk[:, :], R("junk"), xn[i % 2][:, :], R("xn", i % 2),
                            PB[7], HT[:, :, i * 128:(i + 1) * 128], R("HT", i // 4), 2 * (i % 2))
              upb = Rot([PB[0], PB[1], PB[2]])
              dnb = Rot([PB[3], PB[4], PB[5], PB[6]])
              cnt = 0
              for fg in range(8):
                  wu, wu_r = wU[fg % 2], R("wU", fg % 2)
                  wd_, wd_r = wD[fg % 2], R("wD", fg % 2)
                  load_w(wu[:, :, :], wup_d[:, fg * 512:(fg + 1) * 512].rearrange("(c p) n -> p c n", p=128), f"wU{fg % 2}", wu_r)
                  load_w(wd_[:, :, :], wdn_d[fg * 512:(fg + 1) * 512, :].rearrange("(c p) n -> p c n", p=128), f"wD{fg % 2}", wd_r)
                  for tt in range(4):
                      tsl = slice(tt * 512, (tt + 1) * 512)
                      cnt += 1
                      hd, hd_r = hid[cnt % 2], R("hid", cnt % 2)
                      for fc in range(4):
                          bk, bk_r = upb.next()
                          P.op("pe", [(I("matmul",
                              bk[:, :], lhsT=wu[:, c, fc * 128:(fc + 1) * 128], rhs=HT[:, c, tsl],
                              start=(c == 0), stop=(c == 7))) for c in range(8)],
                              reads=[wu_r, R("HT", tt)], writes=[bk_r])
                          r_, r_r = rl[fc % 2], R("rl", fc % 2)
                          P.op("act", I("activation", out=r_[:, :], in_=bk[:, :], func=AF.Relu),
                               reads=[bk_r], writes=[r_r])
                          P.op("pool", I("tensor_tensor", out=hd[:, fc, :], in0=r_[:, :], in1=r_[:, :],
                                                                                         op=ALU.mult),
                               reads=[r_r], writes=[hd_r])
                      for t4 in range(4):
                          tb = tt * 4 + t4
                          for half in range(2):
                              bk, bk_r = dnb.next()
                              P.op("pe", [(I("matmul",
                                  bk[:, :], lhsT=hd[:, fc, t4 * 128:(t4 + 1) * 128], rhs=wd_[:, fc, half * 512:(half + 1) * 512],
                                  start=(fc == 0), stop=(fc == 3))) for fc in range(4)],
                                  reads=[hd_r, wd_r], writes=[bk_r])
                              P.op("dve", I("tensor_tensor",
                                  out=XR[:, tb, half * 512:(half + 1) * 512], in0=bk[:, :], in1=XR[:, tb, half * 512:(half + 1) * 512],
                                  op=ALU.add), reads=[bk_r, R("XR", tb)], writes=[R("XR", tb)])
              for i in range(16):
                  src = XR[:, i, :]
                  ssc = 2 * (i % 2)
                  ss, rs = stat[:, ssc:ssc + 1], stat[:, ssc + 1:ssc + 2]
                  ss_r, rs_r = R("stat", ssc), R("stat", ssc + 1)
                  o_, o_r = ot[i % 2], R("ot", i % 2)
                  P.op("act", I("activation", out=junk[:, :], in_=src, func=AF.Square, accum_out=ss),
                       reads=[R("XR", i)], writes=[R("junk"), ss_r])
                  P.op("act", I("activation", out=rs, in_=ss, func=AF.Sqrt, scale=1.0 / D, bias=1e-6),
                       reads=[ss_r], writes=[rs_r])
                  P.op("dve", I("reciprocal", out=rs, in_=rs), reads=[rs_r], writes=[rs_r])
                  P.op("dve", I("scalar_tensor_tensor",
                      out=o_[:, :], in0=src, scalar=rs, in1=gF[:, :], op0=ALU.mult, op1=ALU.mult),
                      reads=[R("XR", i), rs_r, R("gF")], writes=[o_r])
                  out_toks.append(P.dma("sp", I("dma_start", out=out_d[b, i * 128:(i + 1) * 128, :], in_=o_[:, :]),
                                        f"ot{i % 2}", reads=[o_r]))
              P.barrier()

        except _Stop:
            P.barrier()
        P.wait_tokens("sp", out_toks)
        with nc.Block() as block:
            P.emit(block)
    nc._mk_stats = (P.nops, P.nwaits)
    return nc


_NC_CACHE = {}


def kernel(x, mem, norm_mix_g, w_in, kv_norm_g, w_uk, w_uv, w_out, norm_cross_g, norm_mem_g,
           w_q_cross, w_kv_cross, w_o_cross, norm_mlp_g, w_up, w_down, norm_final_g):
    f = lambda a: np.ascontiguousarray(np.asarray(a, dtype=np.float32))
    x, mem = f(x), f(mem)
    win_cols, _ = _win_plan()
    win = f(f(w_in)[0][:, win_cols])
    wukT = f(np.transpose(f(w_uk)[0], (2, 0, 1)).reshape(128, 512))
    wuv = f(np.transpose(f(w_uv)[0], (1, 0, 2)).reshape(128, 512))
    gcol = lambda g: f(g).reshape(8, 128).T
    gall = f(np.concatenate([gcol(norm_mix_g), gcol(norm_cross_g), gcol(norm_mem_g), gcol(norm_mlp_g),
                             f(kv_norm_g).reshape(128, 1)], axis=1))
    gF = f(np.broadcast_to(f(norm_final_g).reshape(1, D), (128, D)))
    consts = _consts()
    if "nc" not in _NC_CACHE:
        _NC_CACHE["nc"] = build_nc(NB)
    nc = _NC_CACHE["nc"]
    shared = dict(win=win, wukT=wukT, wuv=wuv, wout=f(w_out)[0], wq=f(w_q_cross)[0], wkv=f(w_kv_cross)[0],
                  wo=f(w_o_cross)[0], wup=f(w_up)[0], wdn=f(w_down)[0], gall=gall, gF=gF, **consts)
    in_maps = []
    for c in range(NCORES):
        m = dict(shared)
        m["x"] = x[c * NB:(c + 1) * NB]
        m["mem"] = mem[c * NB:(c + 1) * NB]
        in_maps.append(m)
    res = run_bass_kernel_spmd(nc, in_maps, core_ids=list(range(NCORES)))
    return np.concatenate([res.results[c]["out"] for c in range(NCORES)], axis=0).astype(np.float32)
```

```python
import math
from contextlib import ExitStack
import numpy as np
import concourse.bass as bass
import concourse.mybir as mybir
from concourse.bass_utils import run_bass_kernel_spmd

F32 = mybir.dt.float32
BF16 = mybir.dt.bfloat16
AF = mybir.ActivationFunctionType
ALU = mybir.AluOpType
AX = mybir.AxisListType

NCORES = 8
NB = 4
S = 2048
D = 1024
BIS_ITERS = 18
NEG = -1.0e30
STRIP_W = 2816


class Res:
    __slots__ = ("name", "writer", "readers", "excl")

    def __init__(self, name):
        self.name = name
        self.writer = None
        self.readers = []
        self.excl = (name[0] == "psum")


class ResReg:
    def __init__(self):
        self.d = {}

    def __call__(self, *key):
        r = self.d.get(key)
        if r is None:
            r = self.d[key] = Res(key)
        return r


class Eng:
    def __init__(self, name, sem, is_pe=False):
        self.name, self.sem, self.is_pe = name, sem, is_pe
        self.count = 0
        self.ops = []
        self.waited = {}


class Prog:
    def __init__(self, nc, sems, dma_sem_pool):
        self.nc = nc
        self.E = {n: Eng(n, sems[n], is_pe=(n == "pe")) for n in ("pe", "act", "dve", "pool", "sp")}
        self.dma_sems = {}
        self.sem_pool = dma_sem_pool
        self.nwaits = 0
        self.nops = 0

    def _deps(self, reads, writes):
        deps = []
        for r in reads:
            if r.writer is not None:
                deps.append(r.writer)
            if r.excl:
                deps.extend(r.readers)
        for w in writes:
            if w.writer is not None:
                deps.append(w.writer)
            deps.extend(w.readers)
        return deps

    def _waits(self, eng, deps):
        need = {}
        for d in deps:
            key = (d[0], d[1])
            if need.get(key, 0) < d[2]:
                need[key] = d[2]
        waits = []
        for key, v in need.items():
            if key[0] == "e" and key[1] == eng.name and eng.is_pe:
                continue
            if eng.waited.get(key, 0) >= v:
                continue
            eng.waited[key] = v
            if key[0] == "e":
                waits.append((self.E[key[1]].sem, v))
            else:
                waits.append((self.dma_sems[key[1]][0], v * 16))
        self.nwaits += len(waits)
        return waits

    def _commit(self, tok, reads, writes):
        for r in reads:
            if r.excl:
                r.writer = tok
                r.readers = []
            else:
                r.readers.append(tok)
        for w in writes:
            w.writer = tok
            w.readers = []

    def op(self, engname, fns, reads=(), writes=()):
        eng = self.E[engname]
        if isinstance(fns, tuple):
            fns = [fns]
        waits = self._waits(eng, self._deps(reads, writes))
        eng.count += 1
        tok = ("e", engname, eng.count)
        eng.ops.append((waits, fns, ("e", eng.sem)))
        self._commit(tok, reads, writes)
        self.nops += len(fns)
        return tok

    def dma(self, queue, fn, semkey, reads=(), writes=()):
        eng = self.E[queue]
        if semkey not in self.dma_sems:
            self.dma_sems[semkey] = [self.sem_pool.pop(), 0]
        ent = self.dma_sems[semkey]
        deps = self._deps(reads, writes)
        if ent[1] > 0:
            deps.append(("d", semkey, ent[1]))
        waits = self._waits(eng, deps)
        ent[1] += 1
        tok = ("d", semkey, ent[1])
        eng.ops.append((waits, [fn], ("d", ent[0])))
        self._commit(tok, reads, writes)
        self.nops += 1
        return tok

    def wait_tokens(self, engname, toks):
        eng = self.E[engname]
        waits = self._waits(eng, list(toks))
        if waits:
            eng.ops.append((waits, [], None))

    def barrier(self):
        toks = []
        for e in self.E.values():
            if e.count > 0:
                toks.append(("e", e.name, e.count))
        for k, (s, c) in self.dma_sems.items():
            if c > 0:
                toks.append(("d", k, c))
        for e in self.E.values():
            self.wait_tokens(e.name, [t for t in toks if not (t[0] == "e" and t[1] == e.name and e.is_pe)])

    def new_epoch(self, sems, reg):
        for e in self.E.values():
            e.sem = sems[e.name]
            e.count = 0
            e.waited = {k: v for k, v in e.waited.items() if k[0] == "d"}
        for r in reg.d.values():
            if r.writer is not None and r.writer[0] == "e":
                r.writer = None
            r.readers = [t for t in r.readers if t[0] != "e"]

    def emit(self, block):
        def run(eng, h):
            for waits, fns, inc in eng.ops:
                for sem, v in waits:
                    h.wait_ge(sem, v)
                ins = None
                for f in fns:
                    ins = getattr(h, f[0])(*f[1], **f[2])
                if inc is not None and ins is not None:
                    ins.then_inc(inc[1], 1 if inc[0] == "e" else 16)

        block.tensor(lambda h: run(self.E["pe"], h))
        block.scalar(lambda h: run(self.E["act"], h))
        block.vector(lambda h: run(self.E["dve"], h))
        block.gpsimd(lambda h: run(self.E["pool"], h))
        block.sync(lambda h: run(self.E["sp"], h))


def I(name, *a, **k):
    return (name, a, k)


class Rot:
    def __init__(self, items):
        self.items, self.i = items, 0

    def next(self):
        it = self.items[self.i % len(self.items)]
        self.i += 1
        return it


O_QN, O_QR, O_CKV, O_KR, O_QI, O_KI, O_WI, O_QKV = 0, 512, 768, 896, 928, 1440, 1504, 1512


def _win_plan():
    cols = []
    slabs = []

    def slab(chunks_cols):
        off = len(cols)
        chunks = []
        for kind, idx, cc in chunks_cols:
            chunks.append(dict(kind=kind, idx=idx, coff=len(cols) - off, M=len(cc)))
            cols.extend(cc)
        slabs.append((off, len(cols) - off, chunks))

    def qa(h):
        return list(range(O_QN + 64 * h, O_QN + 64 * h + 64)) + list(range(O_QR + 32 * h, O_QR + 32 * h + 32))

    r = lambda a, n: list(range(a, a + n))
    slab([("QA", h, qa(h)) for h in range(0, 4)])
    slab([("QA", h, qa(h)) for h in range(4, 8)] + [("KR", 0, r(O_KI, 64) + r(O_KR, 32))])
    slab([("QI", j, r(O_QI + 128 * j, 128)) for j in range(4)])
    slab([("QB", j, r(O_QKV + 128 * j, 128)) for j in range(4)])
    slab([("KB", j, r(O_QKV + 512 + 128 * j, 128)) for j in range(4)])
    slab([("KI", 0, r(O_KI, 64) + r(O_KI, 64)), ("CKV", 0, r(O_CKV, 128))])
    slab([("VB", 0, r(O_QKV + 1024, 512))])
    slab([("WI", 0, r(O_WI, 8))])
    return np.array(cols, dtype=np.int64), slabs


def _consts():
    c = {}
    c["ident"] = np.eye(128, dtype=np.float32)
    tri = np.zeros((128, 128), np.float32)
    tri[np.triu_indices(128, 1)] = NEG
    c["tri"] = tri
    i = np.arange(128)[:, None]
    cc = np.arange(STRIP_W)[None, :]
    dl = cc - i - 384
    m = ((dl >= 0) & (dl <= 128)).astype(np.float32) + ((dl >= 0) & (dl % 4 == 0) & (dl <= 512)) + \
        ((dl >= 0) & (dl % 16 == 0) & (dl <= 2048))
    c["strip"] = m.astype(np.float32)
    p32 = np.zeros((128, 128), np.float32)
    for k in range(16):
        p32[64 + k, 80 + k] = 1
        p32[80 + k, 64 + k] = 1
    p64 = np.zeros((128, 128), np.float32)
    for hb in (0, 64):
        for k in range(32):
            p64[hb + k, hb + 32 + k] = 1
            p64[hb + 32 + k, hb + k] = 1
    c["p32"], c["p64"] = p32, p64
    c["cvec"] = np.tile((2.0 ** -(np.arange(BIS_ITERS) + 1.0))[None, :], (128, 1)).astype(np.float32)
    pos = np.arange(S, dtype=np.float32)[None, :]
    inv32 = (10000.0 ** (-np.arange(0, 32, 2, dtype=np.float32) / 32)).astype(np.float32)
    inv64 = (10000.0 ** (-np.arange(0, 64, 2, dtype=np.float32) / 64)).astype(np.float32)
    cos32 = np.ones((128, S), np.float32)
    sin32 = np.zeros((128, S), np.float32)
    a32 = inv32[:, None] * pos
    cos32[64:80], cos32[80:96] = np.cos(a32), np.cos(a32)
    sin32[64:80], sin32[80:96] = -np.sin(a32), np.sin(a32)
    a64 = inv64[:, None] * pos
    cos64 = np.zeros((128, S), np.float32)
    sin64 = np.zeros((128, S), np.float32)
    for hb in (0, 64):
        cos64[hb:hb + 32], cos64[hb + 32:hb + 64] = np.cos(a64), np.cos(a64)
        sin64[hb:hb + 32], sin64[hb + 32:hb + 64] = -np.sin(a64), np.sin(a64)
    c["cos32"], c["sin32"], c["cos64"], c["sin64"] = cos32, sin32, cos64, sin64
    return c


class _Stop(Exception):
    pass


def build_nc(nb=NB, stop=None, dbg_step=0):
    nc = bass.Bass("TRN2", target_bir_lowering=False)
    win_cols, slabs = _win_plan()
    NCOL = len(win_cols)

    def din(name, shape, dt=F32):
        return nc.dram_tensor(name, list(shape), dt, kind="ExternalInput").ap()

    x_d = din("x", [nb, S, D])
    mem_d = din("mem", [nb, 256, D])
    win_d = din("win", [D, NCOL])
    wuk_d = din("wukT", [128, 512])
    wuv_d = din("wuv", [128, 512])
    wout_d = din("wout", [D, D])
    wq_d = din("wq", [D, D])
    wkv_d = din("wkv", [D, 2 * D])
    wo_d = din("wo", [D, D])
    wup_d = din("wup", [D, 4 * D])
    wdn_d = din("wdn", [4 * D, D])
    gall_d = din("gall", [128, 33])
    gF_d = din("gF", [128, D])
    c_ident = din("ident", [128, 128])
    c_tri = din("tri", [128, 128])
    c_strip = din("strip", [128, STRIP_W])
    c_p32 = din("p32", [128, 128])
    c_p64 = din("p64", [128, 128])
    c_cvec = din("cvec", [128, BIS_ITERS])
    c_tab = {k: din(k, [128, S]) for k in ("cos32", "sin32", "cos64", "sin64")}
    out_d = nc.dram_tensor("out", [nb, S, D], F32, kind="ExternalOutput").ap()
    qa_s = nc.dram_tensor("qa_s", [8, 96, S], BF16, kind="ExternalOutput").ap()
    qi_s = nc.dram_tensor("qi_s", [4, 128, S], BF16, kind="ExternalOutput").ap()
    qb_s = nc.dram_tensor("qb_s", [4, 128, S], BF16, kind="ExternalOutput").ap()

    with ExitStack() as es:
        def sb(name, shape, dt):
            return es.enter_context(nc.sbuf_tensor("sb_" + name, list(shape), dt))

        def sem(name):
            return es.enter_context(nc.semaphore(name))

        sems = {n: sem("s_" + n) for n in ("pe", "act", "dve", "pool", "sp")}
        P = Prog(nc, sems, [sem(f"dq{i}") for i in range(40)])
        R = ResReg()

        HT = sb("HT", [128, 8, S], BF16)
        identb = sb("identb", [128, 128], BF16)
        onesb = sb("onesb", [128, 128], BF16)
        tri = sb("tri", [128, 128], F32)
        strip = sb("strip", [128, STRIP_W], BF16)
        p32 = sb("p32", [128, 128], BF16)
        p64 = sb("p64", [128, 128], BF16)
        cvec = sb("cvec", [128, BIS_ITERS], F32)
        gall = sb("gall", [128, 33], F32)
        wukT = sb("wukT", [128, 512], BF16)
        wuv = sb("wuv", [128, 512], BF16)
        weff = sb("weff", [128, 16, 8], F32)
        stat = sb("stat", [128, 64], F32)
        ARENA_B = 156 * 1024
        arena = sb("arena", [128, ARENA_B // 2], BF16)
        psum = [es.enter_context(nc.psum_tensor(f"ps{i}", [128, 512], F32)) for i in range(8)]
        PB = [(psum[i], R("psum", i)) for i in range(8)]

        class Carver:
            def __init__(self):
                self.off = 0

            def take(self, shape, dt):
                esz = 4 if dt == F32 else 2
                n = int(np.prod(shape[1:]))
                nbytes = n * esz
                nbytes_al = (nbytes + 63) // 64 * 64
                a = arena[:, self.off // 2: (self.off + nbytes) // 2]
                self.off += nbytes_al
                assert self.off <= ARENA_B, ("arena overflow", self.off)
                if dt == F32:
                    a = a.bitcast(F32)
                if len(shape) == 3:
                    a = a.rearrange("p (a b) -> p a b", a=shape[1])
                elif len(shape) == 4:
                    a = a.rearrange("p (a b c) -> p a b c", a=shape[1], b=shape[2])
                return a

        def bfv(ps_ap):
            return ps_ap.bitcast(BF16)

        def ld(dst, src, key, queue="sp"):
            P.dma(queue, I("dma_start", out=dst, in_=src), key, writes=[R(key)])

        ld(identb[:], c_ident[:, :], "identb", "pool")
        P.dma("pool", I("dma_start", out=strip[:, 0:1408], in_=c_strip[:, 0:1408]), "strip", writes=[R("strip")])
        P.dma("pool", I("dma_start", out=strip[:, 1408:STRIP_W], in_=c_strip[:, 1408:STRIP_W]), "strip2", writes=[R("strip2")])
        ld(p32[:], c_p32[:, :], "p32", "pool")
        ld(p64[:], c_p64[:, :], "p64", "pool")
        ld(wukT[:], wuk_d[:, :], "wukT", "pool")
        ld(wuv[:], wuv_d[:, :], "wuv", "pool")
        ld(tri[:], c_tri[:, :], "tri")
        ld(cvec[:], c_cvec[:, :], "cvec")
        ld(gall[:], gall_d[:, :], "gall")
        P.op("dve", I("memset", onesb[:], 1.0), writes=[R("onesb")])
        CONST_R = [R(k) for k in ("identb", "strip", "strip2", "p32", "p64", "wukT", "wuv", "tri", "cvec", "gall", "onesb")]
        P.barrier()

        out_toks = []

        def evac(i, dst, src, reads, writes):
            if i % 2 == 0:
                P.op("act", I("activation", out=dst, in_=src, func=AF.Copy), reads=reads, writes=writes)
            else:
                P.op("dve", I("tensor_copy", out=dst, in_=src), reads=reads, writes=writes)

        def norm_tile(src, src_res, gcol, i, junk, junk_r, xn, xn_r, tbank, dstT, dst_res, ssc):
            ss = stat[:, ssc:ssc + 1]
            rs = stat[:, ssc + 1:ssc + 2]
            ss_r, rs_r = R("stat", ssc), R("stat", ssc + 1)
            P.op("act", I("activation", out=junk, in_=src, func=AF.Square, accum_out=ss),
                 reads=[src_res], writes=[junk_r, ss_r])
            P.op("act", I("activation", out=rs, in_=ss, func=AF.Sqrt, scale=1.0 / D, bias=1e-6),
                 reads=[ss_r], writes=[rs_r])
            P.op("dve", I("reciprocal", out=rs, in_=rs), reads=[rs_r], writes=[rs_r])
            P.op("act", I("activation", out=xn, in_=src, func=AF.Copy, scale=rs),
                 reads=[src_res, rs_r], writes=[xn_r])
            tb_ap, tb_r = tbank
            tv = bfv(tb_ap[:, :]).rearrange("p (a b) -> p a b", a=8)
            P.op("pe", [(I("transpose", out=tv[:, c, :], in_=xn[:, c * 128:(c + 1) * 128], identity=identb[:]))
                        for c in range(8)], reads=[xn_r, R("identb")], writes=[tb_r])
            P.op("dve", I("tensor_tensor", out=dstT, in0=tv,
                                                   in1=gall[:, gcol:gcol + 8].unsqueeze(2).to_broadcast([128, 8, 128]),
                                                   op=ALU.mult),
                 reads=[tb_r, R("gall")], writes=[dst_res])

        def load_w(dst, src_rows_cols, key, res):
            P.dma("pool", I("dma_start", out=dst, in_=src_rows_cols), key, writes=[res])

        def chk(tag):
            if stop == tag:
                raise _Stop()

        try:
          for b in range(nb):
              P.barrier()
              P.new_epoch({n: sem(f"s{b}_" + n) for n in ("pe", "act", "dve", "pool", "sp")}, R)
              cv = Carver()
              Kp = cv.take([128, 8, S], BF16)
              Vp = cv.take([128, 16, 768], BF16)
              kidx = cv.take([128, S], BF16)
              kbT = cv.take([128, 4, S], BF16)
              Vb = cv.take([128, 16, 768], BF16)
              keys_end = cv.off
              xs = [cv.take([128, D], F32) for _ in range(2)]
              junk = cv.take([128, D], BF16)
              xn = [cv.take([128, D], BF16) for _ in range(2)]
              cv.off = keys_end
              tabc = cv.take([128, S], F32)
              tabs_ = cv.take([128, S], F32)
              wsl = [cv.take([128, 8, 512], BF16) for _ in range(2)]
              qbuf = [cv.take([128, 512], BF16) for _ in range(2)]
              t1b = [cv.take([128, 512], F32) for _ in range(2)]
              t2b = [cv.take([128, 512], F32) for _ in range(2)]
              stg = [cv.take([128, 512], BF16) for _ in range(2)]
              ckf = cv.take([128, 512], F32)
              sqb = cv.take([128, 512], BF16)
              rtb = cv.take([128, 512], F32)
              ckvT = cv.take([128, S], BF16)

              for (T, nm) in ((Vp, "Vp"), (Vb, "Vb")):
                  tv4 = T.rearrange("p k (a c) -> p k a c", a=4)
                  P.op("pool", I("memset", tv4[:, :, :, 64:128], 1.0),
                       writes=[R(nm, kb) for kb in range(16)])
              for i in range(16):
                  P.dma("sp", I("dma_start", out=xs[i % 2][:, :], in_=x_d[b, i * 128:(i + 1) * 128, :]),
                        f"xs{i % 2}", writes=[R("xs", i % 2)])
                  norm_tile(xs[i % 2][:, :], R("xs", i % 2), 0, i, junk[:, :], R("junk"), xn[i % 2][:, :], R("xn", i % 2),
                            PB[7], HT[:, :, i * 128:(i + 1) * 128], R("HT", i // 4), 2 * (i % 2))
              P.barrier()
              chk("norm1")
              cur_tab = [None]
              mmb = Rot([PB[0], PB[1], PB[2]])
              ppb = Rot([PB[3], PB[4]])
              msb = Rot([PB[5], PB[6]])
              ecnt = [0]
              for si, (soff, sw, chunks) in enumerate(slabs):
                  if si > 0:
                      chk(f"slab{si - 1}")
                  ws, ws_r = wsl[si % 2], R("wsl", si % 2)
                  load_w(ws[:, :, 0:sw], win_d[:, soff:soff + sw].rearrange("(c p) n -> p c n", p=128), f"wsl{si % 2}", ws_r)
                  for ch in chunks:
                      kind, idx, coff, M = ch["kind"], ch["idx"], ch["coff"], ch["M"]
                      if kind in ("VB", "WI"):
                          for tb in range(16):
                              bk, bk_r = mmb.next()
                              P.op("pe", [(I("matmul",
                                  bk[:, 0:M], lhsT=HT[:, c, tb * 128:(tb + 1) * 128], rhs=ws[:, c, coff:coff + M],
                                  start=(c == 0), stop=(c == 7))) for c in range(8)],
                                  reads=[ws_r, R("HT", tb // 4)], writes=[bk_r])
                              if kind == "WI":
                                  P.op("act", I("activation",
                                      out=weff[:, tb, :], in_=bk[:, 0:8], func=AF.Copy, scale=float(8 ** -0.5 * 64 ** -0.5)),
                                      reads=[bk_r], writes=[R("weff", tb)])
                              else:
                                  src4 = bk[:, :].rearrange("p (a c) -> p a c", a=4)
                                  dst4 = Vb[:, tb, :].rearrange("p (a c) -> p a c", a=4)
                                  P.op("act", I("activation", out=dst4[:, :, 0:64], in_=src4[:, :, 0:64], func=AF.Copy),
                                       reads=[bk_r], writes=[R("Vb", tb)])
                                  P.op("dve", I("tensor_copy", out=dst4[:, :, 128:192], in_=src4[:, :, 64:128]),
                                       reads=[bk_r], writes=[R("Vb", tb)])
                          continue
                      for tt in range(4):
                          tsl = slice(tt * 512, (tt + 1) * 512)
                          bk, bk_r = mmb.next()
                          P.op("pe", [(I("matmul",
                              bk[0:M, :], lhsT=ws[:, c, coff:coff + M], rhs=HT[:, c, tsl],
                              start=(c == 0), stop=(c == 7))) for c in range(8)],
                              reads=[ws_r, R("HT", tt)], writes=[bk_r])
                          if dbg_step == 1:
                              raise _Stop()
                          if kind == "CKV":
                              P.op("act", I("activation", out=ckf[:, :], in_=bk[:, :], func=AF.Copy),
                                   reads=[bk_r], writes=[R("ckf")])
                              P.op("act", I("activation", out=sqb[:, :], in_=bk[:, :], func=AF.Square),
                                   reads=[bk_r], writes=[R("sqb")])
                              sm, sm_r = msb.next()
                              P.op("pe", I("matmul", sm[:, :], lhsT=onesb[:, :], rhs=sqb[:, :], start=True, stop=True),
                                   reads=[R("sqb"), R("onesb")], writes=[sm_r])
                              P.op("act", I("activation", out=rtb[:, :], in_=sm[:, :], func=AF.Sqrt,
                                                                          scale=1.0 / 128, bias=1e-6),
                                   reads=[sm_r], writes=[R("rtb")])
                              P.op("dve", I("reciprocal", out=rtb[:, :], in_=rtb[:, :]), reads=[R("rtb")], writes=[R("rtb")])
                              P.op("dve", I("scalar_tensor_tensor",
                                  out=ckvT[:, tsl], in0=ckf[:, :], scalar=gall[:, 32:33], in1=rtb[:, :],
                                  op0=ALU.mult, op1=ALU.mult),
                                  reads=[R("ckf"), R("rtb"), R("gall")], writes=[R("ckvT", tt)])
                              for hh in range(8):
                                  kb_, kb_r = msb.next()
                                  P.op("pe", I("matmul",
                                      kb_[0:64, :], lhsT=wukT[:, hh * 64:(hh + 1) * 64], rhs=ckvT[:, tsl], start=True, stop=True),
                                      reads=[R("ckvT", tt), R("wukT")], writes=[kb_r])
                                  ecnt[0] += 1
                                  evac(ecnt[0], Kp[0:64, hh, tsl], kb_[0:64, :], [kb_r], [R("Kp", hh, tt)])
                              for j in range(4):
                                  kb = tt * 4 + j
                                  vb_, vb_r = msb.next()
                                  P.op("pe", I("matmul",
                                      vb_[:, :], lhsT=ckvT[:, kb * 128:(kb + 1) * 128], rhs=wuv[:, :], start=True, stop=True),
                                      reads=[R("ckvT", tt), R("wuv")], writes=[vb_r])
                                  src4 = vb_[:, :].rearrange("p (a c) -> p a c", a=4)
                                  dst4 = Vp[:, kb, :].rearrange("p (a c) -> p a c", a=4)
                                  P.op("act", I("activation", out=dst4[:, :, 0:64], in_=src4[:, :, 0:64], func=AF.Copy),
                                       reads=[vb_r], writes=[R("Vp", kb)])
                                  P.op("dve", I("tensor_copy", out=dst4[:, :, 128:192], in_=src4[:, :, 64:128]),
                                       reads=[vb_r], writes=[R("Vp", kb)])
                              continue
                          t32 = kind in ("QA", "KR")
                          tk = "32" if t32 else "64"
                          if cur_tab[0] != tk:
                              cur_tab[0] = tk
                              P.dma("sp", I("dma_start", out=tabc[:, :], in_=c_tab["cos" + tk][:, :]), "tabc", writes=[R("tabc")])
                              P.dma("sp", I("dma_start", out=tabs_[:, :], in_=c_tab["sin" + tk][:, :]), "tabs", writes=[R("tabs")])
                          cosT, sinT = tabc, tabs_
                          cos_r, sin_r = R("tabc"), R("tabs")
                          pm, pm_r = (p32, R("p32")) if t32 else (p64, R("p64"))
                          u = ecnt[0] = ecnt[0] + 1
                          qb_, qb_r = qbuf[u % 2], R("qbuf", u % 2)
                          t1, t1_r = t1b[u % 2], R("t1b", u % 2)
                          t2, t2_r = t2b[u % 2], R("t2b", u % 2)
                          P.op("act", I("activation", out=qb_[0:M, :], in_=bk[0:M, :], func=AF.Copy),
                               reads=[bk_r], writes=[qb_r])
                          if dbg_step == 2:
                              raise _Stop()
                          pb_, pb_r = ppb.next()
                          P.op("pe", I("matmul",
                              pb_[0:M, :], lhsT=pm[0:M, 0:M], rhs=qb_[0:M, :], start=True, stop=True),
                              reads=[qb_r, pm_r], writes=[pb_r])
                          if dbg_step == 3:
                              raise _Stop()
                          import os as _os
                          _v = _os.environ.get("DBG_VAR", "")
                          if _v == "v1":
                              P.op("dve", I("tensor_copy", out=t1[0:M, :], in_=bk[0:M, :]), reads=[bk_r], writes=[t1_r])
                          elif _v == "v5":
                              P.op("dve", I("tensor_copy", out=t1[0:M, :], in_=bk[0:M, :]), reads=[bk_r, qb_r, pb_r], writes=[t1_r])
                          elif _v == "v2":
                              P.op("dve", I("tensor_tensor", out=t1[0:M, :], in0=bk[0:M, :], in1=t2[0:M, :], op=ALU.mult),
                                   reads=[bk_r], writes=[t1_r])
                          elif _v == "v3":
                              P.op("dve", I("tensor_copy", out=t1[0:M, :], in_=cosT[0:M, tsl]), reads=[cos_r], writes=[t1_r])
                          elif _v == "v4":
                              P.op("dve", I("tensor_tensor", out=t1[0:M, :], in0=bk[0:M, :], in1=cosT[0:M, tsl], op=ALU.mult),
                                   reads=[bk_r], writes=[t1_r])
                          else:
                              P.op("dve", I("tensor_tensor",
                                  out=t1[0:M, :], in0=bk[0:M, :], in1=cosT[0:M, tsl], op=ALU.mult),
                                  reads=[bk_r, cos_r], writes=[t1_r])
                          if dbg_step == 4:
                              raise _Stop()
                          P.op("dve", I("tensor_tensor",
                              out=t2[0:M, :], in0=pb_[0:M, :], in1=sinT[0:M, tsl], op=ALU.mult),
                              reads=[pb_r, sin_r], writes=[t2_r])
                          if dbg_step == 5:
                              raise _Stop()
                          if kind == "KR":
                              for hh in range(8):
                                  P.op("pool", I("tensor_tensor",
                                      out=Kp[64:96, hh, tsl], in0=t1[64:96, :], in1=t2[64:96, :], op=ALU.add),
                                      reads=[t1_r, t2_r], writes=[R("Kp2", hh, tt)])
                          elif kind == "KI":
                              P.op("pool", I("tensor_tensor",
                                  out=kidx[:, tsl], in0=t1[:, :], in1=t2[:, :], op=ALU.add),
                                  reads=[t1_r, t2_r], writes=[R("kidx", tt)])
                          elif kind == "KB":
                              P.op("pool", I("tensor_tensor",
                                  out=kbT[:, idx, tsl], in0=t1[:, :], in1=t2[:, :], op=ALU.add),
                                  reads=[t1_r, t2_r], writes=[R("kbT", idx, tt)])
                          else:
                              sg, sg_r = stg[u % 2], R("stg", u % 2)
                              P.op("pool", I("tensor_tensor",
                                  out=sg[0:M, :], in0=t1[0:M, :], in1=t2[0:M, :], op=ALU.add),
                                  reads=[t1_r, t2_r], writes=[sg_r])
                              if dbg_step == 6:
                                  raise _Stop()
                              dst = {"QA": qa_s, "QI": qi_s, "QB": qb_s}[kind]
                              P.dma("sp", I("dma_start",
                                  out=dst[idx, 0:M, tsl], in_=sg[0:M, :]), f"stg{u % 2}",
                                  reads=[sg_r], writes=[R("scr", kind, idx, tt)])
              P.barrier()

              chk("proj")
              cv = Carver()
              cv.off = keys_end
              qaq = cv.take([128, 4, 512], BF16)
              qiq = cv.take([128, 4, 512], BF16)
              qbq = cv.take([128, 4, 512], BF16)
              idx_sb = cv.take([128, S], F32)
              Rb = [cv.take([128, 512], BF16) for _ in range(3)]
              diag = cv.take([128, 8, 128], BF16)
              Mk = cv.take([128, S], BF16)
              maskT = cv.take([128, 16, 512], BF16)
              Eb = [cv.take([128, 512], BF16) for _ in range(2)]
              Pmb = [cv.take([128, 512], BF16) for _ in range(4)]
              dsh = [cv.take([128, 512], F32)] * 2
              bis = cv.take([128, 32], F32)
              steps = cv.take([128, BIS_ITERS], F32)

              Lb = Rot([PB[0], PB[1]])
              accb = PB[2]
              Tb = PB[2]
              Ob = Rot([PB[6], PB[7]])
              sc_a = float((64 + 32) ** -0.5)
              sc_b = 0.125
              ucount = [0]

              seqc = [0]
              inflight = []
              obank = {}
              Sbh = [None]
              Sb_small = Rot([PB[3], PB[4], PB[5]])
              Sb_big = Rot([PB[3], PB[4], PB[5], PB[0], PB[1]])

              def load_qaq(hf, qt_):
                  qs_ = slice(qt_ * 512, (qt_ + 1) * 512)
                  P.dma("sp", I("dma_start", out=qaq[0:96, :, :], in_=qa_s[4 * hf:4 * hf + 4, :, qs_].rearrange("a r t -> r a t")),
                        "qaq", reads=[R("scr", "QA", i, qt_) for i in range(8)], writes=[R("qaq")])

              def emitQK(un):
                  g, hh, kb, qt_, seq, meng = un
                  c0 = max(0, kb - 4 * qt_) * 128
                  sbk, sbk_r = Sbh[0].next()
                  pr = (hh % 2) * 64
                  if g == "A":
                      if hh == 4 and kb == 0:
                          load_qaq(1, qt_)
                      P.op("pe", I("matmul", sbk[:, c0:512], lhsT=Kp[0:96, hh, kb * 128:(kb + 1) * 128],
                                   rhs=qaq[0:96, hh % 4, c0:512], start=True, stop=True),
                           reads=[R("Kp", hh, kb // 4), R("Kp2", hh, kb // 4), R("qaq")], writes=[sbk_r])
                  else:
                      P.op("pe", I("matmul", sbk[:, c0:512], lhsT=kbT[pr:pr + 64, hh // 2, kb * 128:(kb + 1) * 128],
                                   rhs=qbq[pr:pr + 64, hh // 2, c0:512], start=True, stop=True),
                           reads=[R("kbT", hh // 2, kb // 4), R("qbq")], writes=[sbk_r])
                  e, e_r = Eb[seq % 2], R("Eb", seq % 2)
                  P.op("act", I("activation", out=e[:, c0:512], in_=sbk[:, c0:512], func=AF.Exp,
                                scale=(sc_a if g == "A" else sc_b)),
                       reads=[sbk_r], writes=[e_r])
                  pmt, pmt_r = Pmb[seq % 4], R("Pmb", seq % 4)
                  if g == "A":
                      msk, msk_r = maskT[:, kb, c0:512], R("maskT")
                  else:
                      o_ = 128 * (4 * qt_ - kb) + 384
                      msk, msk_r = strip[:, o_ + c0:o_ + 512], R("strip")
                  P.op(meng, I("tensor_tensor", out=pmt[:, c0:512], in0=e[:, c0:512], in1=msk, op=ALU.mult),
                       reads=[e_r, msk_r], writes=[pmt_r])

              def emitPV(un):
                  g, hh, kb, qt_, seq, meng = un
                  NK_ = 4 * qt_ + 4
                  qs_ = slice(qt_ * 512, (qt_ + 1) * 512)
                  c0 = max(0, kb - 4 * qt_) * 128
                  if kb == 0:
                      obank[(g, hh)] = Ob.next()
                  ob, ob_r = obank[(g, hh)]
                  pmt, pmt_r = Pmb[seq % 4], R("Pmb", seq % 4)
                  V = Vp if g == "A" else Vb
                  vo = (hh // 2) * 192 + (hh % 2) * 64
                  P.op("pe", I("matmul", ob[:, c0:512], lhsT=V[:, kb, vo:vo + 128], rhs=pmt[:, c0:512],
                               start=(kb == 0), stop=(kb == NK_ - 1)),
                       reads=[pmt_r, R("Vp" if g == "A" else "Vb", kb)], writes=[ob_r])
                  if kb == NK_ - 1:
                      ev = (hh % 2 == 0)
                      np_, dp_ = (slice(0, 64), slice(64, 128)) if ev else (slice(64, 128), slice(0, 64))
                      d, d_r = dsh[0], R("dsh", 0)
                      P.op("act", I("activation", out=d[np_, :], in_=ob[dp_, :], func=AF.Ln),
                           reads=[ob_r], writes=[d_r])
                      P.op("act", I("activation", out=d[np_, :], in_=d[np_, :], func=AF.Exp, scale=-1.0), reads=[d_r], writes=[d_r])
                      chunk = (hh // 2) + (0 if g == "A" else 4)
                      P.op("dve", I("tensor_tensor", out=HT[np_, chunk, qs_], in0=ob[np_, :], in1=d[np_, :], op=ALU.mult),
                           reads=[ob_r, d_r], writes=[R("HT", qt_)])

              def feed(g, hh, kb, qt_, meng, LA):
                  un = (g, hh, kb, qt_, seqc[0], meng)
                  seqc[0] += 1
                  emitQK(un)
                  inflight.append(un)
                  while len(inflight) > LA:
                      emitPV(inflight.pop(0))

              def flush():
                  while inflight:
                      emitPV(inflight.pop(0))

              for qt in range(4):
                  qsl = slice(qt * 512, (qt + 1) * 512)
                  NK = 4 * qt + 4
                  load_qaq(0, qt)
                  P.dma("sp", I("dma_start", out=qiq[:, :, :], in_=qi_s[:, :, qsl].rearrange("a r t -> r a t")),
                        "qiq", reads=[R("scr", "QI", i, qt) for i in range(4)], writes=[R("qiq")])
                  P.dma("sp", I("dma_start", out=qbq[:, :, :], in_=qb_s[:, :, qsl].rearrange("a r t -> r a t")),
                        "qbq", reads=[R("scr", "QB", i, qt) for i in range(4)], writes=[R("qbq")])
                  bunits = [(hh, kb) for hh in range(8) for kb in range(NK)]
                  per = len(bunits) // 4
                  for tbl in range(4):
                      tb = 4 * qt + tbl
                      lo = tbl * 128
                      n = 128 * (tb + 1)
                      for hh in range(8):
                          P.op("pool", I("tensor_scalar",
                              out=diag[:, hh, :], in0=identb[:, :], scalar1=weff[:, tb, hh:hh + 1], scalar2=1.0,
                              op0=ALU.mult, op1=ALU.mult),
                              reads=[R("identb"), R("weff", tb)], writes=[R("diag", hh)])
                      for st in range(qt + 1):
                          wd = min(512, n - 512 * st)
                          acc, acc_r = accb

                          def emitL(hh, st=st, wd=wd):
                              lb, lb_r = Lb.next()
                              pr = (hh % 2) * 64
                              P.op("pe", I("matmul",
                                  lb[:, 0:wd], lhsT=qiq[pr:pr + 64, hh // 2, lo:lo + 128],
                                  rhs=kidx[pr:pr + 64, st * 512:st * 512 + wd], start=True, stop=True),
                                  reads=[R("qiq"), R("kidx", st)], writes=[lb_r])
                              rb, rb_r = Rb[hh % 3], R("Rb", hh % 3)
                              P.op("act", I("activation", out=rb[:, 0:wd], in_=lb[:, 0:wd], func=AF.Relu),
                                   reads=[lb_r], writes=[rb_r])

                          def emitA(hh, wd=wd):
                              rb, rb_r = Rb[hh % 3], R("Rb", hh % 3)
                              P.op("pe", I("matmul",
                                  acc[:, 0:wd], lhsT=diag[:, hh, :], rhs=rb[:, 0:wd], start=(hh == 0), stop=(hh == 7)),
                                  reads=[rb_r, R("diag", hh)], writes=[acc_r])

                          emitL(0)
                          emitL(1)
                          for hh in range(8):
                              if hh + 2 < 8:
                                  emitL(hh + 2)
                              emitA(hh)
                          c0 = st * 512
                          if st == qt:
                              if wd > 128:
                                  P.op("act", I("activation",
                                      out=idx_sb[:, c0:c0 + wd - 128], in_=acc[:, 0:wd - 128], func=AF.Copy),
                                      reads=[acc_r], writes=[R("idx")])
                              P.op("dve", I("tensor_tensor",
                                  out=idx_sb[:, c0 + wd - 128:c0 + wd], in0=acc[:, wd - 128:wd], in1=tri[:, :], op=ALU.add),
                                  reads=[acc_r, R("tri")], writes=[R("idx")])
                          else:
                              P.op("act", I("activation", out=idx_sb[:, c0:c0 + 512], in_=acc[:, :], func=AF.Copy),
                                   reads=[acc_r], writes=[R("idx")])
                      thr = bis[:, 0:1]
                      if tb >= 2:
                          mx, mn, W, cnt, a_ = bis[:, 1:2], bis[:, 2:3], bis[:, 3:4], bis[:, 4:5], bis[:, 5:6]
                          RB = R("bis")
                          P.op("dve", I("tensor_reduce", out=mx, in_=idx_sb[:, 0:n], op=ALU.max, axis=AX.X),
                               reads=[R("idx")], writes=[RB])
                          P.op("dve", I("tensor_reduce", out=mn, in_=idx_sb[:, 0:n - 128], op=ALU.min, axis=AX.X),
                               reads=[R("idx")], writes=[RB])
                          P.op("dve", I("tensor_tensor", out=W, in0=mx, in1=mn, op=ALU.subtract), reads=[RB], writes=[RB])
                          P.op("dve", I("tensor_tensor", out=thr, in0=mx, in1=mn, op=ALU.add), reads=[RB], writes=[RB])
                          P.op("dve", I("tensor_scalar", out=thr, in0=thr, scalar1=0.5, scalar2=None, op0=ALU.mult),
                               reads=[RB], writes=[RB])
                          P.op("dve", I("tensor_scalar", out=steps[:, :], in0=cvec[:, :], scalar1=W, scalar2=None, op0=ALU.mult),
                               reads=[RB, R("cvec")], writes=[RB])
                          for it in range(BIS_ITERS):
                              P.op("dve", I("tensor_scalar",
                                  out=Mk[:, 0:n], in0=idx_sb[:, 0:n], scalar1=thr, scalar2=None,
                                  op0=ALU.is_ge, op1=ALU.add, accum_out=cnt),
                                  reads=[R("idx"), RB], writes=[R("Mk"), RB])
                              P.op("dve", I("tensor_scalar", out=a_, in0=cnt, scalar1=255.5, scalar2=0.5,
                                            op0=ALU.is_ge, op1=ALU.subtract),
                                   reads=[RB], writes=[RB])
                              P.op("dve", I("scalar_tensor_tensor",
                                  out=thr, in0=a_, scalar=steps[:, it:it + 1], in1=thr, op0=ALU.mult, op1=ALU.add),
                                  reads=[RB], writes=[RB])
                          P.op("dve", I("tensor_tensor", out=thr, in0=thr, in1=steps[:, BIS_ITERS - 1:BIS_ITERS], op=ALU.subtract),
                               reads=[RB], writes=[RB])
                          P.op("dve", I("tensor_scalar", out=Mk[:, 0:n], in0=idx_sb[:, 0:n], scalar1=thr, scalar2=None,
                                        op0=ALU.is_ge),
                               reads=[R("idx"), RB], writes=[R("Mk")])
                      else:
                          P.op("dve", I("tensor_scalar", out=Mk[:, 0:n], in0=idx_sb[:, 0:n], scalar1=-1.0e29, scalar2=None,
                                        op0=ALU.is_ge),
                               reads=[R("idx")], writes=[R("Mk")])
                      Sbh[0] = Sb_small
                      for (hh, kb) in bunits[tbl * per:(tbl + 1) * per]:
                          feed("B", hh, kb, qt, "pool", 2)
                      tbk, tbk_r = Tb
                      tv = bfv(tbk[:, :]).rearrange("p (a b) -> p a b", a=8)
                      for g0 in range(0, tb + 1, 8):
                          g1 = min(tb + 1, g0 + 8)
                          P.op("pe", [(I("transpose", out=tv[:, kb - g0, :], in_=Mk[:, kb * 128:(kb + 1) * 128],
                                         identity=identb[:, :])) for kb in range(g0, g1)],
                               reads=[R("Mk"), R("identb")], writes=[tbk_r])
                          ucount[0] += 1
                          evac(ucount[0], maskT[:, g0:g1, lo:lo + 128], tv[:, 0:g1 - g0, :], [tbk_r], [R("maskT")])

                  if qt == 0:
                      chk("idx0")
                  Sbh[0] = Sb_big
                  for hh in range(8):
                      for kb in range(NK):
                          feed("A", hh, kb, qt, ("pool" if seqc[0] % 3 == 0 else "dve"), 3)
                  flush()
              P.barrier()

              chk("attn")
              cv = Carver()
              XR = cv.take([128, 16, D], F32)
              x_end = cv.off
              wA = [cv.take([128, 8, 512], BF16) for _ in range(2)]
              mmb = Rot([PB[0], PB[1], PB[2], PB[3]])
              for q4 in range(4):
                  P.dma("sp", I("dma_start",
                      out=XR[:, q4 * 4:(q4 + 1) * 4, :], in_=x_d[b, q4 * 512:(q4 + 1) * 512, :].rearrange("(n p) d -> p n d", p=128)),
                      f"xr{q4}", writes=[R("XR", t) for t in range(q4 * 4, q4 * 4 + 4)])

              def out_proj(w_d, srcT, src_res_fn, keyp):
                  for half in range(2):
                      ws, ws_r = wA[half], R("wA", half)
                      load_w(ws[:, :, :], w_d[:, half * 512:(half + 1) * 512].rearrange("(c p) n -> p c n", p=128), f"wA{half}", ws_r)
                      for tb in range(16):
                          bk, bk_r = mmb.next()
                          P.op("pe", [(I("matmul",
                              bk[:, :], lhsT=srcT[:, c, tb * 128:(tb + 1) * 128], rhs=ws[:, c, :],
                              start=(c == 0), stop=(c == 7))) for c in range(8)],
                              reads=[ws_r, src_res_fn(tb)], writes=[bk_r])
                          P.op("dve", I("tensor_tensor",
                              out=XR[:, tb, half * 512:(half + 1) * 512], in0=bk[:, :], in1=XR[:, tb, half * 512:(half + 1) * 512],
                              op=ALU.add), reads=[bk_r, R("XR", tb)], writes=[R("XR", tb)])

              out_proj(wout_d, HT, lambda tb: R("HT", tb // 4), "wout")
              P.barrier()

              chk("wout")
              cv = Carver()
              cv.off = x_end
              OC = cv.take([128, 8, S], BF16)
              wA = [cv.take([128, 8, 512], BF16) for _ in range(2)]
              junk = cv.take([128, D], BF16)
              xn = [cv.take([128, D], BF16)] * 2
              ms = [cv.take([128, D], F32)] * 2
              mT = cv.take([128, 8, 256], BF16)
              KcT = cv.take([128, 8, 256], BF16)
              Vc = cv.take([128, 2, D], BF16)
              qcT = cv.take([128, 8, 512], BF16)
              Ec = [cv.take([128, 512], BF16) for _ in range(4)]
              rdn = [cv.take([128, 512], F32)] * 2
              for i in range(16):
                  norm_tile(XR[:, i, :], R("XR", i), 8, i, junk[:, :], R("junk"), xn[0][:, :], R("xn", 0),
                            PB[7], HT[:, :, i * 128:(i + 1) * 128], R("HT", i // 4), 2 * (i % 2))
              for i in range(2):
                  P.dma("sp", I("dma_start", out=ms[i][:, :], in_=mem_d[b, i * 128:(i + 1) * 128, :]), "ms0",
                        writes=[R("ms", 0)])
                  norm_tile(ms[i][:, :], R("ms", 0), 16, i, junk[:, :], R("junk"), xn[0][:, :], R("xn", 0),
                            PB[7], mT[:, :, i * 128:(i + 1) * 128], R("mT"), 2 * (i % 2))
              mmb = Rot([PB[0], PB[1], PB[2]])
              ec = 0
              for half in range(2):
                  ws, ws_r = wA[half], R("wA", half)
                  load_w(ws[:, :, :], wkv_d[:, half * 512:(half + 1) * 512].rearrange("(c p) n -> p c n", p=128), f"wA{half}", ws_r)
                  for j4 in range(4):
                      j = half * 4 + j4
                      bk, bk_r = mmb.next()
                      P.op("pe", [(I("matmul",
                          bk[:, 0:256], lhsT=ws[:, c, j4 * 128:(j4 + 1) * 128], rhs=mT[:, c, :], start=(c == 0), stop=(c == 7)))
                          for c in range(8)], reads=[ws_r, R("mT")], writes=[bk_r])
                      ec += 1
                      evac(ec, KcT[:, j, :], bk[:, 0:256], [bk_r], [R("KcT", j)])
              for half in range(2):
                  ws, ws_r = wA[half], R("wA", half)
                  load_w(ws[:, :, :], wkv_d[:, D + half * 512:D + (half + 1) * 512].rearrange("(c p) n -> p c n", p=128),
                         f"wA{half}", ws_r)
                  for mc in range(2):
                      bk, bk_r = mmb.next()
                      P.op("pe", [(I("matmul",
                          bk[:, :], lhsT=mT[:, c, mc * 128:(mc + 1) * 128], rhs=ws[:, c, :], start=(c == 0), stop=(c == 7)))
                          for c in range(8)], reads=[ws_r, R("mT")], writes=[bk_r])
                      ec += 1
                      evac(ec, Vc[:, mc, half * 512:(half + 1) * 512], bk[:, :], [bk_r], [R("Vc", mc)])
              for half in range(2):
                  load_w(wA[half][:, :, :], wq_d[:, half * 512:(half + 1) * 512].rearrange("(c p) n -> p c n", p=128),
                         f"wA{half}", R("wA", half))
              sbk2 = Rot([PB[3], PB[4]])
              obk = Rot([PB[5], PB[6]])
              dbk = PB[7]
              for tt in range(4):
                  tsl = slice(tt * 512, (tt + 1) * 512)
                  for j in range(8):
                      bk, bk_r = mmb.next()
                      P.op("pe", [(I("matmul",
                          bk[:, :], lhsT=wA[j // 4][:, c, (j % 4) * 128:(j % 4 + 1) * 128], rhs=HT[:, c, tsl],
                          start=(c == 0), stop=(c == 7))) for c in range(8)],
                          reads=[R("wA", j // 4), R("HT", tt)], writes=[bk_r])
                      ec += 1
                      evac(ec, qcT[:, j, :], bk[:, :], [bk_r], [R("qcT", j)])
                  for hh in range(4):
                      for mc in range(2):
                          sk, sk_r = sbk2.next()
                          P.op("pe", [(I("matmul",
                              sk[:, :], lhsT=KcT[:, 2 * hh + jj, mc * 128:(mc + 1) * 128], rhs=qcT[:, 2 * hh + jj, :],
                              start=(jj == 0), stop=(jj == 1))) for jj in range(2)],
                              reads=[R("KcT", 2 * hh), R("KcT", 2 * hh + 1), R("qcT", 2 * hh), R("qcT", 2 * hh + 1)], writes=[sk_r])
                          e, e_r = Ec[(hh % 2) * 2 + mc], R("Ec", (hh % 2) * 2 + mc)
                          P.op("act", I("activation", out=e[:, :], in_=sk[:, :], func=AF.Exp, scale=1.0 / 16),
                               reads=[sk_r], writes=[e_r])
                      e0, e1 = Ec[(hh % 2) * 2], Ec[(hh % 2) * 2 + 1]
                      er = [R("Ec", (hh % 2) * 2), R("Ec", (hh % 2) * 2 + 1)]
                      db, db_r = dbk
                      P.op("pe", [(I("matmul", db[:, :], lhsT=onesb[:, :], rhs=e[:, :],
                                                                  start=(mc == 0), stop=(mc == 1))) for mc, e in ((0, e0), (1, e1))],
                           reads=er + [R("onesb")], writes=[db_r])
                      rd, rd_r = rdn[0], R("rdn", 0)
                      P.op("dve", I("reciprocal", out=rd[:, :], in_=db[:, :]), reads=[db_r], writes=[rd_r])
                      for jj in range(2):
                          ob, ob_r = obk.next()
                          P.op("pe", [(I("matmul",
                              ob[:, :], lhsT=Vc[:, mc, (2 * hh + jj) * 128:(2 * hh + jj + 1) * 128], rhs=e[:, :],
                              start=(mc == 0), stop=(mc == 1))) for mc, e in ((0, e0), (1, e1))],
                              reads=er + [R("Vc", 0), R("Vc", 1)], writes=[ob_r])
                          P.op("dve", I("tensor_tensor",
                              out=OC[:, 2 * hh + jj, tsl], in0=ob[:, :], in1=rd[:, :], op=ALU.mult),
                              reads=[ob_r, rd_r], writes=[R("OC", tt)])
              mmb = Rot([PB[0], PB[1], PB[2], PB[3]])
              P.barrier()

              def out_proj2(w_d, srcT, src_res_fn):
                  for half in range(2):
                      ws, ws_r = wA[half], R("wA", half)
                      load_w(ws[:, :, :], w_d[:, half * 512:(half + 1) * 512].rearrange("(c p) n -> p c n", p=128), f"wA{half}", ws_r)
                      for tb in range(16):
                          bk, bk_r = mmb.next()
                          P.op("pe", [(I("matmul",
                              bk[:, :], lhsT=srcT[:, c, tb * 128:(tb + 1) * 128], rhs=ws[:, c, :],
                              start=(c == 0), stop=(c == 7))) for c in range(8)],
                              reads=[ws_r, src_res_fn(tb)], writes=[bk_r])
                          P.op("dve", I("tensor_tensor",
                              out=XR[:, tb, half * 512:(half + 1) * 512], in0=bk[:, :], in1=XR[:, tb, half * 512:(half + 1) * 512],
                              op=ALU.add), reads=[bk_r, R("XR", tb)], writes=[R("XR", tb)])

              out_proj2(wo_d, OC, lambda tb: R("OC", tb // 4))
              P.barrier()

              chk("cross")
              cv = Carver()
              cv.off = x_end
              junk = cv.take([128, D], BF16)
              xn = [cv.take([128, D], BF16) for _ in range(2)]
              wU = [cv.take([128, 8, 512], BF16) for _ in range(2)]
              wD = [cv.take([128, 4, D], BF16) for _ in range(2)]
              hid = [cv.take([128, 4, 512], BF16) for _ in range(2)]
              rl = [cv.take([128, 512], BF16) for _ in range(2)]
              gF = cv.take([128, D], F32)
              ot = [cv.take([128, D], F32) for _ in range(2)]
              P.dma("sp", I("dma_start", out=gF[:, :], in_=gF_d[:, :]), "gF", writes=[R("gF")])
              for i in range(16):
                  norm_tile(XR[:, i, :], R("XR", i), 24, i, junk[:, :], R("junk"), xn[i % 2][:, :], R("xn", i % 2),
                            PB[7], HT[:, :, i * 128:(i + 1) * 128], R("HT", i // 4), 2 * (i % 2))
              upb = Rot([PB[0], PB[1], PB[2]])
              dnb = Rot([PB[3], PB[4], PB[5], PB[6]])
              cnt = 0
              for fg in range(8):
                  wu, wu_r = wU[fg % 2], R("wU", fg % 2)
                  wd_, wd_r = wD[fg % 2], R("wD", fg % 2)
                  load_w(wu[:, :, :], wup_d[:, fg * 512:(fg + 1) * 512].rearrange("(c p) n -> p c n", p=128), f"wU{fg % 2}", wu_r)
                  load_w(wd_[:, :, :], wdn_d[fg * 512:(fg + 1) * 512, :].rearrange("(c p) n -> p c n", p=128), f"wD{fg % 2}", wd_r)
                  for tt in range(4):
                      tsl = slice(tt * 512, (tt + 1) * 512)
                      cnt += 1
                      hd, hd_r = hid[cnt % 2], R("hid", cnt % 2)
                      for fc in range(4):
                          bk, bk_r = upb.next()
                          P.op("pe", [(I("matmul",
                              bk[:, :], lhsT=wu[:, c, fc * 128:(fc + 1) * 128], rhs=HT[:, c, tsl],
                              start=(c == 0), stop=(c == 7))) for c in range(8)],
                              reads=[wu_r, R("HT", tt)], writes=[bk_r])
                          r_, r_r = rl[fc % 2], R("rl", fc % 2)
                          P.op("act", I("activation", out=r_[:, :], in_=bk[:, :], func=AF.Relu),
                               reads=[bk_r], writes=[r_r])
                          P.op("pool", I("tensor_tensor", out=hd[:, fc, :], in0=r_[:, :], in1=r_[:, :],
                                                                                         op=ALU.mult),
                               reads=[r_r], writes=[hd_r])
                      for t4 in range(4):
                          tb = tt * 4 + t4
                          for half in range(2):
                              bk, bk_r = dnb.next()
                              P.op("pe", [(I("matmul",
                                  bk[:, :], lhsT=hd[:, fc, t4 * 128:(t4 + 1) * 128], rhs=wd_[:, fc, half * 512:(half + 1) * 512],
                                  start=(fc == 0), stop=(fc == 3))) for fc in range(4)],
                                  reads=[hd_r, wd_r], writes=[bk_r])
                              P.op("dve", I("tensor_tensor",
                                  out=XR[:, tb, half * 512:(half + 1) * 512], in0=bk[:, :], in1=XR[:, tb, half * 512:(half + 1) * 512],
                                  op=ALU.add), reads=[bk_r, R("XR", tb)], writes=[R("XR", tb)])
              for i in range(16):
                  src = XR[:, i, :]
                  ssc = 2 * (i % 2)
                  ss, rs = stat[:, ssc:ssc + 1], stat[:, ssc + 1:ssc + 2]
                  ss_r, rs_r = R("stat", ssc), R("stat", ssc + 1)
                  o_, o_r = ot[i % 2], R("ot", i % 2)
                  P.op("act", I("activation", out=junk[:, :], in_=src, func=AF.Square, accum_out=ss),
                       reads=[R("XR", i)], writes=[R("junk"), ss_r])
                  P.op("act", I("activation", out=rs, in_=ss, func=AF.Sqrt, scale=1.0 / D, bias=1e-6),
                       reads=[ss_r], writes=[rs_r])
                  P.op("dve", I("reciprocal", out=rs, in_=rs), reads=[rs_r], writes=[rs_r])
                  P.op("dve", I("scalar_tensor_tensor",
                      out=o_[:, :], in0=src, scalar=rs, in1=gF[:, :], op0=ALU.mult, op1=ALU.mult),
                      reads=[R("XR", i), rs_r, R("gF")], writes=[o_r])
                  out_toks.append(P.dma("sp", I("dma_start", out=out_d[b, i * 128:(i + 1) * 128, :], in_=o_[:, :]),
                                        f"ot{i % 2}", reads=[o_r]))
              P.barrier()

        except _Stop:
            P.barrier()
        P.wait_tokens("sp", out_toks)
        with nc.Block() as block:
            P.emit(block)
    nc._mk_stats = (P.nops, P.nwaits)
    return nc


_NC_CACHE = {}


def kernel(x, mem, norm_mix_g, w_in, kv_norm_g, w_uk, w_uv, w_out, norm_cross_g, norm_mem_g,
           w_q_cross, w_kv_cross, w_o_cross, norm_mlp_g, w_up, w_down, norm_final_g):
    f = lambda a: np.ascontiguousarray(np.asarray(a, dtype=np.float32))
    x, mem = f(x), f(mem)
    win_cols, _ = _win_plan()
    win = f(f(w_in)[0][:, win_cols])
    wukT = f(np.transpose(f(w_uk)[0], (2, 0, 1)).reshape(128, 512))
    wuv = f(np.transpose(f(w_uv)[0], (1, 0, 2)).reshape(128, 512))
    gcol = lambda g: f(g).reshape(8, 128).T
    gall = f(np.concatenate([gcol(norm_mix_g), gcol(norm_cross_g), gcol(norm_mem_g), gcol(norm_mlp_g),
                             f(kv_norm_g).reshape(128, 1)], axis=1))
    gF = f(np.broadcast_to(f(norm_final_g).reshape(1, D), (128, D)))
    consts = _consts()
    if "nc" not in _NC_CACHE:
        _NC_CACHE["nc"] = build_nc(NB)
    nc = _NC_CACHE["nc"]
    shared = dict(win=win, wukT=wukT, wuv=wuv, wout=f(w_out)[0], wq=f(w_q_cross)[0], wkv=f(w_kv_cross)[0],
                  wo=f(w_o_cross)[0], wup=f(w_up)[0], wdn=f(w_down)[0], gall=gall, gF=gF, **consts)
    in_maps = []
    for c in range(NCORES):
        m = dict(shared)
        m["x"] = x[c * NB:(c + 1) * NB]
        m["mem"] = mem[c * NB:(c + 1) * NB]
        in_maps.append(m)
    res = run_bass_kernel_spmd(nc, in_maps, core_ids=list(range(NCORES)))
    return np.concatenate([res.results[c]["out"] for c in range(NCORES)], axis=0).astype(np.float32)
```

```python
import math
from contextlib import ExitStack
import numpy as np
import concourse.bass as bass
import concourse.mybir as mybir
from concourse.bass_utils import run_bass_kernel_spmd

F32 = mybir.dt.float32
BF16 = mybir.dt.bfloat16
AF = mybir.ActivationFunctionType
ALU = mybir.AluOpType
AX = mybir.AxisListType

NCORES = 8
NB = 4
S = 2048
D = 1024
BIS_ITERS = 18
NEG = -1.0e30
STRIP_W = 2816


class Res:
    __slots__ = ("name", "writer", "readers", "excl")

    def __init__(self, name):
        self.name = name
        self.writer = None
        self.readers = []
        self.excl = (name[0] == "psum")


class ResReg:
    def __init__(self):
        self.d = {}

    def __call__(self, *key):
        r = self.d.get(key)
        if r is None:
            r = self.d[key] = Res(key)
        return r


class Eng:
    def __init__(self, name, sem, is_pe=False):
        self.name, self.sem, self.is_pe = name, sem, is_pe
        self.count = 0
        self.ops = []
        self.waited = {}


class Prog:
    def __init__(self, nc, sems, dma_sem_pool):
        self.nc = nc
        self.E = {n: Eng(n, sems[n], is_pe=(n == "pe")) for n in ("pe", "act", "dve", "pool", "sp")}
        self.dma_sems = {}
        self.sem_pool = dma_sem_pool
        self.nwaits = 0
        self.nops = 0

    def _deps(self, reads, writes):
        deps = []
        for r in reads:
            if r.writer is not None:
                deps.append(r.writer)
            if r.excl:
                deps.extend(r.readers)
        for w in writes:
            if w.writer is not None:
                deps.append(w.writer)
            deps.extend(w.readers)
        return deps

    def _waits(self, eng, deps):
        need = {}
        for d in deps:
            key = (d[0], d[1])
            if need.get(key, 0) < d[2]:
                need[key] = d[2]
        waits = []
        for key, v in need.items():
            if key[0] == "e" and key[1] == eng.name and eng.is_pe:
                continue
            if eng.waited.get(key, 0) >= v:
                continue
            eng.waited[key] = v
            if key[0] == "e":
                waits.append((self.E[key[1]].sem, v))
            else:
                waits.append((self.dma_sems[key[1]][0], v * 16))
        self.nwaits += len(waits)
        return waits

    def _commit(self, tok, reads, writes):
        for r in reads:
            if r.excl:
                r.writer = tok
                r.readers = []
            else:
                r.readers.append(tok)
        for w in writes:
            w.writer = tok
            w.readers = []

    def op(self, engname, fns, reads=(), writes=()):
        eng = self.E[engname]
        if isinstance(fns, tuple):
            fns = [fns]
        waits = self._waits(eng, self._deps(reads, writes))
        eng.count += 1
        tok = ("e", engname, eng.count)
        eng.ops.append((waits, fns, ("e", eng.sem)))
        self._commit(tok, reads, writes)
        self.nops += len(fns)
        return tok

    def dma(self, queue, fn, semkey, reads=(), writes=()):
        eng = self.E[queue]
        if semkey not in self.dma_sems:
            self.dma_sems[semkey] = [self.sem_pool.pop(), 0]
        ent = self.dma_sems[semkey]
        deps = self._deps(reads, writes)
        if ent[1] > 0:
            deps.append(("d", semkey, ent[1]))
        waits = self._waits(eng, deps)
        ent[1] += 1
        tok = ("d", semkey, ent[1])
        eng.ops.append((waits, [fn], ("d", ent[0])))
        self._commit(tok, reads, writes)
        self.nops += 1
        return tok

    def wait_tokens(self, engname, toks):
        eng = self.E[engname]
        waits = self._waits(eng, list(toks))
        if waits:
            eng.ops.append((waits, [], None))

    def barrier(self):
        toks = []
        for e in self.E.values():
            if e.count > 0:
                toks.append(("e", e.name, e.count))
        for k, (s, c) in self.dma_sems.items():
            if c > 0:
                toks.append(("d", k, c))
        for e in self.E.values():
            self.wait_tokens(e.name, [t for t in toks if not (t[0] == "e" and t[1] == e.name and e.is_pe)])

    def new_epoch(self, sems, reg):
        for e in self.E.values():
            e.sem = sems[e.name]
            e.count = 0
            e.waited = {k: v for k, v in e.waited.items() if k[0] == "d"}
        for r in reg.d.values():
            if r.writer is not None and r.writer[0] == "e":
                r.writer = None
            r.readers = [t for t in r.readers if t[0] != "e"]

    def emit(self, block):
        def run(eng, h):
            for waits, fns, inc in eng.ops:
                for sem, v in waits:
                    h.wait_ge(sem, v)
                ins = None
                for f in fns:
                    ins = getattr(h, f[0])(*f[1], **f[2])
                if inc is not None and ins is not None:
                    ins.then_inc(inc[1], 1 if inc[0] == "e" else 16)

        block.tensor(lambda h: run(self.E["pe"], h))
        block.scalar(lambda h: run(self.E["act"], h))
        block.vector(lambda h: run(self.E["dve"], h))
        block.gpsimd(lambda h: run(self.E["pool"], h))
        block.sync(lambda h: run(self.E["sp"], h))


def I(name, *a, **k):
    return (name, a, k)


class Rot:
    def __init__(self, items):
        self.items, self.i = items, 0

    def next(self):
        it = self.items[self.i % len(self.items)]
        self.i += 1
        return it


O_QN, O_QR, O_CKV, O_KR, O_QI, O_KI, O_WI, O_QKV = 0, 512, 768, 896, 928, 1440, 1504, 1512


def _win_plan():
    cols = []
    slabs = []

    def slab(chunks_cols):
        off = len(cols)
        chunks = []
        for kind, idx, cc in chunks_cols:
            chunks.append(dict(kind=kind, idx=idx, coff=len(cols) - off, M=len(cc)))
            cols.extend(cc)
        slabs.append((off, len(cols) - off, chunks))

    def qa(h):
        return list(range(O_QN + 64 * h, O_QN + 64 * h + 64)) + list(range(O_QR + 32 * h, O_QR + 32 * h + 32))

    r = lambda a, n: list(range(a, a + n))
    slab([("QA", h, qa(h)) for h in range(0, 4)])
    slab([("QA", h, qa(h)) for h in range(4, 8)] + [("KR", 0, r(O_KI, 64) + r(O_KR, 32))])
    slab([("QI", j, r(O_QI + 128 * j, 128)) for j in range(4)])
    slab([("QB", j, r(O_QKV + 128 * j, 128)) for j in range(4)])
    slab([("KB", j, r(O_QKV + 512 + 128 * j, 128)) for j in range(4)])
    slab([("KI", 0, r(O_KI, 64) + r(O_KI, 64)), ("CKV", 0, r(O_CKV, 128))])
    slab([("VB", 0, r(O_QKV + 1024, 512))])
    slab([("WI", 0, r(O_WI, 8))])
    return np.array(cols, dtype=np.int64), slabs


def _consts():
    c = {}
    c["ident"] = np.eye(128, dtype=np.float32)
    tri = np.zeros((128, 128), np.float32)
    tri[np.triu_indices(128, 1)] = NEG
    c["tri"] = tri
    i = np.arange(128)[:, None]
    cc = np.arange(STRIP_W)[None, :]
    dl = cc - i - 384
    m = ((dl >= 0) & (dl <= 128)).astype(np.float32) + ((dl >= 0) & (dl % 4 == 0) & (dl <= 512)) + \
        ((dl >= 0) & (dl % 16 == 0) & (dl <= 2048))
    c["strip"] = m.astype(np.float32)
    p32 = np.zeros((128, 128), np.float32)
    for k in range(16):
        p32[64 + k, 80 + k] = 1
        p32[80 + k, 64 + k] = 1
    p64 = np.zeros((128, 128), np.float32)
    for hb in (0, 64):
        for k in range(32):
            p64[hb + k, hb + 32 + k] = 1
            p64[hb + 32 + k, hb + k] = 1
    c["p32"], c["p64"] = p32, p64
    c["cvec"] = np.tile((2.0 ** -(np.arange(BIS_ITERS) + 1.0))[None, :], (128, 1)).astype(np.float32)
    pos = np.arange(S, dtype=np.float32)[None, :]
    inv32 = (10000.0 ** (-np.arange(0, 32, 2, dtype=np.float32) / 32)).astype(np.float32)
    inv64 = (10000.0 ** (-np.arange(0, 64, 2, dtype=np.float32) / 64)).astype(np.float32)
    cos32 = np.ones((128, S), np.float32)
    sin32 = np.zeros((128, S), np.float32)
    a32 = inv32[:, None] * pos
    cos32[64:80], cos32[80:96] = np.cos(a32), np.cos(a32)
    sin32[64:80], sin32[80:96] = -np.sin(a32), np.sin(a32)
    a64 = inv64[:, None] * pos
    cos64 = np.zeros((128, S), np.float32)
    sin64 = np.zeros((128, S), np.float32)
    for hb in (0, 64):
        cos64[hb:hb + 32], cos64[hb + 32:hb + 64] = np.cos(a64), np.cos(a64)
        sin64[hb:hb + 32], sin64[hb + 32:hb + 64] = -np.sin(a64), np.sin(a64)
    c["cos32"], c["sin32"], c["cos64"], c["sin64"] = cos32, sin32, cos64, sin64
    return c


class _Stop(Exception):
    pass


def build_nc(nb=NB, stop=None, dbg_step=0):
    nc = bass.Bass("TRN2", target_bir_lowering=False)
    win_cols, slabs = _win_plan()
    NCOL = len(win_cols)

    def din(name, shape, dt=F32):
        return nc.dram_tensor(name, list(shape), dt, kind="ExternalInput").ap()

    x_d = din("x", [nb, S, D])
    mem_d = din("mem", [nb, 256, D])
    win_d = din("win", [D, NCOL])
    wuk_d = din("wukT", [128, 512])
    wuv_d = din("wuv", [128, 512])
    wout_d = din("wout", [D, D])
    wq_d = din("wq", [D, D])
    wkv_d = din("wkv", [D, 2 * D])
    wo_d = din("wo", [D, D])
    wup_d = din("wup", [D, 4 * D])
    wdn_d = din("wdn", [4 * D, D])
    gall_d = din("gall", [128, 33])
    gF_d = din("gF", [128, D])
    c_ident = din("ident", [128, 128])
    c_tri = din("tri", [128, 128])
    c_strip = din("strip", [128, STRIP_W])
    c_p32 = din("p32", [128, 128])
    c_p64 = din("p64", [128, 128])
    c_cvec = din("cvec", [128, BIS_ITERS])
    c_tab = {k: din(k, [128, S]) for k in ("cos32", "sin32", "cos64", "sin64")}
    out_d = nc.dram_tensor("out", [nb, S, D], F32, kind="ExternalOutput").ap()
    qa_s = nc.dram_tensor("qa_s", [8, 96, S], BF16, kind="ExternalOutput").ap()
    qi_s = nc.dram_tensor("qi_s", [4, 128, S], BF16, kind="ExternalOutput").ap()
    qb_s = nc.dram_tensor("qb_s", [4, 128, S], BF16, kind="ExternalOutput").ap()

    with ExitStack() as es:
        def sb(name, shape, dt):
            return es.enter_context(nc.sbuf_tensor("sb_" + name, list(shape), dt))

        def sem(name):
            return es.enter_context(nc.semaphore(name))

        sems = {n: sem("s_" + n) for n in ("pe", "act", "dve", "pool", "sp")}
        P = Prog(nc, sems, [sem(f"dq{i}") for i in range(40)])
        R = ResReg()

        HT = sb("HT", [128, 8, S], BF16)
        identb = sb("identb", [128, 128], BF16)
        onesb = sb("onesb", [128, 128], BF16)
        tri = sb("tri", [128, 128], F32)
        strip = sb("strip", [128, STRIP_W], BF16)
        p32 = sb("p32", [128, 128], BF16)
        p64 = sb("p64", [128, 128], BF16)
        cvec = sb("cvec", [128, BIS_ITERS], F32)
        gall = sb("gall", [128, 33], F32)
        wukT = sb("wukT", [128, 512], BF16)
        wuv = sb("wuv", [128, 512], BF16)
        weff = sb("weff", [128, 16, 8], F32)
        stat = sb("stat", [128, 64], F32)
        ARENA_B = 162 * 1024
        arena = sb("arena", [128, ARENA_B // 2], BF16)
        psum = [es.enter_context(nc.psum_tensor(f"ps{i}", [128, 512], F32)) for i in range(8)]
        PB = [(psum[i], R("psum", i)) for i in range(8)]

        class Carver:
            def __init__(self):
                self.off = 0

            def take(self, shape, dt):
                esz = 4 if dt == F32 else 2
                n = int(np.prod(shape[1:]))
                nbytes = n * esz
                nbytes_al = (nbytes + 63) // 64 * 64
                a = arena[:, self.off // 2: (self.off + nbytes) // 2]
                self.off += nbytes_al
                assert self.off <= ARENA_B, ("arena overflow", self.off)
                if dt == F32:
                    a = a.bitcast(F32)
                if len(shape) == 3:
                    a = a.rearrange("p (a b) -> p a b", a=shape[1])
                elif len(shape) == 4:
                    a = a.rearrange("p (a b c) -> p a b c", a=shape[1], b=shape[2])
                return a

        def bfv(ps_ap):
            return ps_ap.bitcast(BF16)

        def ld(dst, src, key, queue="sp"):
            P.dma(queue, I("dma_start", out=dst, in_=src), key, writes=[R(key)])

        ld(identb[:], c_ident[:, :], "identb", "pool")
        P.dma("pool", I("dma_start", out=strip[:, 0:1408], in_=c_strip[:, 0:1408]), "strip", writes=[R("strip")])
        P.dma("pool", I("dma_start", out=strip[:, 1408:STRIP_W], in_=c_strip[:, 1408:STRIP_W]), "strip2", writes=[R("strip2")])
        ld(p32[:], c_p32[:, :], "p32", "pool")
        ld(p64[:], c_p64[:, :], "p64", "pool")
        ld(wukT[:], wuk_d[:, :], "wukT", "pool")
        ld(wuv[:], wuv_d[:, :], "wuv", "pool")
        ld(tri[:], c_tri[:, :], "tri")
        ld(cvec[:], c_cvec[:, :], "cvec")
        ld(gall[:], gall_d[:, :], "gall")
        P.op("dve", I("memset", onesb[:], 1.0), writes=[R("onesb")])
        CONST_R = [R(k) for k in ("identb", "strip", "strip2", "p32", "p64", "wukT", "wuv", "tri", "cvec", "gall", "onesb")]
        P.barrier()

        out_toks = []

        def evac(i, dst, src, reads, writes):
            if i % 2 == 0:
                P.op("act", I("activation", out=dst, in_=src, func=AF.Copy), reads=reads, writes=writes)
            else:
                P.op("dve", I("tensor_copy", out=dst, in_=src), reads=reads, writes=writes)

        def norm_tile(src, src_res, gcol, i, junk, junk_r, xn, xn_r, tbank, dstT, dst_res, ssc):
            ss = stat[:, ssc:ssc + 1]
            rs = stat[:, ssc + 1:ssc + 2]
            ss_r, rs_r = R("stat", ssc), R("stat", ssc + 1)
            P.op("act", I("activation", out=junk, in_=src, func=AF.Square, accum_out=ss),
                 reads=[src_res], writes=[junk_r, ss_r])
            P.op("act", I("activation", out=rs, in_=ss, func=AF.Sqrt, scale=1.0 / D, bias=1e-6),
                 reads=[ss_r], writes=[rs_r])
            P.op("dve", I("reciprocal", out=rs, in_=rs), reads=[rs_r], writes=[rs_r])
            P.op("act", I("activation", out=xn, in_=src, func=AF.Copy, scale=rs),
                 reads=[src_res, rs_r], writes=[xn_r])
            tb_ap, tb_r = tbank
            tv = bfv(tb_ap[:, :]).rearrange("p (a b) -> p a b", a=8)
            P.op("pe", [(I("transpose", out=tv[:, c, :], in_=xn[:, c * 128:(c + 1) * 128], identity=identb[:]))
                        for c in range(8)], reads=[xn_r, R("identb")], writes=[tb_r])
            P.op("dve", I("tensor_tensor", out=dstT, in0=tv,
                                                   in1=gall[:, gcol:gcol + 8].unsqueeze(2).to_broadcast([128, 8, 128]),
                                                   op=ALU.mult),
                 reads=[tb_r, R("gall")], writes=[dst_res])

        def load_w(dst, src_rows_cols, key, res):
            P.dma("pool", I("dma_start", out=dst, in_=src_rows_cols), key, writes=[res])

        def chk(tag):
            if stop == tag:
                raise _Stop()

        try:
          for b in range(nb):
              P.barrier()
              P.new_epoch({n: sem(f"s{b}_" + n) for n in ("pe", "act", "dve", "pool", "sp")}, R)
              cv = Carver()
              Kp = cv.take([128, 8, S], BF16)
              Vp = cv.take([128, 16, 768], BF16)
              kidx = cv.take([128, S], BF16)
              kbT = cv.take([128, 4, S], BF16)
              Vb = cv.take([128, 16, 768], BF16)
              keys_end = cv.off
              xs = [cv.take([128, D], F32) for _ in range(4)]
              junk = cv.take([128, D], BF16)
              xn = [cv.take([128, D], BF16) for _ in range(4)]
              cv.off = keys_end
              tabc = cv.take([128, S], F32)
              tabs_ = cv.take([128, S], F32)
              wsl = [cv.take([128, 8, 512], BF16) for _ in range(2)]
              qbuf = [cv.take([128, 512], BF16) for _ in range(2)]
              t1b = [cv.take([128, 512], F32) for _ in range(2)]
              t2b = [cv.take([128, 512], F32) for _ in range(2)]
              stg = [cv.take([128, 512], BF16) for _ in range(4)]
              ckf = cv.take([128, 512], F32)
              sqb = cv.take([128, 512], BF16)
              rtb = cv.take([128, 512], F32)
              ckvT = cv.take([128, S], BF16)

              for (T, nm) in ((Vp, "Vp"), (Vb, "Vb")):
                  tv4 = T.rearrange("p k (a c) -> p k a c", a=4)
                  P.op("pool", I("memset", tv4[:, :, :, 64:128], 1.0),
                       writes=[R(nm, kb) for kb in range(16)])
              for i in range(16):
                  P.dma("sp", I("dma_start", out=xs[i % 4][:, :], in_=x_d[b, i * 128:(i + 1) * 128, :]),
                        f"xs{i % 4}", writes=[R("xs", i % 4)])
                  norm_tile(xs[i % 4][:, :], R("xs", i % 4), 0, i, junk[:, :], R("junk"), xn[i % 4][:, :], R("xn", i % 4),
                            PB[7 - (i % 2)], HT[:, :, i * 128:(i + 1) * 128], R("HT", i // 4), 2 * (i % 4))
              P.barrier()
              chk("norm1")
              cur_tab = [None]
              mmb = Rot([PB[0], PB[1], PB[2]])
              ppb = Rot([PB[3], PB[4]])
              msb = Rot([PB[5], PB[6]])
              ecnt = [0]
              for si, (soff, sw, chunks) in enumerate(slabs):
                  if si > 0:
                      chk(f"slab{si - 1}")
                  ws, ws_r = wsl[si % 2], R("wsl", si % 2)

                  def _ldslab(sj):
                      so_, sw_, _ = slabs[sj]
                      load_w(wsl[sj % 2][:, :, 0:sw_], win_d[:, so_:so_ + sw_].rearrange("(c p) n -> p c n", p=128),
                             f"wsl{sj % 2}", R("wsl", sj % 2))
                  if si == 0:
                      _ldslab(0)
                  if si + 1 < len(slabs):
                      _ldslab(si + 1)
                  for ch in chunks:
                      kind, idx, coff, M = ch["kind"], ch["idx"], ch["coff"], ch["M"]
                      if kind in ("VB", "WI"):
                          for tb in range(16):
                              bk, bk_r = mmb.next()
                              P.op("pe", [(I("matmul",
                                  bk[:, 0:M], lhsT=HT[:, c, tb * 128:(tb + 1) * 128], rhs=ws[:, c, coff:coff + M],
                                  start=(c == 0), stop=(c == 7))) for c in range(8)],
                                  reads=[ws_r, R("HT", tb // 4)], writes=[bk_r])
                              if kind == "WI":
                                  P.op("act", I("activation",
                                      out=weff[:, tb, :], in_=bk[:, 0:8], func=AF.Copy, scale=float(8 ** -0.5 * 64 ** -0.5)),
                                      reads=[bk_r], writes=[R("weff", tb)])
                              else:
                                  src4 = bk[:, :].rearrange("p (a c) -> p a c", a=4)
                                  dst4 = Vb[:, tb, :].rearrange("p (a c) -> p a c", a=4)
                                  P.op("act", I("activation", out=dst4[:, :, 0:64], in_=src4[:, :, 0:64], func=AF.Copy),
                                       reads=[bk_r], writes=[R("Vb", tb)])
                                  P.op("dve", I("tensor_copy", out=dst4[:, :, 128:192], in_=src4[:, :, 64:128]),
                                       reads=[bk_r], writes=[R("Vb", tb)])
                          continue
                      for tt in range(4):
                          tsl = slice(tt * 512, (tt + 1) * 512)
                          bk, bk_r = mmb.next()
                          P.op("pe", [(I("matmul",
                              bk[0:M, :], lhsT=ws[:, c, coff:coff + M], rhs=HT[:, c, tsl],
                              start=(c == 0), stop=(c == 7))) for c in range(8)],
                              reads=[ws_r, R("HT", tt)], writes=[bk_r])
                          if dbg_step == 1:
                              raise _Stop()
                          if kind == "CKV":
                              P.op("act", I("activation", out=ckf[:, :], in_=bk[:, :], func=AF.Copy),
                                   reads=[bk_r], writes=[R("ckf")])
                              P.op("act", I("activation", out=sqb[:, :], in_=bk[:, :], func=AF.Square),
                                   reads=[bk_r], writes=[R("sqb")])
                              sm, sm_r = msb.next()
                              P.op("pe", I("matmul", sm[:, :], lhsT=onesb[:, :], rhs=sqb[:, :], start=True, stop=True),
                                   reads=[R("sqb"), R("onesb")], writes=[sm_r])
                              P.op("act", I("activation", out=rtb[:, :], in_=sm[:, :], func=AF.Sqrt,
                                                                          scale=1.0 / 128, bias=1e-6),
                                   reads=[sm_r], writes=[R("rtb")])
                              P.op("dve", I("reciprocal", out=rtb[:, :], in_=rtb[:, :]), reads=[R("rtb")], writes=[R("rtb")])
                              P.op("dve", I("scalar_tensor_tensor",
                                  out=ckvT[:, tsl], in0=ckf[:, :], scalar=gall[:, 32:33], in1=rtb[:, :],
                                  op0=ALU.mult, op1=ALU.mult),
                                  reads=[R("ckf"), R("rtb"), R("gall")], writes=[R("ckvT", tt)])
                              for hh in range(8):
                                  kb_, kb_r = msb.next()
                                  P.op("pe", I("matmul",
                                      kb_[0:64, :], lhsT=wukT[:, hh * 64:(hh + 1) * 64], rhs=ckvT[:, tsl], start=True, stop=True),
                                      reads=[R("ckvT", tt), R("wukT")], writes=[kb_r])
                                  ecnt[0] += 1
                                  evac(ecnt[0], Kp[0:64, hh, tsl], kb_[0:64, :], [kb_r], [R("Kp", hh, tt)])
                              for j in range(4):
                                  kb = tt * 4 + j
                                  vb_, vb_r = msb.next()
                                  P.op("pe", I("matmul",
                                      vb_[:, :], lhsT=ckvT[:, kb * 128:(kb + 1) * 128], rhs=wuv[:, :], start=True, stop=True),
                                      reads=[R("ckvT", tt), R("wuv")], writes=[vb_r])
                                  src4 = vb_[:, :].rearrange("p (a c) -> p a c", a=4)
                                  dst4 = Vp[:, kb, :].rearrange("p (a c) -> p a c", a=4)
                                  P.op("act", I("activation", out=dst4[:, :, 0:64], in_=src4[:, :, 0:64], func=AF.Copy),
                                       reads=[vb_r], writes=[R("Vp", kb)])
                                  P.op("dve", I("tensor_copy", out=dst4[:, :, 128:192], in_=src4[:, :, 64:128]),
                                       reads=[vb_r], writes=[R("Vp", kb)])
                              continue
                          t32 = kind in ("QA", "KR")
                          tk = "32" if t32 else "64"
                          if cur_tab[0] != tk:
                              cur_tab[0] = tk
                              P.dma("sp", I("dma_start", out=tabc[:, :], in_=c_tab["cos" + tk][:, :]), "tabc", writes=[R("tabc")])
                              P.dma("sp", I("dma_start", out=tabs_[:, :], in_=c_tab["sin" + tk][:, :]), "tabs", writes=[R("tabs")])
                          cosT, sinT = tabc, tabs_
                          cos_r, sin_r = R("tabc"), R("tabs")
                          pm, pm_r = (p32, R("p32")) if t32 else (p64, R("p64"))
                          u = ecnt[0] = ecnt[0] + 1
                          qb_, qb_r = qbuf[u % 2], R("qbuf", u % 2)
                          t1, t1_r = t1b[u % 2], R("t1b", u % 2)
                          t2, t2_r = t2b[u % 2], R("t2b", u % 2)
                          P.op("act", I("activation", out=qb_[0:M, :], in_=bk[0:M, :], func=AF.Copy),
                               reads=[bk_r], writes=[qb_r])
                          if dbg_step == 2:
                              raise _Stop()
                          pb_, pb_r = ppb.next()
                          P.op("pe", I("matmul",
                              pb_[0:M, :], lhsT=pm[0:M, 0:M], rhs=qb_[0:M, :], start=True, stop=True),
                              reads=[qb_r, pm_r], writes=[pb_r])
                          if dbg_step == 3:
                              raise _Stop()
                          import os as _os
                          _v = _os.environ.get("DBG_VAR", "")
                          if _v == "v1":
                              P.op("dve", I("tensor_copy", out=t1[0:M, :], in_=bk[0:M, :]), reads=[bk_r], writes=[t1_r])
                          elif _v == "v5":
                              P.op("dve", I("tensor_copy", out=t1[0:M, :], in_=bk[0:M, :]), reads=[bk_r, qb_r, pb_r], writes=[t1_r])
                          elif _v == "v2":
                              P.op("dve", I("tensor_tensor", out=t1[0:M, :], in0=bk[0:M, :], in1=t2[0:M, :], op=ALU.mult),
                                   reads=[bk_r], writes=[t1_r])
                          elif _v == "v3":
                              P.op("dve", I("tensor_copy", out=t1[0:M, :], in_=cosT[0:M, tsl]), reads=[cos_r], writes=[t1_r])
                          elif _v == "v4":
                              P.op("dve", I("tensor_tensor", out=t1[0:M, :], in0=bk[0:M, :], in1=cosT[0:M, tsl], op=ALU.mult),
                                   reads=[bk_r], writes=[t1_r])
                          else:
                              P.op("dve", I("tensor_tensor",
                                  out=t1[0:M, :], in0=bk[0:M, :], in1=cosT[0:M, tsl], op=ALU.mult),
                                  reads=[bk_r, cos_r], writes=[t1_r])
                          if dbg_step == 4:
                              raise _Stop()
                          P.op("dve", I("tensor_tensor",
                              out=t2[0:M, :], in0=pb_[0:M, :], in1=sinT[0:M, tsl], op=ALU.mult),
                              reads=[pb_r, sin_r], writes=[t2_r])
                          if dbg_step == 5:
                              raise _Stop()
                          if kind == "KR":
                              for hh in range(8):
                                  P.op("pool", I("tensor_tensor",
                                      out=Kp[64:96, hh, tsl], in0=t1[64:96, :], in1=t2[64:96, :], op=ALU.add),
                                      reads=[t1_r, t2_r], writes=[R("Kp2", hh, tt)])
                          elif kind == "KI":
                              P.op("pool", I("tensor_tensor",
                                  out=kidx[:, tsl], in0=t1[:, :], in1=t2[:, :], op=ALU.add),
                                  reads=[t1_r, t2_r], writes=[R("kidx", tt)])
                          elif kind == "KB":
                              P.op("pool", I("tensor_tensor",
                                  out=kbT[:, idx, tsl], in0=t1[:, :], in1=t2[:, :], op=ALU.add),
                                  reads=[t1_r, t2_r], writes=[R("kbT", idx, tt)])
                          else:
                              sg, sg_r = stg[u % 4], R("stg", u % 4)
                              P.op("pool", I("tensor_tensor",
                                  out=sg[0:M, :], in0=t1[0:M, :], in1=t2[0:M, :], op=ALU.add),
                                  reads=[t1_r, t2_r], writes=[sg_r])
                              if dbg_step == 6:
                                  raise _Stop()
                              dst = {"QA": qa_s, "QI": qi_s, "QB": qb_s}[kind]
                              P.dma("sp", I("dma_start",
                                  out=dst[idx, 0:M, tsl], in_=sg[0:M, :]), f"stg{u % 4}",
                                  reads=[sg_r], writes=[R("scr", kind, idx, tt)])
              P.barrier()

              chk("proj")
              cv = Carver()
              cv.off = keys_end
              qaq = cv.take([128, 4, 512], BF16)
              qiq = cv.take([128, 4, 512], BF16)
              qbq = cv.take([128, 4, 512], BF16)
              idxb = [cv.take([128, S], F32) for _ in range(2)]
              Rb = [cv.take([128, 512], BF16) for _ in range(3)]
              diag = cv.take([128, 8, 128], BF16)
              Mk = cv.take([128, S], BF16)
              maskT = cv.take([128, 16, 512], BF16)
              Eb = [cv.take([128, 512], BF16) for _ in range(2)]
              Pmb = [cv.take([128, 512], BF16) for _ in range(4)]
              dsh = [cv.take([128, 512], F32)] * 2
              bis = cv.take([128, 32], F32)
              steps = cv.take([128, BIS_ITERS], F32)

              Lb = Rot([PB[0], PB[1]])
              accb = PB[2]
              Tb = PB[2]
              Ob = Rot([PB[6], PB[7]])
              sc_a = float((64 + 32) ** -0.5)
              sc_b = 0.125
              ucount = [0]

              seqc = [0]
              inflight = []
              obank = {}
              Sbh = [None]
              Sb_small = Rot([PB[3], PB[4], PB[5]])
              Sb_big = Rot([PB[3], PB[4], PB[5], PB[0], PB[1]])

              def load_qaq(hf, qt_):
                  qs_ = slice(qt_ * 512, (qt_ + 1) * 512)
                  P.dma("sp", I("dma_start", out=qaq[0:96, :, :], in_=qa_s[4 * hf:4 * hf + 4, :, qs_].rearrange("a r t -> r a t")),
                        "qaq", reads=[R("scr", "QA", i, qt_) for i in range(8)], writes=[R("qaq")])

              def emitQK(un):
                  g, hh, kb, qt_, seq, meng = un
                  c0 = max(0, kb - 4 * qt_) * 128
                  sbk, sbk_r = Sbh[0].next()
                  pr = (hh % 2) * 64
                  if g == "A":
                      if hh == 4 and kb == 0:
                          load_qaq(1, qt_)
                      P.op("pe", I("matmul", sbk[:, c0:512], lhsT=Kp[0:96, hh, kb * 128:(kb + 1) * 128],
                                   rhs=qaq[0:96, hh % 4, c0:512], start=True, stop=True),
                           reads=[R("Kp", hh, kb // 4), R("Kp2", hh, kb // 4), R("qaq")], writes=[sbk_r])
                  else:
                      P.op("pe", I("matmul", sbk[:, c0:512], lhsT=kbT[pr:pr + 64, hh // 2, kb * 128:(kb + 1) * 128],
                                   rhs=qbq[pr:pr + 64, hh // 2, c0:512], start=True, stop=True),
                           reads=[R("kbT", hh // 2, kb // 4), R("qbq")], writes=[sbk_r])
                  e, e_r = Eb[seq % 2], R("Eb", seq % 2)
                  P.op("act", I("activation", out=e[:, c0:512], in_=sbk[:, c0:512], func=AF.Exp,
                                scale=(sc_a if g == "A" else sc_b)),
                       reads=[sbk_r], writes=[e_r])
                  pmt, pmt_r = Pmb[seq % 4], R("Pmb", seq % 4)
                  if g == "A":
                      msk, msk_r = maskT[:, kb, c0:512], R("maskT")
                  else:
                      o_ = 128 * (4 * qt_ - kb) + 384
                      msk, msk_r = strip[:, o_ + c0:o_ + 512], R("strip")
                  P.op(meng, I("tensor_tensor", out=pmt[:, c0:512], in0=e[:, c0:512], in1=msk, op=ALU.mult),
                       reads=[e_r, msk_r], writes=[pmt_r])

              def emitPV(un):
                  g, hh, kb, qt_, seq, meng = un
                  NK_ = 4 * qt_ + 4
                  qs_ = slice(qt_ * 512, (qt_ + 1) * 512)
                  c0 = max(0, kb - 4 * qt_) * 128
                  if kb == 0:
                      obank[(g, hh)] = Ob.next()
                  ob, ob_r = obank[(g, hh)]
                  pmt, pmt_r = Pmb[seq % 4], R("Pmb", seq % 4)
                  V = Vp if g == "A" else Vb
                  vo = (hh // 2) * 192 + (hh % 2) * 64
                  P.op("pe", I("matmul", ob[:, c0:512], lhsT=V[:, kb, vo:vo + 128], rhs=pmt[:, c0:512],
                               start=(kb == 0), stop=(kb == NK_ - 1)),
                       reads=[pmt_r, R("Vp" if g == "A" else "Vb", kb)], writes=[ob_r])
                  if kb == NK_ - 1:
                      ev = (hh % 2 == 0)
                      np_, dp_ = (slice(0, 64), slice(64, 128)) if ev else (slice(64, 128), slice(0, 64))
                      d, d_r = dsh[0], R("dsh", 0)
                      P.op("act", I("activation", out=d[np_, :], in_=ob[dp_, :], func=AF.Ln),
                           reads=[ob_r], writes=[d_r])
                      P.op("act", I("activation", out=d[np_, :], in_=d[np_, :], func=AF.Exp, scale=-1.0), reads=[d_r], writes=[d_r])
                      chunk = (hh // 2) + (0 if g == "A" else 4)
                      P.op("dve", I("tensor_tensor", out=HT[np_, chunk, qs_], in0=ob[np_, :], in1=d[np_, :], op=ALU.mult),
                           reads=[ob_r, d_r], writes=[R("HT", qt_)])

              def feed(g, hh, kb, qt_, meng, LA):
                  un = (g, hh, kb, qt_, seqc[0], meng)
                  seqc[0] += 1
                  emitQK(un)
                  inflight.append(un)
                  while len(inflight) > LA:
                      emitPV(inflight.pop(0))

              def flush():
                  while inflight:
                      emitPV(inflight.pop(0))

              for qt in range(4):
                  qsl = slice(qt * 512, (qt + 1) * 512)
                  NK = 4 * qt + 4
                  load_qaq(0, qt)
                  P.dma("sp", I("dma_start", out=qiq[:, :, :], in_=qi_s[:, :, qsl].rearrange("a r t -> r a t")),
                        "qiq", reads=[R("scr", "QI", i, qt) for i in range(4)], writes=[R("qiq")])
                  P.dma("sp", I("dma_start", out=qbq[:, :, :], in_=qb_s[:, :, qsl].rearrange("a r t -> r a t")),
                        "qbq", reads=[R("scr", "QB", i, qt) for i in range(4)], writes=[R("qbq")])
                  bunits = [(hh, kb) for hh in range(8) for kb in range(NK)]
                  per = len(bunits) // 4

                  def indexer(tb):
                      tbl = tb - 4 * qt
                      lo = tbl * 128
                      n = 128 * (tb + 1)
                      idx_sb, idx_r = idxb[tb % 2], R("idx", tb % 2)
                      for hh in range(8):
                          P.op("pool", I("tensor_scalar",
                              out=diag[:, hh, :], in0=identb[:, :], scalar1=weff[:, tb, hh:hh + 1], scalar2=1.0,
                              op0=ALU.mult, op1=ALU.mult),
                              reads=[R("identb"), R("weff", tb)], writes=[R("diag", hh)])
                      for st in range(qt + 1):
                          wd = min(512, n - 512 * st)
                          acc, acc_r = accb

                          def emitL(hh):
                              lb, lb_r = Lb.next()
                              pr = (hh % 2) * 64
                              P.op("pe", I("matmul",
                                  lb[:, 0:wd], lhsT=qiq[pr:pr + 64, hh // 2, lo:lo + 128],
                                  rhs=kidx[pr:pr + 64, st * 512:st * 512 + wd], start=True, stop=True),
                                  reads=[R("qiq"), R("kidx", st)], writes=[lb_r])
                              rb, rb_r = Rb[hh % 3], R("Rb", hh % 3)
                              P.op("act", I("activation", out=rb[:, 0:wd], in_=lb[:, 0:wd], func=AF.Relu),
                                   reads=[lb_r], writes=[rb_r])

                          def emitA(hh):
                              rb, rb_r = Rb[hh % 3], R("Rb", hh % 3)
                              P.op("pe", I("matmul",
                                  acc[:, 0:wd], lhsT=diag[:, hh, :], rhs=rb[:, 0:wd], start=(hh == 0), stop=(hh == 7)),
                                  reads=[rb_r, R("diag", hh)], writes=[acc_r])

                          emitL(0)
                          emitL(1)
                          for hh in range(8):
                              if hh + 2 < 8:
                                  emitL(hh + 2)
                              emitA(hh)
                          c0 = st * 512
                          P.op("act", I("activation", out=idx_sb[:, c0:c0 + wd], in_=acc[:, 0:wd], func=AF.Copy),
                               reads=[acc_r], writes=[idx_r])
                          if st == qt:
                              P.op("pool", I("tensor_tensor",
                                  out=idx_sb[:, c0 + wd - 128:c0 + wd], in0=idx_sb[:, c0 + wd - 128:c0 + wd], in1=tri[:, :],
                                  op=ALU.add), reads=[idx_r, R("tri")], writes=[idx_r])

                  def bisect(tb):
                      n = 128 * (tb + 1)
                      idx_sb, idx_r = idxb[tb % 2], R("idx", tb % 2)
                      thr = bis[:, 0:1]
                      if tb >= 2:
                          mx, mn, W, cnt, a_ = bis[:, 1:2], bis[:, 2:3], bis[:, 3:4], bis[:, 4:5], bis[:, 5:6]
                          RB = R("bis")
                          P.op("dve", I("tensor_reduce", out=mx, in_=idx_sb[:, 0:n], op=ALU.max, axis=AX.X),
                               reads=[idx_r], writes=[RB])
                          P.op("dve", I("tensor_reduce", out=mn, in_=idx_sb[:, 0:n - 128], op=ALU.min, axis=AX.X),
                               reads=[idx_r], writes=[RB])
                          P.op("dve", I("tensor_tensor", out=W, in0=mx, in1=mn, op=ALU.subtract), reads=[RB], writes=[RB])
                          P.op("dve", I("tensor_tensor", out=thr, in0=mx, in1=mn, op=ALU.add), reads=[RB], writes=[RB])
                          P.op("dve", I("tensor_scalar", out=thr, in0=thr, scalar1=0.5, scalar2=None, op0=ALU.mult),
                               reads=[RB], writes=[RB])
                          P.op("dve", I("tensor_scalar", out=steps[:, :], in0=cvec[:, :], scalar1=W, scalar2=None, op0=ALU.mult),
                               reads=[RB, R("cvec")], writes=[RB])
                          for it in range(BIS_ITERS):
                              P.op("dve", I("tensor_scalar",
                                  out=Mk[:, 0:n], in0=idx_sb[:, 0:n], scalar1=thr, scalar2=None,
                                  op0=ALU.is_ge, op1=ALU.add, accum_out=cnt),
                                  reads=[idx_r, RB], writes=[R("Mk"), RB])
                              P.op("dve", I("tensor_scalar", out=a_, in0=cnt, scalar1=255.5, scalar2=0.5,
                                            op0=ALU.is_ge, op1=ALU.subtract),
                                   reads=[RB], writes=[RB])
                              P.op("dve", I("scalar_tensor_tensor",
                                  out=thr, in0=a_, scalar=steps[:, it:it + 1], in1=thr, op0=ALU.mult, op1=ALU.add),
                                  reads=[RB], writes=[RB])
                          P.op("dve", I("tensor_tensor", out=thr, in0=thr, in1=steps[:, BIS_ITERS - 1:BIS_ITERS], op=ALU.subtract),
                               reads=[RB], writes=[RB])
                          P.op("dve", I("tensor_scalar", out=Mk[:, 0:n], in0=idx_sb[:, 0:n], scalar1=thr, scalar2=None,
                                        op0=ALU.is_ge),
                               reads=[idx_r, RB], writes=[R("Mk")])
                      else:
                          P.op("dve", I("tensor_scalar", out=Mk[:, 0:n], in0=idx_sb[:, 0:n], scalar1=-1.0e29, scalar2=None,
                                        op0=ALU.is_ge),
                               reads=[idx_r], writes=[R("Mk")])

                  def transposes(tb):
                      lo = (tb - 4 * qt) * 128
                      tbk, tbk_r = Tb
                      tv = bfv(tbk[:, :]).rearrange("p (a b) -> p a b", a=8)
                      for g0 in range(0, tb + 1, 8):
                          g1 = min(tb + 1, g0 + 8)
                          P.op("pe", [(I("transpose", out=tv[:, kb - g0, :], in_=Mk[:, kb * 128:(kb + 1) * 128],
                                         identity=identb[:, :])) for kb in range(g0, g1)],
                               reads=[R("Mk"), R("identb")], writes=[tbk_r])
                          ucount[0] += 1
                          evac(ucount[0], maskT[:, g0:g1, lo:lo + 128], tv[:, 0:g1 - g0, :], [tbk_r], [R("maskT")])

                  indexer(4 * qt)
                  for tbl in range(4):
                      tb = 4 * qt + tbl
                      bisect(tb)
                      if tbl < 3:
                          indexer(tb + 1)
                      Sbh[0] = Sb_small
                      for (hh, kb) in bunits[tbl * per:(tbl + 1) * per]:
                          feed("B", hh, kb, qt, "pool", 2)
                      transposes(tb)

                  if qt == 0:
                      chk("idx0")
                  Sbh[0] = Sb_big
                  for hh in range(8):
                      for kb in range(NK):
                          feed("A", hh, kb, qt, ("pool" if seqc[0] % 3 == 0 else "dve"), 3)
                  flush()
              P.barrier()

              chk("attn")
              cv = Carver()
              XR = cv.take([128, 16, D], F32)
              x_end = cv.off
              wA = [cv.take([128, 8, 512], BF16) for _ in range(2)]
              mmb = Rot([PB[0], PB[1], PB[2], PB[3]])
              for q4 in range(4):
                  P.dma("sp", I("dma_start",
                      out=XR[:, q4 * 4:(q4 + 1) * 4, :], in_=x_d[b, q4 * 512:(q4 + 1) * 512, :].rearrange("(n p) d -> p n d", p=128)),
                      f"xr{q4}", writes=[R("XR", t) for t in range(q4 * 4, q4 * 4 + 4)])

              def out_proj(w_d, srcT, src_res_fn, keyp):
                  for half in range(2):
                      load_w(wA[half][:, :, :], w_d[:, half * 512:(half + 1) * 512].rearrange("(c p) n -> p c n", p=128),
                             f"wA{half}", R("wA", half))
                  for half in range(2):
                      ws, ws_r = wA[half], R("wA", half)
                      for tb in range(16):
                          bk, bk_r = mmb.next()
                          P.op("pe", [(I("matmul",
                              bk[:, :], lhsT=srcT[:, c, tb * 128:(tb + 1) * 128], rhs=ws[:, c, :],
                              start=(c == 0), stop=(c == 7))) for c in range(8)],
                              reads=[ws_r, src_res_fn(tb)], writes=[bk_r])
                          P.op("dve", I("tensor_tensor",
                              out=XR[:, tb, half * 512:(half + 1) * 512], in0=bk[:, :], in1=XR[:, tb, half * 512:(half + 1) * 512],
                              op=ALU.add), reads=[bk_r, R("XR", tb)], writes=[R("XR", tb)])

              out_proj(wout_d, HT, lambda tb: R("HT", tb // 4), "wout")
              P.barrier()

              chk("wout")
              cv = Carver()
              cv.off = x_end
              OC = cv.take([128, 8, S], BF16)
              wA = [cv.take([128, 8, 512], BF16) for _ in range(2)]
              junk = cv.take([128, D], BF16)
              xn = [cv.take([128, D], BF16) for _ in range(4)]
              ms = [cv.take([128, D], F32)] * 2
              mT = cv.take([128, 8, 256], BF16)
              KcT = cv.take([128, 8, 256], BF16)
              Vc = cv.take([128, 2, D], BF16)
              qcT = cv.take([128, 8, 512], BF16)
              Ec = [cv.take([128, 512], BF16) for _ in range(4)]
              rdn = [cv.take([128, 512], F32)] * 2
              for i in range(16):
                  norm_tile(XR[:, i, :], R("XR", i), 8, i, junk[:, :], R("junk"), xn[i % 4][:, :], R("xn", i % 4),
                            PB[7 - (i % 2)], HT[:, :, i * 128:(i + 1) * 128], R("HT", i // 4), 2 * (i % 4))
              for i in range(2):
                  P.dma("sp", I("dma_start", out=ms[i][:, :], in_=mem_d[b, i * 128:(i + 1) * 128, :]), "ms0",
                        writes=[R("ms", 0)])
                  norm_tile(ms[i][:, :], R("ms", 0), 16, i, junk[:, :], R("junk"), xn[i % 4][:, :], R("xn", i % 4),
                            PB[7 - (i % 2)], mT[:, :, i * 128:(i + 1) * 128], R("mT"), 2 * (i % 4))
              mmb = Rot([PB[0], PB[1], PB[2]])
              ec = 0
              for half in range(2):
                  ws, ws_r = wA[half], R("wA", half)
                  load_w(ws[:, :, :], wkv_d[:, half * 512:(half + 1) * 512].rearrange("(c p) n -> p c n", p=128), f"wA{half}", ws_r)
                  for j4 in range(4):
                      j = half * 4 + j4
                      bk, bk_r = mmb.next()
                      P.op("pe", [(I("matmul",
                          bk[:, 0:256], lhsT=ws[:, c, j4 * 128:(j4 + 1) * 128], rhs=mT[:, c, :], start=(c == 0), stop=(c == 7)))
                          for c in range(8)], reads=[ws_r, R("mT")], writes=[bk_r])
                      ec += 1
                      evac(ec, KcT[:, j, :], bk[:, 0:256], [bk_r], [R("KcT", j)])
              for half in range(2):
                  ws, ws_r = wA[half], R("wA", half)
                  load_w(ws[:, :, :], wkv_d[:, D + half * 512:D + (half + 1) * 512].rearrange("(c p) n -> p c n", p=128),
                         f"wA{half}", ws_r)
                  for mc in range(2):
                      bk, bk_r = mmb.next()
                      P.op("pe", [(I("matmul",
                          bk[:, :], lhsT=mT[:, c, mc * 128:(mc + 1) * 128], rhs=ws[:, c, :], start=(c == 0), stop=(c == 7)))
                          for c in range(8)], reads=[ws_r, R("mT")], writes=[bk_r])
                      ec += 1
                      evac(ec, Vc[:, mc, half * 512:(half + 1) * 512], bk[:, :], [bk_r], [R("Vc", mc)])
              for half in range(2):
                  load_w(wA[half][:, :, :], wq_d[:, half * 512:(half + 1) * 512].rearrange("(c p) n -> p c n", p=128),
                         f"wA{half}", R("wA", half))
              sbk2 = Rot([PB[3], PB[4]])
              obk = Rot([PB[5], PB[6]])
              dbk = PB[7]
              for tt in range(4):
                  tsl = slice(tt * 512, (tt + 1) * 512)
                  for j in range(8):
                      bk, bk_r = mmb.next()
                      P.op("pe", [(I("matmul",
                          bk[:, :], lhsT=wA[j // 4][:, c, (j % 4) * 128:(j % 4 + 1) * 128], rhs=HT[:, c, tsl],
                          start=(c == 0), stop=(c == 7))) for c in range(8)],
                          reads=[R("wA", j // 4), R("HT", tt)], writes=[bk_r])
                      ec += 1
                      evac(ec, qcT[:, j, :], bk[:, :], [bk_r], [R("qcT", j)])
                  for hh in range(4):
                      for mc in range(2):
                          sk, sk_r = sbk2.next()
                          P.op("pe", [(I("matmul",
                              sk[:, :], lhsT=KcT[:, 2 * hh + jj, mc * 128:(mc + 1) * 128], rhs=qcT[:, 2 * hh + jj, :],
                              start=(jj == 0), stop=(jj == 1))) for jj in range(2)],
                              reads=[R("KcT", 2 * hh), R("KcT", 2 * hh + 1), R("qcT", 2 * hh), R("qcT", 2 * hh + 1)], writes=[sk_r])
                          e, e_r = Ec[(hh % 2) * 2 + mc], R("Ec", (hh % 2) * 2 + mc)
                          P.op("act", I("activation", out=e[:, :], in_=sk[:, :], func=AF.Exp, scale=1.0 / 16),
                               reads=[sk_r], writes=[e_r])
                      e0, e1 = Ec[(hh % 2) * 2], Ec[(hh % 2) * 2 + 1]
                      er = [R("Ec", (hh % 2) * 2), R("Ec", (hh % 2) * 2 + 1)]
                      db, db_r = dbk
                      P.op("pe", [(I("matmul", db[:, :], lhsT=onesb[:, :], rhs=e[:, :],
                                                                  start=(mc == 0), stop=(mc == 1))) for mc, e in ((0, e0), (1, e1))],
                           reads=er + [R("onesb")], writes=[db_r])
                      rd, rd_r = rdn[0], R("rdn", 0)
                      P.op("dve", I("reciprocal", out=rd[:, :], in_=db[:, :]), reads=[db_r], writes=[rd_r])
                      for jj in range(2):
                          ob, ob_r = obk.next()
                          P.op("pe", [(I("matmul",
                              ob[:, :], lhsT=Vc[:, mc, (2 * hh + jj) * 128:(2 * hh + jj + 1) * 128], rhs=e[:, :],
                              start=(mc == 0), stop=(mc == 1))) for mc, e in ((0, e0), (1, e1))],
                              reads=er + [R("Vc", 0), R("Vc", 1)], writes=[ob_r])
                          P.op("dve", I("tensor_tensor",
                              out=OC[:, 2 * hh + jj, tsl], in0=ob[:, :], in1=rd[:, :], op=ALU.mult),
                              reads=[ob_r, rd_r], writes=[R("OC", tt)])
              mmb = Rot([PB[0], PB[1], PB[2], PB[3]])
              P.barrier()

              def out_proj2(w_d, srcT, src_res_fn):
                  for half in range(2):
                      load_w(wA[half][:, :, :], w_d[:, half * 512:(half + 1) * 512].rearrange("(c p) n -> p c n", p=128),
                             f"wA{half}", R("wA", half))
                  for half in range(2):
                      ws, ws_r = wA[half], R("wA", half)
                      for tb in range(16):
                          bk, bk_r = mmb.next()
                          P.op("pe", [(I("matmul",
                              bk[:, :], lhsT=srcT[:, c, tb * 128:(tb + 1) * 128], rhs=ws[:, c, :],
                              start=(c == 0), stop=(c == 7))) for c in range(8)],
                              reads=[ws_r, src_res_fn(tb)], writes=[bk_r])
                          P.op("dve", I("tensor_tensor",
                              out=XR[:, tb, half * 512:(half + 1) * 512], in0=bk[:, :], in1=XR[:, tb, half * 512:(half + 1) * 512],
                              op=ALU.add), reads=[bk_r, R("XR", tb)], writes=[R("XR", tb)])

              out_proj2(wo_d, OC, lambda tb: R("OC", tb // 4))
              P.barrier()

              chk("cross")
              cv = Carver()
              cv.off = x_end
              junk = cv.take([128, D], BF16)
              xn = [cv.take([128, D], BF16) for _ in range(4)]
              wU = [cv.take([128, 8, 512], BF16) for _ in range(2)]
              wD = [cv.take([128, 4, D], BF16) for _ in range(2)]
              hid = [cv.take([128, 4, 512], BF16) for _ in range(2)]
              rl = [cv.take([128, 512], BF16) for _ in range(2)]
              gF = cv.take([128, D], F32)
              ot = [cv.take([128, D], F32) for _ in range(2)]
              P.dma("sp", I("dma_start", out=gF[:, :], in_=gF_d[:, :]), "gF", writes=[R("gF")])
              for i in range(16):
                  norm_tile(XR[:, i, :], R("XR", i), 24, i, junk[:, :], R("junk"), xn[i % 4][:, :], R("xn", i % 4),
                            PB[7 - (i % 2)], HT[:, :, i * 128:(i + 1) * 128], R("HT", i // 4), 2 * (i % 4))
              upb = Rot([PB[0], PB[1], PB[2]])
              dnb = Rot([PB[3], PB[4], PB[5], PB[6]])
              cnt = 0
              for fg in range(8):
                  wu, wu_r = wU[fg % 2], R("wU", fg % 2)
                  wd_, wd_r = wD[fg % 2], R("wD", fg % 2)
                  def _ldmlp(fj):
                      load_w(wU[fj % 2][:, :, :], wup_d[:, fj * 512:(fj + 1) * 512].rearrange("(c p) n -> p c n", p=128),
                             f"wU{fj % 2}", R("wU", fj % 2))
                      load_w(wD[fj % 2][:, :, :], wdn_d[fj * 512:(fj + 1) * 512, :].rearrange("(c p) n -> p c n", p=128),
                             f"wD{fj % 2}", R("wD", fj % 2))
                  if fg == 0:
                      _ldmlp(0)
                  if fg + 1 < 8:
                      _ldmlp(fg + 1)
                  for tt in range(4):
                      tsl = slice(tt * 512, (tt + 1) * 512)
                      cnt += 1
                      hd, hd_r = hid[cnt % 2], R("hid", cnt % 2)
                      for fc in range(4):
                          bk, bk_r = upb.next()
                          P.op("pe", [(I("matmul",
                              bk[:, :], lhsT=wu[:, c, fc * 128:(fc + 1) * 128], rhs=HT[:, c, tsl],
                              start=(c == 0), stop=(c == 7))) for c in range(8)],
                              reads=[wu_r, R("HT", tt)], writes=[bk_r])
                          r_, r_r = rl[fc % 2], R("rl", fc % 2)
                          P.op("act", I("activation", out=r_[:, :], in_=bk[:, :], func=AF.Relu),
                               reads=[bk_r], writes=[r_r])
                          P.op("pool", I("tensor_tensor", out=hd[:, fc, :], in0=r_[:, :], in1=r_[:, :],
                                                                                         op=ALU.mult),
                               reads=[r_r], writes=[hd_r])
                      for t4 in range(4):
                          tb = tt * 4 + t4
                          for half in range(2):
                              bk, bk_r = dnb.next()
                              P.op("pe", [(I("matmul",
                                  bk[:, :], lhsT=hd[:, fc, t4 * 128:(t4 + 1) * 128], rhs=wd_[:, fc, half * 512:(half + 1) * 512],
                                  start=(fc == 0), stop=(fc == 3))) for fc in range(4)],
                                  reads=[hd_r, wd_r], writes=[bk_r])
                              P.op("dve", I("tensor_tensor",
                                  out=XR[:, tb, half * 512:(half + 1) * 512], in0=bk[:, :], in1=XR[:, tb, half * 512:(half + 1) * 512],
                                  op=ALU.add), reads=[bk_r, R("XR", tb)], writes=[R("XR", tb)])
              for i in range(16):
                  src = XR[:, i, :]
                  ssc = 2 * (i % 2)
                  ss, rs = stat[:, ssc:ssc + 1], stat[:, ssc + 1:ssc + 2]
                  ss_r, rs_r = R("stat", ssc), R("stat", ssc + 1)
                  o_, o_r = ot[i % 2], R("ot", i % 2)
                  P.op("act", I("activation", out=junk[:, :], in_=src, func=AF.Square, accum_out=ss),
                       reads=[R("XR", i)], writes=[R("junk"), ss_r])
                  P.op("act", I("activation", out=rs, in_=ss, func=AF.Sqrt, scale=1.0 / D, bias=1e-6),
                       reads=[ss_r], writes=[rs_r])
                  P.op("dve", I("reciprocal", out=rs, in_=rs), reads=[rs_r], writes=[rs_r])
                  P.op("dve", I("scalar_tensor_tensor",
                      out=o_[:, :], in0=src, scalar=rs, in1=gF[:, :], op0=ALU.mult, op1=ALU.mult),
                      reads=[R("XR", i), rs_r, R("gF")], writes=[o_r])
                  out_toks.append(P.dma("sp", I("dma_start", out=out_d[b, i * 128:(i + 1) * 128, :], in_=o_[:, :]),
                                        f"ot{i % 2}", reads=[o_r]))
              P.barrier()

        except _Stop:
            P.barrier()
        P.wait_tokens("sp", out_toks)
        with nc.Block() as block:
            P.emit(block)
    nc._mk_stats = (P.nops, P.nwaits)
    return nc


_NC_CACHE = {}


def kernel(x, mem, norm_mix_g, w_in, kv_norm_g, w_uk, w_uv, w_out, norm_cross_g, norm_mem_g,
           w_q_cross, w_kv_cross, w_o_cross, norm_mlp_g, w_up, w_down, norm_final_g):
    f = lambda a: np.ascontiguousarray(np.asarray(a, dtype=np.float32))
    x, mem = f(x), f(mem)
    win_cols, _ = _win_plan()
    win = f(f(w_in)[0][:, win_cols])
    wukT = f(np.transpose(f(w_uk)[0], (2, 0, 1)).reshape(128, 512))
    wuv = f(np.transpose(f(w_uv)[0], (1, 0, 2)).reshape(128, 512))
    gcol = lambda g: f(g).reshape(8, 128).T
    gall = f(np.concatenate([gcol(norm_mix_g), gcol(norm_cross_g), gcol(norm_mem_g), gcol(norm_mlp_g),
                             f(kv_norm_g).reshape(128, 1)], axis=1))
    gF = f(np.broadcast_to(f(norm_final_g).reshape(1, D), (128, D)))
    consts = _consts()
    if "nc" not in _NC_CACHE:
        _NC_CACHE["nc"] = build_nc(NB)
    nc = _NC_CACHE["nc"]
    shared = dict(win=win, wukT=wukT, wuv=wuv, wout=f(w_out)[0], wq=f(w_q_cross)[0], wkv=f(w_kv_cross)[0],
                  wo=f(w_o_cross)[0], wup=f(w_up)[0], wdn=f(w_down)[0], gall=gall, gF=gF, **consts)
    in_maps = []
    for c in range(NCORES):
        m = dict(shared)
        m["x"] = x[c * NB:(c + 1) * NB]
        m["mem"] = mem[c * NB:(c + 1) * NB]
        in_maps.append(m)
    res = run_bass_kernel_spmd(nc, in_maps, core_ids=list(range(NCORES)))
    return np.concatenate([res.results[c]["out"] for c in range(NCORES)], axis=0).astype(np.float32)
```

```python
import math
from contextlib import ExitStack
import numpy as np
import concourse.bass as bass
import concourse.mybir as mybir
from concourse.bass_utils import run_bass_kernel_spmd

F32 = mybir.dt.float32
BF16 = mybir.dt.bfloat16
AF = mybir.ActivationFunctionType
ALU = mybir.AluOpType
AX = mybir.AxisListType

NCORES = 8
NB = 4
S = 2048
D = 1024
BIS_ITERS = 18
NEG = -1.0e30
STRIP_W = 2816


class Res:
    __slots__ = ("name", "writer", "readers", "excl")

    def __init__(self, name):
        self.name = name
        self.writer = None
        self.readers = []
        self.excl = (name[0] == "psum")


class ResReg:
    def __init__(self):
        self.d = {}

    def __call__(self, *key):
        r = self.d.get(key)
        if r is None:
            r = self.d[key] = Res(key)
        return r


class Eng:
    def __init__(self, name, sem, is_pe=False):
        self.name, self.sem, self.is_pe = name, sem, is_pe
        self.count = 0
        self.ops = []
        self.waited = {}


class Prog:
    def __init__(self, nc, sems, dma_sem_pool):
        self.nc = nc
        self.E = {n: Eng(n, sems[n], is_pe=(n == "pe")) for n in ("pe", "act", "dve", "pool", "sp")}
        self.dma_sems = {}
        self.sem_pool = dma_sem_pool
        self.nwaits = 0
        self.nops = 0

    def _deps(self, reads, writes):
        deps = []
        for r in reads:
            if r.writer is not None:
                deps.append(r.writer)
            if r.excl:
                deps.extend(r.readers)
        for w in writes:
            if w.writer is not None:
                deps.append(w.writer)
            deps.extend(w.readers)
        return deps

    def _waits(self, eng, deps):
        need = {}
        for d in deps:
            key = (d[0], d[1])
            if need.get(key, 0) < d[2]:
                need[key] = d[2]
        waits = []
        for key, v in need.items():
            if key[0] == "e" and key[1] == eng.name and eng.is_pe:
                continue
            if eng.waited.get(key, 0) >= v:
                continue
            eng.waited[key] = v
            if key[0] == "e":
                waits.append((self.E[key[1]].sem, v))
            else:
                waits.append((self.dma_sems[key[1]][0], v * 16))
        self.nwaits += len(waits)
        return waits

    def _commit(self, tok, reads, writes):
        for r in reads:
            if r.excl:
                r.writer = tok
                r.readers = []
            else:
                r.readers.append(tok)
        for w in writes:
            w.writer = tok
            w.readers = []

    def op(self, engname, fns, reads=(), writes=()):
        eng = self.E[engname]
        if isinstance(fns, tuple):
            fns = [fns]
        waits = self._waits(eng, self._deps(reads, writes))
        eng.count += 1
        tok = ("e", engname, eng.count)
        eng.ops.append((waits, fns, ("e", eng.sem)))
        self._commit(tok, reads, writes)
        self.nops += len(fns)
        return tok

    def dma(self, queue, fn, semkey, reads=(), writes=()):
        eng = self.E[queue]
        if semkey not in self.dma_sems:
            self.dma_sems[semkey] = [self.sem_pool.pop(), 0]
        ent = self.dma_sems[semkey]
        deps = self._deps(reads, writes)
        if ent[1] > 0:
            deps.append(("d", semkey, ent[1]))
        waits = self._waits(eng, deps)
        ent[1] += 1
        tok = ("d", semkey, ent[1])
        eng.ops.append((waits, [fn], ("d", ent[0])))
        self._commit(tok, reads, writes)
        self.nops += 1
        return tok

    def wait_tokens(self, engname, toks):
        eng = self.E[engname]
        waits = self._waits(eng, list(toks))
        if waits:
            eng.ops.append((waits, [], None))

    def barrier(self):
        toks = []
        for e in self.E.values():
            if e.count > 0:
                toks.append(("e", e.name, e.count))
        for k, (s, c) in self.dma_sems.items():
            if c > 0:
                toks.append(("d", k, c))
        for e in self.E.values():
            self.wait_tokens(e.name, [t for t in toks if not (t[0] == "e" and t[1] == e.name and e.is_pe)])

    def new_epoch(self, sems, reg):
        for e in self.E.values():
            e.sem = sems[e.name]
            e.count = 0
            e.waited = {k: v for k, v in e.waited.items() if k[0] == "d"}
        for r in reg.d.values():
            if r.writer is not None and r.writer[0] == "e":
                r.writer = None
            r.readers = [t for t in r.readers if t[0] != "e"]

    def emit(self, block):
        def run(eng, h):
            for waits, fns, inc in eng.ops:
                for sem, v in waits:
                    h.wait_ge(sem, v)
                ins = None
                for f in fns:
                    ins = getattr(h, f[0])(*f[1], **f[2])
                if inc is not None and ins is not None:
                    ins.then_inc(inc[1], 1 if inc[0] == "e" else 16)

        block.tensor(lambda h: run(self.E["pe"], h))
        block.scalar(lambda h: run(self.E["act"], h))
        block.vector(lambda h: run(self.E["dve"], h))
        block.gpsimd(lambda h: run(self.E["pool"], h))
        block.sync(lambda h: run(self.E["sp"], h))


def I(name, *a, **k):
    return (name, a, k)


class Rot:
    def __init__(self, items):
        self.items, self.i = items, 0

    def next(self):
        it = self.items[self.i % len(self.items)]
        self.i += 1
        return it


O_QN, O_QR, O_CKV, O_KR, O_QI, O_KI, O_WI, O_QKV = 0, 512, 768, 896, 928, 1440, 1504, 1512


def _win_plan():
    cols = []
    slabs = []

    def slab(chunks_cols):
        off = len(cols)
        chunks = []
        for kind, idx, cc in chunks_cols:
            chunks.append(dict(kind=kind, idx=idx, coff=len(cols) - off, M=len(cc)))
            cols.extend(cc)
        slabs.append((off, len(cols) - off, chunks))

    def qa(h):
        return list(range(O_QN + 64 * h, O_QN + 64 * h + 64)) + list(range(O_QR + 32 * h, O_QR + 32 * h + 32))

    r = lambda a, n: list(range(a, a + n))
    slab([("QA", h, qa(h)) for h in range(0, 4)])
    slab([("QA", h, qa(h)) for h in range(4, 8)] + [("KR", 0, r(O_KI, 64) + r(O_KR, 32))])
    slab([("QI", j, r(O_QI + 128 * j, 128)) for j in range(4)])
    slab([("QB", j, r(O_QKV + 128 * j, 128)) for j in range(4)])
    slab([("KB", j, r(O_QKV + 512 + 128 * j, 128)) for j in range(4)])
    slab([("KI", 0, r(O_KI, 64) + r(O_KI, 64)), ("CKV", 0, r(O_CKV, 128))])
    slab([("VB", 0, r(O_QKV + 1024, 512))])
    slab([("WI", 0, r(O_WI, 8))])
    return np.array(cols, dtype=np.int64), slabs


def _consts():
    c = {}
    c["ident"] = np.eye(128, dtype=np.float32)
    tri = np.zeros((128, 128), np.float32)
    tri[np.triu_indices(128, 1)] = NEG
    c["tri"] = tri
    i = np.arange(128)[:, None]
    cc = np.arange(STRIP_W)[None, :]
    dl = cc - i - 384
    m = ((dl >= 0) & (dl <= 128)).astype(np.float32) + ((dl >= 0) & (dl % 4 == 0) & (dl <= 512)) + \
        ((dl >= 0) & (dl % 16 == 0) & (dl <= 2048))
    c["strip"] = m.astype(np.float32)
    p32 = np.zeros((128, 128), np.float32)
    for k in range(16):
        p32[64 + k, 80 + k] = 1
        p32[80 + k, 64 + k] = 1
    p64 = np.zeros((128, 128), np.float32)
    for hb in (0, 64):
        for k in range(32):
            p64[hb + k, hb + 32 + k] = 1
            p64[hb + 32 + k, hb + k] = 1
    c["p32"], c["p64"] = p32, p64
    c["cvec"] = np.tile((2.0 ** -(np.arange(BIS_ITERS) + 1.0))[None, :], (128, 1)).astype(np.float32)
    pos = np.arange(S, dtype=np.float32)[None, :]
    inv32 = (10000.0 ** (-np.arange(0, 32, 2, dtype=np.float32) / 32)).astype(np.float32)
    inv64 = (10000.0 ** (-np.arange(0, 64, 2, dtype=np.float32) / 64)).astype(np.float32)
    cos32 = np.ones((128, S), np.float32)
    sin32 = np.zeros((128, S), np.float32)
    a32 = inv32[:, None] * pos
    cos32[64:80], cos32[80:96] = np.cos(a32), np.cos(a32)
    sin32[64:80], sin32[80:96] = -np.sin(a32), np.sin(a32)
    a64 = inv64[:, None] * pos
    cos64 = np.zeros((128, S), np.float32)
    sin64 = np.zeros((128, S), np.float32)
    for hb in (0, 64):
        cos64[hb:hb + 32], cos64[hb + 32:hb + 64] = np.cos(a64), np.cos(a64)
        sin64[hb:hb + 32], sin64[hb + 32:hb + 64] = -np.sin(a64), np.sin(a64)
    c["cos32"], c["sin32"], c["cos64"], c["sin64"] = cos32, sin32, cos64, sin64
    return c


class _Stop(Exception):
    pass


def build_nc(nb=NB, stop=None, dbg_step=0):
    nc = bass.Bass("TRN2", target_bir_lowering=False)
    win_cols, slabs = _win_plan()
    NCOL = len(win_cols)

    def din(name, shape, dt=F32):
        return nc.dram_tensor(name, list(shape), dt, kind="ExternalInput").ap()

    x_d = din("x", [nb, S, D])
    mem_d = din("mem", [nb, 256, D])
    win_d = din("win", [D, NCOL])
    wuk_d = din("wukT", [128, 512])
    wuv_d = din("wuv", [128, 512])
    wout_d = din("wout", [D, D])
    wq_d = din("wq", [D, D])
    wkv_d = din("wkv", [D, 2 * D])
    wo_d = din("wo", [D, D])
    wup_d = din("wup", [D, 4 * D])
    wdn_d = din("wdn", [4 * D, D])
    gall_d = din("gall", [128, 33])
    gF_d = din("gF", [128, D])
    c_ident = din("ident", [128, 128])
    c_tri = din("tri", [128, 128])
    c_strip = din("strip", [128, STRIP_W])
    c_p32 = din("p32", [128, 128])
    c_p64 = din("p64", [128, 128])
    c_cvec = din("cvec", [128, BIS_ITERS])
    c_tab = {k: din(k, [128, S]) for k in ("cos32", "sin32", "cos64", "sin64")}
    out_d = nc.dram_tensor("out", [nb, S, D], F32, kind="ExternalOutput").ap()
    qa_s = nc.dram_tensor("qa_s", [8, 96, S], BF16, kind="ExternalOutput").ap()
    qi_s = nc.dram_tensor("qi_s", [4, 128, S], BF16, kind="ExternalOutput").ap()
    qb_s = nc.dram_tensor("qb_s", [4, 128, S], BF16, kind="ExternalOutput").ap()

    with ExitStack() as es:
        def sb(name, shape, dt):
            return es.enter_context(nc.sbuf_tensor("sb_" + name, list(shape), dt))

        def sem(name):
            return es.enter_context(nc.semaphore(name))

        sems = {n: sem("s_" + n) for n in ("pe", "act", "dve", "pool", "sp")}
        P = Prog(nc, sems, [sem(f"dq{i}") for i in range(40)])
        R = ResReg()

        HT = sb("HT", [128, 8, S], BF16)
        identb = sb("identb", [128, 128], BF16)
        onesb = sb("onesb", [128, 128], BF16)
        tri = sb("tri", [128, 128], F32)
        strip = sb("strip", [128, STRIP_W], BF16)
        p32 = sb("p32", [128, 128], BF16)
        p64 = sb("p64", [128, 128], BF16)
        cvec = sb("cvec", [128, BIS_ITERS], F32)
        gall = sb("gall", [128, 33], F32)
        wukT = sb("wukT", [128, 512], BF16)
        wuv = sb("wuv", [128, 512], BF16)
        weff = sb("weff", [128, 16, 8], F32)
        stat = sb("stat", [128, 64], F32)
        ARENA_B = 165 * 1024
        arena = sb("arena", [128, ARENA_B // 2], BF16)
        psum = [es.enter_context(nc.psum_tensor(f"ps{i}", [128, 512], F32)) for i in range(8)]
        PB = [(psum[i], R("psum", i)) for i in range(8)]

        class Carver:
            def __init__(self):
                self.off = 0

            def take(self, shape, dt):
                esz = 4 if dt == F32 else 2
                n = int(np.prod(shape[1:]))
                nbytes = n * esz
                nbytes_al = (nbytes + 63) // 64 * 64
                a = arena[:, self.off // 2: (self.off + nbytes) // 2]
                self.off += nbytes_al
                assert self.off <= ARENA_B, ("arena overflow", self.off)
                if dt == F32:
                    a = a.bitcast(F32)
                if len(shape) == 3:
                    a = a.rearrange("p (a b) -> p a b", a=shape[1])
                elif len(shape) == 4:
                    a = a.rearrange("p (a b c) -> p a b c", a=shape[1], b=shape[2])
                return a

        def bfv(ps_ap):
            return ps_ap.bitcast(BF16)

        def ld(dst, src, key, queue="sp"):
            P.dma(queue, I("dma_start", out=dst, in_=src), key, writes=[R(key)])

        ld(identb[:], c_ident[:, :], "identb", "pool")
        P.dma("pool", I("dma_start", out=strip[:, 0:1408], in_=c_strip[:, 0:1408]), "strip", writes=[R("strip")])
        P.dma("pool", I("dma_start", out=strip[:, 1408:STRIP_W], in_=c_strip[:, 1408:STRIP_W]), "strip2", writes=[R("strip2")])
        ld(p32[:], c_p32[:, :], "p32", "pool")
        ld(p64[:], c_p64[:, :], "p64", "pool")
        ld(wukT[:], wuk_d[:, :], "wukT", "pool")
        ld(wuv[:], wuv_d[:, :], "wuv", "pool")
        ld(tri[:], c_tri[:, :], "tri")
        ld(cvec[:], c_cvec[:, :], "cvec")
        ld(gall[:], gall_d[:, :], "gall")
        P.op("dve", I("memset", onesb[:], 1.0), writes=[R("onesb")])
        CONST_R = [R(k) for k in ("identb", "strip", "strip2", "p32", "p64", "wukT", "wuv", "tri", "cvec", "gall", "onesb")]
        P.barrier()

        out_toks = []

        def evac(i, dst, src, reads, writes):
            if i % 2 == 0:
                P.op("act", I("activation", out=dst, in_=src, func=AF.Copy), reads=reads, writes=writes)
            else:
                P.op("dve", I("tensor_copy", out=dst, in_=src), reads=reads, writes=writes)

        def norm_tile(src, src_res, gcol, i, junk, junk_r, xn, xn_r, tbank, dstT, dst_res, ssc, part=None):
            ss = stat[:, ssc:ssc + 1]
            rs = stat[:, ssc + 1:ssc + 2]
            ss_r, rs_r = R("stat", ssc), R("stat", ssc + 1)
            if part != "b":
                P.op("act", I("activation", out=junk, in_=src, func=AF.Square, accum_out=ss),
                     reads=[src_res], writes=[junk_r, ss_r])
                P.op("act", I("activation", out=rs, in_=ss, func=AF.Sqrt, scale=1.0 / D, bias=1e-6),
                     reads=[ss_r], writes=[rs_r])
                P.op("dve", I("reciprocal", out=rs, in_=rs), reads=[rs_r], writes=[rs_r])
                P.op("act", I("activation", out=xn, in_=src, func=AF.Copy, scale=rs),
                     reads=[src_res, rs_r], writes=[xn_r])
            if part == "a":
                return
            tb_ap, tb_r = tbank
            tv = bfv(tb_ap[:, :]).rearrange("p (a b) -> p a b", a=8)
            P.op("pe", [(I("transpose", out=tv[:, c, :], in_=xn[:, c * 128:(c + 1) * 128], identity=identb[:]))
                        for c in range(8)], reads=[xn_r, R("identb")], writes=[tb_r])
            P.op("dve", I("tensor_tensor", out=dstT, in0=tv,
                                                   in1=gall[:, gcol:gcol + 8].unsqueeze(2).to_broadcast([128, 8, 128]),
                                                   op=ALU.mult),
                 reads=[tb_r, R("gall")], writes=[dst_res])

        def load_w(dst, src_rows_cols, key, res):
            P.dma("pool", I("dma_start", out=dst, in_=src_rows_cols), key, writes=[res])

        def chk(tag):
            if stop == tag:
                raise _Stop()

        try:
          for b in range(nb):
              P.barrier()
              P.new_epoch({n: sem(f"s{b}_" + n) for n in ("pe", "act", "dve", "pool", "sp")}, R)
              cv = Carver()
              Kp = cv.take([128, 8, S], BF16)
              Vp = cv.take([128, 16, 768], BF16)
              kidx = cv.take([128, S], BF16)
              kbT = cv.take([128, 4, S], BF16)
              Vb = cv.take([128, 16, 768], BF16)
              keys_end = cv.off
              xs = [cv.take([128, D], F32) for _ in range(4)]
              junk = cv.take([128, D], BF16)
              xn = [cv.take([128, D], BF16) for _ in range(4)]
              cv.off = keys_end
              tabc = cv.take([128, S], F32)
              tabs_ = cv.take([128, S], F32)
              wsl = [cv.take([128, 8, 512], BF16) for _ in range(2)]
              qbuf = [cv.take([128, 512], BF16) for _ in range(2)]
              t1b = [cv.take([128, 512], F32) for _ in range(2)]
              t2b = [cv.take([128, 512], F32) for _ in range(2)]
              stg = [cv.take([128, 512], BF16) for _ in range(4)]
              ckf = cv.take([128, 512], F32)
              sqb = cv.take([128, 512], BF16)
              rtb = cv.take([128, 512], F32)
              ckvT = cv.take([128, S], BF16)

              for (T, nm) in ((Vp, "Vp"), (Vb, "Vb")):
                  tv4 = T.rearrange("p k (a c) -> p k a c", a=4)
                  P.op("pool", I("memset", tv4[:, :, :, 64:128], 1.0),
                       writes=[R(nm, kb) for kb in range(16)])
              for i in range(16):
                  P.dma("sp", I("dma_start", out=xs[i % 4][:, :], in_=x_d[b, i * 128:(i + 1) * 128, :]),
                        f"xs{i % 4}", writes=[R("xs", i % 4)])
                  norm_tile(xs[i % 4][:, :], R("xs", i % 4), 0, i, junk[:, :], R("junk"), xn[i % 4][:, :], R("xn", i % 4),
                            PB[7 - (i % 2)], HT[:, :, i * 128:(i + 1) * 128], R("HT", i // 4), 2 * (i % 4))
              P.barrier()
              chk("norm1")
              cur_tab = [None]
              mmb = Rot([PB[0], PB[1], PB[2]])
              ppb = Rot([PB[3], PB[4]])
              msb = Rot([PB[5], PB[6]])
              ecnt = [0]
              for si, (soff, sw, chunks) in enumerate(slabs):
                  if si > 0:
                      chk(f"slab{si - 1}")
                  ws, ws_r = wsl[si % 2], R("wsl", si % 2)

                  def _ldslab(sj):
                      so_, sw_, _ = slabs[sj]
                      load_w(wsl[sj % 2][:, :, 0:sw_], win_d[:, so_:so_ + sw_].rearrange("(c p) n -> p c n", p=128),
                             f"wsl{sj % 2}", R("wsl", sj % 2))
                  if si == 0:
                      _ldslab(0)
                  if si + 1 < len(slabs):
                      _ldslab(si + 1)
                  for ch in chunks:
                      kind, idx, coff, M = ch["kind"], ch["idx"], ch["coff"], ch["M"]
                      if kind in ("VB", "WI"):
                          for tb in range(16):
                              bk, bk_r = mmb.next()
                              P.op("pe", [(I("matmul",
                                  bk[:, 0:M], lhsT=HT[:, c, tb * 128:(tb + 1) * 128], rhs=ws[:, c, coff:coff + M],
                                  start=(c == 0), stop=(c == 7))) for c in range(8)],
                                  reads=[ws_r, R("HT", tb // 4)], writes=[bk_r])
                              if kind == "WI":
                                  P.op("act", I("activation",
                                      out=weff[:, tb, :], in_=bk[:, 0:8], func=AF.Copy, scale=float(8 ** -0.5 * 64 ** -0.5)),
                                      reads=[bk_r], writes=[R("weff", tb)])
                              else:
                                  src4 = bk[:, :].rearrange("p (a c) -> p a c", a=4)
                                  dst4 = Vb[:, tb, :].rearrange("p (a c) -> p a c", a=4)
                                  P.op("act", I("activation", out=dst4[:, :, 0:64], in_=src4[:, :, 0:64], func=AF.Copy),
                                       reads=[bk_r], writes=[R("Vb", tb)])
                                  P.op("dve", I("tensor_copy", out=dst4[:, :, 128:192], in_=src4[:, :, 64:128]),
                                       reads=[bk_r], writes=[R("Vb", tb)])
                          continue
                      for tt in range(4):
                          tsl = slice(tt * 512, (tt + 1) * 512)
                          bk, bk_r = mmb.next()
                          P.op("pe", [(I("matmul",
                              bk[0:M, :], lhsT=ws[:, c, coff:coff + M], rhs=HT[:, c, tsl],
                              start=(c == 0), stop=(c == 7))) for c in range(8)],
                              reads=[ws_r, R("HT", tt)], writes=[bk_r])
                          if dbg_step == 1:
                              raise _Stop()
                          if kind == "CKV":
                              P.op("act", I("activation", out=ckf[:, :], in_=bk[:, :], func=AF.Copy),
                                   reads=[bk_r], writes=[R("ckf")])
                              P.op("act", I("activation", out=sqb[:, :], in_=bk[:, :], func=AF.Square),
                                   reads=[bk_r], writes=[R("sqb")])
                              sm, sm_r = msb.next()
                              P.op("pe", I("matmul", sm[:, :], lhsT=onesb[:, :], rhs=sqb[:, :], start=True, stop=True),
                                   reads=[R("sqb"), R("onesb")], writes=[sm_r])
                              P.op("act", I("activation", out=rtb[:, :], in_=sm[:, :], func=AF.Sqrt,
                                                                          scale=1.0 / 128, bias=1e-6),
                                   reads=[sm_r], writes=[R("rtb")])
                              P.op("dve", I("reciprocal", out=rtb[:, :], in_=rtb[:, :]), reads=[R("rtb")], writes=[R("rtb")])
                              P.op("dve", I("scalar_tensor_tensor",
                                  out=ckvT[:, tsl], in0=ckf[:, :], scalar=gall[:, 32:33], in1=rtb[:, :],
                                  op0=ALU.mult, op1=ALU.mult),
                                  reads=[R("ckf"), R("rtb"), R("gall")], writes=[R("ckvT", tt)])
                              for hh in range(8):
                                  kb_, kb_r = msb.next()
                                  P.op("pe", I("matmul",
                                      kb_[0:64, :], lhsT=wukT[:, hh * 64:(hh + 1) * 64], rhs=ckvT[:, tsl], start=True, stop=True),
                                      reads=[R("ckvT", tt), R("wukT")], writes=[kb_r])
                                  ecnt[0] += 1
                                  evac(ecnt[0], Kp[0:64, hh, tsl], kb_[0:64, :], [kb_r], [R("Kp", hh, tt)])
                              for j in range(4):
                                  kb = tt * 4 + j
                                  vb_, vb_r = msb.next()
                                  P.op("pe", I("matmul",
                                      vb_[:, :], lhsT=ckvT[:, kb * 128:(kb + 1) * 128], rhs=wuv[:, :], start=True, stop=True),
                                      reads=[R("ckvT", tt), R("wuv")], writes=[vb_r])
                                  src4 = vb_[:, :].rearrange("p (a c) -> p a c", a=4)
                                  dst4 = Vp[:, kb, :].rearrange("p (a c) -> p a c", a=4)
                                  P.op("act", I("activation", out=dst4[:, :, 0:64], in_=src4[:, :, 0:64], func=AF.Copy),
                                       reads=[vb_r], writes=[R("Vp", kb)])
                                  P.op("dve", I("tensor_copy", out=dst4[:, :, 128:192], in_=src4[:, :, 64:128]),
                                       reads=[vb_r], writes=[R("Vp", kb)])
                              continue
                          t32 = kind in ("QA", "KR")
                          tk = "32" if t32 else "64"
                          if cur_tab[0] != tk:
                              cur_tab[0] = tk
                              P.dma("sp", I("dma_start", out=tabc[:, :], in_=c_tab["cos" + tk][:, :]), "tabc", writes=[R("tabc")])
                              P.dma("sp", I("dma_start", out=tabs_[:, :], in_=c_tab["sin" + tk][:, :]), "tabs", writes=[R("tabs")])
                          cosT, sinT = tabc, tabs_
                          cos_r, sin_r = R("tabc"), R("tabs")
                          pm, pm_r = (p32, R("p32")) if t32 else (p64, R("p64"))
                          u = ecnt[0] = ecnt[0] + 1
                          qb_, qb_r = qbuf[u % 2], R("qbuf", u % 2)
                          t1, t1_r = t1b[u % 2], R("t1b", u % 2)
                          t2, t2_r = t2b[u % 2], R("t2b", u % 2)
                          P.op("act", I("activation", out=qb_[0:M, :], in_=bk[0:M, :], func=AF.Copy),
                               reads=[bk_r], writes=[qb_r])
                          if dbg_step == 2:
                              raise _Stop()
                          pb_, pb_r = ppb.next()
                          P.op("pe", I("matmul",
                              pb_[0:M, :], lhsT=pm[0:M, 0:M], rhs=qb_[0:M, :], start=True, stop=True),
                              reads=[qb_r, pm_r], writes=[pb_r])
                          if dbg_step == 3:
                              raise _Stop()
                          import os as _os
                          _v = _os.environ.get("DBG_VAR", "")
                          if _v == "v1":
                              P.op("dve", I("tensor_copy", out=t1[0:M, :], in_=bk[0:M, :]), reads=[bk_r], writes=[t1_r])
                          elif _v == "v5":
                              P.op("dve", I("tensor_copy", out=t1[0:M, :], in_=bk[0:M, :]), reads=[bk_r, qb_r, pb_r], writes=[t1_r])
                          elif _v == "v2":
                              P.op("dve", I("tensor_tensor", out=t1[0:M, :], in0=bk[0:M, :], in1=t2[0:M, :], op=ALU.mult),
                                   reads=[bk_r], writes=[t1_r])
                          elif _v == "v3":
                              P.op("dve", I("tensor_copy", out=t1[0:M, :], in_=cosT[0:M, tsl]), reads=[cos_r], writes=[t1_r])
                          elif _v == "v4":
                              P.op("dve", I("tensor_tensor", out=t1[0:M, :], in0=bk[0:M, :], in1=cosT[0:M, tsl], op=ALU.mult),
                                   reads=[bk_r], writes=[t1_r])
                          else:
                              P.op("dve", I("tensor_tensor",
                                  out=t1[0:M, :], in0=bk[0:M, :], in1=cosT[0:M, tsl], op=ALU.mult),
                                  reads=[bk_r, cos_r], writes=[t1_r])
                          if dbg_step == 4:
                              raise _Stop()
                          P.op("dve", I("tensor_tensor",
                              out=t2[0:M, :], in0=pb_[0:M, :], in1=sinT[0:M, tsl], op=ALU.mult),
                              reads=[pb_r, sin_r], writes=[t2_r])
                          if dbg_step == 5:
                              raise _Stop()
                          if kind == "KR":
                              for hh in range(8):
                                  P.op("pool", I("tensor_tensor",
                                      out=Kp[64:96, hh, tsl], in0=t1[64:96, :], in1=t2[64:96, :], op=ALU.add),
                                      reads=[t1_r, t2_r], writes=[R("Kp2", hh, tt)])
                          elif kind == "KI":
                              P.op("pool", I("tensor_tensor",
                                  out=kidx[:, tsl], in0=t1[:, :], in1=t2[:, :], op=ALU.add),
                                  reads=[t1_r, t2_r], writes=[R("kidx", tt)])
                          elif kind == "KB":
                              P.op("pool", I("tensor_tensor",
                                  out=kbT[:, idx, tsl], in0=t1[:, :], in1=t2[:, :], op=ALU.add),
                                  reads=[t1_r, t2_r], writes=[R("kbT", idx, tt)])
                          else:
                              sg, sg_r = stg[u % 4], R("stg", u % 4)
                              P.op("pool", I("tensor_tensor",
                                  out=sg[0:M, :], in0=t1[0:M, :], in1=t2[0:M, :], op=ALU.add),
                                  reads=[t1_r, t2_r], writes=[sg_r])
                              if dbg_step == 6:
                                  raise _Stop()
                              dst = {"QA": qa_s, "QI": qi_s, "QB": qb_s}[kind]
                              P.dma("sp", I("dma_start",
                                  out=dst[idx, 0:M, tsl], in_=sg[0:M, :]), f"stg{u % 4}",
                                  reads=[sg_r], writes=[R("scr", kind, idx, tt)])
              P.barrier()

              chk("proj")
              cv = Carver()
              cv.off = keys_end
              qaq = cv.take([128, 4, 512], BF16)
              qiq = cv.take([128, 4, 512], BF16)
              qbq = cv.take([128, 4, 512], BF16)
              idxb = [cv.take([128, S], F32) for _ in range(2)]
              Rb = [cv.take([128, 512], BF16) for _ in range(3)]
              diag = cv.take([128, 8, 128], BF16)
              Mk = cv.take([128, S], BF16)
              maskT = cv.take([128, 16, 512], BF16)
              Eb = [cv.take([128, 512], BF16) for _ in range(3)]
              Pmb = [cv.take([128, 512], BF16) for _ in range(6)]
              dsh = [cv.take([128, 512], F32)] * 2
              bis = cv.take([128, 32], F32)
              steps = cv.take([128, BIS_ITERS], F32)

              Lb = Rot([PB[0], PB[1]])
              accb = PB[2]
              Tb = PB[2]
              Ob = Rot([PB[6], PB[7]])
              sc_a = float((64 + 32) ** -0.5)
              sc_b = 0.125
              ucount = [0]

              seqc = [0]
              inflight = []
              obank = {}
              Sbh = [None]
              Sb_small = Rot([PB[3], PB[4], PB[5]])
              Sb_big = Rot([PB[3], PB[4], PB[5], PB[0], PB[1]])

              def load_qaq(hf, qt_):
                  qs_ = slice(qt_ * 512, (qt_ + 1) * 512)
                  P.dma("sp", I("dma_start", out=qaq[0:96, :, :], in_=qa_s[4 * hf:4 * hf + 4, :, qs_].rearrange("a r t -> r a t")),
                        "qaq", reads=[R("scr", "QA", i, qt_) for i in range(8)], writes=[R("qaq")])

              def emitQK(un):
                  g, hh, kb, qt_, seq, meng = un
                  c0 = max(0, kb - 4 * qt_) * 128
                  sbk, sbk_r = Sbh[0].next()
                  pr = (hh % 2) * 64
                  if g == "A":
                      if hh == 4 and kb == 0:
                          load_qaq(1, qt_)
                      P.op("pe", I("matmul", sbk[:, c0:512], lhsT=Kp[0:96, hh, kb * 128:(kb + 1) * 128],
                                   rhs=qaq[0:96, hh % 4, c0:512], start=True, stop=True),
                           reads=[R("Kp", hh, kb // 4), R("Kp2", hh, kb // 4), R("qaq")], writes=[sbk_r])
                  else:
                      P.op("pe", I("matmul", sbk[:, c0:512], lhsT=kbT[pr:pr + 64, hh // 2, kb * 128:(kb + 1) * 128],
                                   rhs=qbq[pr:pr + 64, hh // 2, c0:512], start=True, stop=True),
                           reads=[R("kbT", hh // 2, kb // 4), R("qbq")], writes=[sbk_r])
                  e, e_r = Eb[seq % 3], R("Eb", seq % 3)
                  P.op("act", I("activation", out=e[:, c0:512], in_=sbk[:, c0:512], func=AF.Exp,
                                scale=(sc_a if g == "A" else sc_b)),
                       reads=[sbk_r], writes=[e_r])
                  pmt, pmt_r = Pmb[seq % 6], R("Pmb", seq % 6)
                  if g == "A":
                      msk, msk_r = maskT[:, kb, c0:512], R("maskT")
                  else:
                      o_ = 128 * (4 * qt_ - kb) + 384
                      msk, msk_r = strip[:, o_ + c0:o_ + 512], R("strip")
                  P.op(meng, I("tensor_tensor", out=pmt[:, c0:512], in0=e[:, c0:512], in1=msk, op=ALU.mult),
                       reads=[e_r, msk_r], writes=[pmt_r])

              def emitPV(un):
                  g, hh, kb, qt_, seq, meng = un
                  NK_ = 4 * qt_ + 4
                  qs_ = slice(qt_ * 512, (qt_ + 1) * 512)
                  c0 = max(0, kb - 4 * qt_) * 128
                  if kb == 0:
                      obank[(g, hh)] = Ob.next()
                  ob, ob_r = obank[(g, hh)]
                  pmt, pmt_r = Pmb[seq % 6], R("Pmb", seq % 6)
                  V = Vp if g == "A" else Vb
                  vo = (hh // 2) * 192 + (hh % 2) * 64
                  P.op("pe", I("matmul", ob[:, c0:512], lhsT=V[:, kb, vo:vo + 128], rhs=pmt[:, c0:512],
                               start=(kb == 0), stop=(kb == NK_ - 1)),
                       reads=[pmt_r, R("Vp" if g == "A" else "Vb", kb)], writes=[ob_r])
                  if kb == NK_ - 1:
                      ev = (hh % 2 == 0)
                      np_, dp_ = (slice(0, 64), slice(64, 128)) if ev else (slice(64, 128), slice(0, 64))
                      d, d_r = dsh[0], R("dsh", 0)
                      P.op("act", I("activation", out=d[np_, :], in_=ob[dp_, :], func=AF.Ln),
                           reads=[ob_r], writes=[d_r])
                      P.op("act", I("activation", out=d[np_, :], in_=d[np_, :], func=AF.Exp, scale=-1.0), reads=[d_r], writes=[d_r])
                      chunk = (hh // 2) + (0 if g == "A" else 4)
                      P.op("dve", I("tensor_tensor", out=HT[np_, chunk, qs_], in0=ob[np_, :], in1=d[np_, :], op=ALU.mult),
                           reads=[ob_r, d_r], writes=[R("HT", qt_)])

              def feed(g, hh, kb, qt_, meng, LA):
                  un = (g, hh, kb, qt_, seqc[0], meng)
                  seqc[0] += 1
                  emitQK(un)
                  inflight.append(un)
                  while len(inflight) > LA:
                      emitPV(inflight.pop(0))

              def flush():
                  while inflight:
                      emitPV(inflight.pop(0))

              for qt in range(4):
                  qsl = slice(qt * 512, (qt + 1) * 512)
                  NK = 4 * qt + 4
                  load_qaq(0, qt)
                  P.dma("sp", I("dma_start", out=qiq[:, :, :], in_=qi_s[:, :, qsl].rearrange("a r t -> r a t")),
                        "qiq", reads=[R("scr", "QI", i, qt) for i in range(4)], writes=[R("qiq")])
                  P.dma("sp", I("dma_start", out=qbq[:, :, :], in_=qb_s[:, :, qsl].rearrange("a r t -> r a t")),
                        "qbq", reads=[R("scr", "QB", i, qt) for i in range(4)], writes=[R("qbq")])
                  bunits = [(hh, kb) for hh in range(8) for kb in range(NK)]
                  per = len(bunits) // 4

                  def indexer(tb):
                      tbl = tb - 4 * qt
                      lo = tbl * 128
                      n = 128 * (tb + 1)
                      idx_sb, idx_r = idxb[tb % 2], R("idx", tb % 2)
                      for hh in range(8):
                          P.op("pool", I("tensor_scalar",
                              out=diag[:, hh, :], in0=identb[:, :], scalar1=weff[:, tb, hh:hh + 1], scalar2=1.0,
                              op0=ALU.mult, op1=ALU.mult),
                              reads=[R("identb"), R("weff", tb)], writes=[R("diag", hh)])
                      for st in range(qt + 1):
                          wd = min(512, n - 512 * st)
                          acc, acc_r = accb

                          def emitL(hh):
                              lb, lb_r = Lb.next()
                              pr = (hh % 2) * 64
                              P.op("pe", I("matmul",
                                  lb[:, 0:wd], lhsT=qiq[pr:pr + 64, hh // 2, lo:lo + 128],
                                  rhs=kidx[pr:pr + 64, st * 512:st * 512 + wd], start=True, stop=True),
                                  reads=[R("qiq"), R("kidx", st)], writes=[lb_r])
                              rb, rb_r = Rb[hh % 3], R("Rb", hh % 3)
                              P.op("act", I("activation", out=rb[:, 0:wd], in_=lb[:, 0:wd], func=AF.Relu),
                                   reads=[lb_r], writes=[rb_r])

                          def emitA(hh):
                              rb, rb_r = Rb[hh % 3], R("Rb", hh % 3)
                              P.op("pe", I("matmul",
                                  acc[:, 0:wd], lhsT=diag[:, hh, :], rhs=rb[:, 0:wd], start=(hh == 0), stop=(hh == 7)),
                                  reads=[rb_r, R("diag", hh)], writes=[acc_r])

                          emitL(0)
                          emitL(1)
                          for hh in range(8):
                              if hh + 2 < 8:
                                  emitL(hh + 2)
                              emitA(hh)
                          c0 = st * 512
                          P.op("act", I("activation", out=idx_sb[:, c0:c0 + wd], in_=acc[:, 0:wd], func=AF.Copy),
                               reads=[acc_r], writes=[idx_r])
                          if st == qt:
                              P.op("pool", I("tensor_tensor",
                                  out=idx_sb[:, c0 + wd - 128:c0 + wd], in0=idx_sb[:, c0 + wd - 128:c0 + wd], in1=tri[:, :],
                                  op=ALU.add), reads=[idx_r, R("tri")], writes=[idx_r])

                  def bisect(tb):
                      n = 128 * (tb + 1)
                      idx_sb, idx_r = idxb[tb % 2], R("idx", tb % 2)
                      thr = bis[:, 0:1]
                      if tb >= 2:
                          mx, mn, W, cnt, a_ = bis[:, 1:2], bis[:, 2:3], bis[:, 3:4], bis[:, 4:5], bis[:, 5:6]
                          RB = R("bis")
                          P.op("dve", I("tensor_reduce", out=mx, in_=idx_sb[:, 0:n], op=ALU.max, axis=AX.X),
                               reads=[idx_r], writes=[RB])
                          P.op("dve", I("tensor_reduce", out=mn, in_=idx_sb[:, 0:n - 128], op=ALU.min, axis=AX.X),
                               reads=[idx_r], writes=[RB])
                          P.op("dve", I("tensor_tensor", out=W, in0=mx, in1=mn, op=ALU.subtract), reads=[RB], writes=[RB])
                          P.op("dve", I("tensor_tensor", out=thr, in0=mx, in1=mn, op=ALU.add), reads=[RB], writes=[RB])
                          P.op("dve", I("tensor_scalar", out=thr, in0=thr, scalar1=0.5, scalar2=None, op0=ALU.mult),
                               reads=[RB], writes=[RB])
                          P.op("dve", I("tensor_scalar", out=steps[:, :], in0=cvec[:, :], scalar1=W, scalar2=None, op0=ALU.mult),
                               reads=[RB, R("cvec")], writes=[RB])
                          for it in range(BIS_ITERS):
                              P.op("dve", I("tensor_scalar",
                                  out=Mk[:, 0:n], in0=idx_sb[:, 0:n], scalar1=thr, scalar2=None,
                                  op0=ALU.is_ge, op1=ALU.add, accum_out=cnt),
                                  reads=[idx_r, RB], writes=[R("Mk"), RB])
                              P.op("dve", I("tensor_scalar", out=a_, in0=cnt, scalar1=255.5, scalar2=0.5,
                                            op0=ALU.is_ge, op1=ALU.subtract),
                                   reads=[RB], writes=[RB])
                              P.op("dve", I("scalar_tensor_tensor",
                                  out=thr, in0=a_, scalar=steps[:, it:it + 1], in1=thr, op0=ALU.mult, op1=ALU.add),
                                  reads=[RB], writes=[RB])
                          P.op("dve", I("tensor_tensor", out=thr, in0=thr, in1=steps[:, BIS_ITERS - 1:BIS_ITERS], op=ALU.subtract),
                               reads=[RB], writes=[RB])
                          P.op("dve", I("tensor_scalar", out=Mk[:, 0:n], in0=idx_sb[:, 0:n], scalar1=thr, scalar2=None,
                                        op0=ALU.is_ge),
                               reads=[idx_r, RB], writes=[R("Mk")])
                      else:
                          P.op("dve", I("tensor_scalar", out=Mk[:, 0:n], in0=idx_sb[:, 0:n], scalar1=-1.0e29, scalar2=None,
                                        op0=ALU.is_ge),
                               reads=[idx_r], writes=[R("Mk")])

                  def transposes(tb):
                      lo = (tb - 4 * qt) * 128
                      tbk, tbk_r = Tb
                      tv = bfv(tbk[:, :]).rearrange("p (a b) -> p a b", a=8)
                      for g0 in range(0, tb + 1, 8):
                          g1 = min(tb + 1, g0 + 8)
                          P.op("pe", [(I("transpose", out=tv[:, kb - g0, :], in_=Mk[:, kb * 128:(kb + 1) * 128],
                                         identity=identb[:, :])) for kb in range(g0, g1)],
                               reads=[R("Mk"), R("identb")], writes=[tbk_r])
                          ucount[0] += 1
                          evac(ucount[0], maskT[:, g0:g1, lo:lo + 128], tv[:, 0:g1 - g0, :], [tbk_r], [R("maskT")])

                  indexer(4 * qt)
                  for tbl in range(4):
                      tb = 4 * qt + tbl
                      bisect(tb)
                      if tbl < 3:
                          indexer(tb + 1)
                      Sbh[0] = Sb_small
                      for (hh, kb) in bunits[tbl * per:(tbl + 1) * per]:
                          feed("B", hh, kb, qt, "pool", 3)
                      transposes(tb)

                  if qt == 0:
                      chk("idx0")
                  Sbh[0] = Sb_big
                  for hh in range(8):
                      for kb in range(NK):
                          feed("A", hh, kb, qt, ("pool" if seqc[0] % 3 == 0 else "dve"), 5)
                  flush()
              P.barrier()

              chk("attn")
              cv = Carver()
              XR = cv.take([128, 16, D], F32)
              x_end = cv.off
              wA = [cv.take([128, 8, 512], BF16) for _ in range(2)]
              junk = cv.take([128, D], BF16)
              xn = [cv.take([128, D], BF16) for _ in range(4)]
              mmb = Rot([PB[0], PB[1], PB[2], PB[3]])
              for q4 in range(4):
                  P.dma("sp", I("dma_start",
                      out=XR[:, q4 * 4:(q4 + 1) * 4, :], in_=x_d[b, q4 * 512:(q4 + 1) * 512, :].rearrange("(n p) d -> p n d", p=128)),
                      f"xr{q4}", writes=[R("XR", t) for t in range(q4 * 4, q4 * 4 + 4)])

              def out_proj(w_d, srcT, src_res_fn, gcol):
                  for half in range(2):
                      load_w(wA[half][:, :, :], w_d[:, half * 512:(half + 1) * 512].rearrange("(c p) n -> p c n", p=128),
                             f"wA{half}", R("wA", half))
                  for tb in range(16):
                      for half in range(2):
                          ws, ws_r = wA[half], R("wA", half)
                          bk, bk_r = mmb.next()
                          P.op("pe", [(I("matmul",
                              bk[:, :], lhsT=srcT[:, c, tb * 128:(tb + 1) * 128], rhs=ws[:, c, :],
                              start=(c == 0), stop=(c == 7))) for c in range(8)],
                              reads=[ws_r, src_res_fn(tb)], writes=[bk_r])
                          P.op("dve", I("tensor_tensor",
                              out=XR[:, tb, half * 512:(half + 1) * 512], in0=bk[:, :], in1=XR[:, tb, half * 512:(half + 1) * 512],
                              op=ALU.add), reads=[bk_r, R("XR", tb)], writes=[R("XR", tb)])
                      def _nrm(t_, part):
                          norm_tile(XR[:, t_, :], R("XR", t_), gcol, t_, junk[:, :], R("junk"), xn[t_ % 4][:, :], R("xn", t_ % 4),
                                    PB[7 - (t_ % 2)], HT[:, :, t_ * 128:(t_ + 1) * 128], R("HTb", t_), 2 * (t_ % 4), part=part)
                      _nrm(tb, "a")
                      if tb >= 2:
                          _nrm(tb - 2, "b")
                  _nrm(14, "b")
                  _nrm(15, "b")

              out_proj(wout_d, HT, lambda tb: R("HTb", tb), 8)
              P.barrier()

              chk("wout")
              cv = Carver()
              cv.off = x_end
              OC = cv.take([128, 8, S], BF16)
              wA = [cv.take([128, 8, 512], BF16) for _ in range(2)]
              junk = cv.take([128, D], BF16)
              xn = [cv.take([128, D], BF16) for _ in range(4)]
              ms = [cv.take([128, D], F32)] * 2
              mT = cv.take([128, 8, 256], BF16)
              KcT = cv.take([128, 8, 256], BF16)
              Vc = cv.take([128, 2, D], BF16)
              qcT = cv.take([128, 8, 512], BF16)
              Ec = [cv.take([128, 512], BF16) for _ in range(4)]
              rdn = [cv.take([128, 512], F32)] * 2
              for i in range(2):
                  P.dma("sp", I("dma_start", out=ms[i][:, :], in_=mem_d[b, i * 128:(i + 1) * 128, :]), "ms0",
                        writes=[R("ms", 0)])
                  norm_tile(ms[i][:, :], R("ms", 0), 16, i, junk[:, :], R("junk"), xn[i % 4][:, :], R("xn", i % 4),
                            PB[7 - (i % 2)], mT[:, :, i * 128:(i + 1) * 128], R("mT"), 2 * (i % 4))
              mmb = Rot([PB[0], PB[1], PB[2]])
              ec = 0
              for half in range(2):
                  ws, ws_r = wA[half], R("wA", half)
                  load_w(ws[:, :, :], wkv_d[:, half * 512:(half + 1) * 512].rearrange("(c p) n -> p c n", p=128), f"wA{half}", ws_r)
                  for j4 in range(4):
                      j = half * 4 + j4
                      bk, bk_r = mmb.next()
                      P.op("pe", [(I("matmul",
                          bk[:, 0:256], lhsT=ws[:, c, j4 * 128:(j4 + 1) * 128], rhs=mT[:, c, :], start=(c == 0), stop=(c == 7)))
                          for c in range(8)], reads=[ws_r, R("mT")], writes=[bk_r])
                      ec += 1
                      evac(ec, KcT[:, j, :], bk[:, 0:256], [bk_r], [R("KcT", j)])
              for half in range(2):
                  ws, ws_r = wA[half], R("wA", half)
                  load_w(ws[:, :, :], wkv_d[:, D + half * 512:D + (half + 1) * 512].rearrange("(c p) n -> p c n", p=128),
                         f"wA{half}", ws_r)
                  for mc in range(2):
                      bk, bk_r = mmb.next()
                      P.op("pe", [(I("matmul",
                          bk[:, :], lhsT=mT[:, c, mc * 128:(mc + 1) * 128], rhs=ws[:, c, :], start=(c == 0), stop=(c == 7)))
                          for c in range(8)], reads=[ws_r, R("mT")], writes=[bk_r])
                      ec += 1
                      evac(ec, Vc[:, mc, half * 512:(half + 1) * 512], bk[:, :], [bk_r], [R("Vc", mc)])
              for half in range(2):
                  load_w(wA[half][:, :, :], wq_d[:, half * 512:(half + 1) * 512].rearrange("(c p) n -> p c n", p=128),
                         f"wA{half}", R("wA", half))
              sbk2 = Rot([PB[3], PB[4]])
              obk = Rot([PB[5], PB[6]])
              dbk = PB[7]
              for tt in range(4):
                  tsl = slice(tt * 512, (tt + 1) * 512)
                  for j in range(8):
                      bk, bk_r = mmb.next()
                      P.op("pe", [(I("matmul",
                          bk[:, :], lhsT=wA[j // 4][:, c, (j % 4) * 128:(j % 4 + 1) * 128], rhs=HT[:, c, tsl],
                          start=(c == 0), stop=(c == 7))) for c in range(8)],
                          reads=[R("wA", j // 4), R("HT", tt)], writes=[bk_r])
                      ec += 1
                      evac(ec, qcT[:, j, :], bk[:, :], [bk_r], [R("qcT", j)])
                  for hh in range(4):
                      for mc in range(2):
                          sk, sk_r = sbk2.next()
                          P.op("pe", [(I("matmul",
                              sk[:, :], lhsT=KcT[:, 2 * hh + jj, mc * 128:(mc + 1) * 128], rhs=qcT[:, 2 * hh + jj, :],
                              start=(jj == 0), stop=(jj == 1))) for jj in range(2)],
                              reads=[R("KcT", 2 * hh), R("KcT", 2 * hh + 1), R("qcT", 2 * hh), R("qcT", 2 * hh + 1)], writes=[sk_r])
                          e, e_r = Ec[(hh % 2) * 2 + mc], R("Ec", (hh % 2) * 2 + mc)
                          P.op("act", I("activation", out=e[:, :], in_=sk[:, :], func=AF.Exp, scale=1.0 / 16),
                               reads=[sk_r], writes=[e_r])
                      e0, e1 = Ec[(hh % 2) * 2], Ec[(hh % 2) * 2 + 1]
                      er = [R("Ec", (hh % 2) * 2), R("Ec", (hh % 2) * 2 + 1)]
                      db, db_r = dbk
                      P.op("pe", [(I("matmul", db[:, :], lhsT=onesb[:, :], rhs=e[:, :],
                                                                  start=(mc == 0), stop=(mc == 1))) for mc, e in ((0, e0), (1, e1))],
                           reads=er + [R("onesb")], writes=[db_r])
                      rd, rd_r = rdn[0], R("rdn", 0)
                      P.op("dve", I("reciprocal", out=rd[:, :], in_=db[:, :]), reads=[db_r], writes=[rd_r])
                      for jj in range(2):
                          ob, ob_r = obk.next()
                          P.op("pe", [(I("matmul",
                              ob[:, :], lhsT=Vc[:, mc, (2 * hh + jj) * 128:(2 * hh + jj + 1) * 128], rhs=e[:, :],
                              start=(mc == 0), stop=(mc == 1))) for mc, e in ((0, e0), (1, e1))],
                              reads=er + [R("Vc", 0), R("Vc", 1)], writes=[ob_r])
                          P.op("dve", I("tensor_tensor",
                              out=OC[:, 2 * hh + jj, tsl], in0=ob[:, :], in1=rd[:, :], op=ALU.mult),
                              reads=[ob_r, rd_r], writes=[R("OC", tt)])
              mmb = Rot([PB[0], PB[1], PB[2], PB[3]])
              P.barrier()

              out_proj(wo_d, OC, lambda tb: R("OC", tb // 4), 24)
              P.barrier()

              chk("cross")
              cv = Carver()
              cv.off = x_end
              junk = cv.take([128, D], BF16)
              xn = [cv.take([128, D], BF16) for _ in range(4)]
              wU = [cv.take([128, 8, 512], BF16) for _ in range(2)]
              wD = [cv.take([128, 4, D], BF16) for _ in range(2)]
              hid = [cv.take([128, 4, 512], BF16) for _ in range(2)]
              rl = [cv.take([128, 512], BF16) for _ in range(2)]
              gF = cv.take([128, D], F32)
              ot = [cv.take([128, D], F32) for _ in range(2)]
              P.dma("sp", I("dma_start", out=gF[:, :], in_=gF_d[:, :]), "gF", writes=[R("gF")])
              def final_norm(i):
                  src = XR[:, i, :]
                  ssc = 8 + 2 * (i % 4)
                  ss, rs = stat[:, ssc:ssc + 1], stat[:, ssc + 1:ssc + 2]
                  ss_r, rs_r = R("stat", ssc), R("stat", ssc + 1)
                  o_, o_r = ot[i % 2], R("ot", i % 2)
                  P.op("act", I("activation", out=junk[:, :], in_=src, func=AF.Square, accum_out=ss),
                       reads=[R("XR", i)], writes=[R("junk"), ss_r])
                  P.op("act", I("activation", out=rs, in_=ss, func=AF.Sqrt, scale=1.0 / D, bias=1e-6),
                       reads=[ss_r], writes=[rs_r])
                  P.op("dve", I("reciprocal", out=rs, in_=rs), reads=[rs_r], writes=[rs_r])
                  P.op("dve", I("scalar_tensor_tensor",
                      out=o_[:, :], in0=src, scalar=rs, in1=gF[:, :], op0=ALU.mult, op1=ALU.mult),
                      reads=[R("XR", i), rs_r, R("gF")], writes=[o_r])
                  out_toks.append(P.dma("sp", I("dma_start", out=out_d[b, i * 128:(i + 1) * 128, :], in_=o_[:, :]),
                                        f"ot{i % 2}", reads=[o_r]))

              upb = Rot([PB[0], PB[1], PB[2]])
              dnb = Rot([PB[3], PB[4], PB[5], PB[6]])
              cnt = 0
              for fg in range(8):
                  wu, wu_r = wU[fg % 2], R("wU", fg % 2)
                  wd_, wd_r = wD[fg % 2], R("wD", fg % 2)
                  def _ldmlp(fj):
                      load_w(wU[fj % 2][:, :, :], wup_d[:, fj * 512:(fj + 1) * 512].rearrange("(c p) n -> p c n", p=128),
                             f"wU{fj % 2}", R("wU", fj % 2))
                      load_w(wD[fj % 2][:, :, :], wdn_d[fj * 512:(fj + 1) * 512, :].rearrange("(c p) n -> p c n", p=128),
                             f"wD{fj % 2}", R("wD", fj % 2))
                  if fg == 0:
                      _ldmlp(0)
                  if fg + 1 < 8:
                      _ldmlp(fg + 1)
                  for tt in range(4):
                      tsl = slice(tt * 512, (tt + 1) * 512)
                      cnt += 1
                      hd, hd_r = hid[cnt % 2], R("hid", cnt % 2)
                      for fc in range(4):
                          bk, bk_r = upb.next()
                          P.op("pe", [(I("matmul",
                              bk[:, :], lhsT=wu[:, c, fc * 128:(fc + 1) * 128], rhs=HT[:, c, tsl],
                              start=(c == 0), stop=(c == 7))) for c in range(8)],
                              reads=[wu_r, R("HT", tt)], writes=[bk_r])
                          r_, r_r = rl[fc % 2], R("rl", fc % 2)
                          P.op("act", I("activation", out=r_[:, :], in_=bk[:, :], func=AF.Relu),
                               reads=[bk_r], writes=[r_r])
                          P.op("pool", I("tensor_tensor", out=hd[:, fc, :], in0=r_[:, :], in1=r_[:, :],
                                                                                         op=ALU.mult),
                               reads=[r_r], writes=[hd_r])
                      for t4 in range(4):
                          tb = tt * 4 + t4
                          for half in range(2):
                              bk, bk_r = dnb.next()
                              P.op("pe", [(I("matmul",
                                  bk[:, :], lhsT=hd[:, fc, t4 * 128:(t4 + 1) * 128], rhs=wd_[:, fc, half * 512:(half + 1) * 512],
                                  start=(fc == 0), stop=(fc == 3))) for fc in range(4)],
                                  reads=[hd_r, wd_r], writes=[bk_r])
                              P.op("dve", I("tensor_tensor",
                                  out=XR[:, tb, half * 512:(half + 1) * 512], in0=bk[:, :], in1=XR[:, tb, half * 512:(half + 1) * 512],
                                  op=ALU.add), reads=[bk_r, R("XR", tb)], writes=[R("XR", tb)])
                          if fg == 7:
                              final_norm(tb)
              P.barrier()

        except _Stop:
            P.barrier()
        P.wait_tokens("sp", out_toks)
        with nc.Block() as block:
            P.emit(block)
    nc._mk_stats = (P.nops, P.nwaits)
    return nc


_NC_CACHE = {}


def kernel(x, mem, norm_mix_g, w_in, kv_norm_g, w_uk, w_uv, w_out, norm_cross_g, norm_mem_g,
           w_q_cross, w_kv_cross, w_o_cross, norm_mlp_g, w_up, w_down, norm_final_g):
    f = lambda a: np.ascontiguousarray(np.asarray(a, dtype=np.float32))
    x, mem = f(x), f(mem)
    win_cols, _ = _win_plan()
    win = f(f(w_in)[0][:, win_cols])
    wukT = f(np.transpose(f(w_uk)[0], (2, 0, 1)).reshape(128, 512))
    wuv = f(np.transpose(f(w_uv)[0], (1, 0, 2)).reshape(128, 512))
    gcol = lambda g: f(g).reshape(8, 128).T
    gall = f(np.concatenate([gcol(norm_mix_g), gcol(norm_cross_g), gcol(norm_mem_g), gcol(norm_mlp_g),
                             f(kv_norm_g).reshape(128, 1)], axis=1))
    gF = f(np.broadcast_to(f(norm_final_g).reshape(1, D), (128, D)))
    consts = _consts()
    if "nc" not in _NC_CACHE:
        _NC_CACHE["nc"] = build_nc(NB)
    nc = _NC_CACHE["nc"]
    shared = dict(win=win, wukT=wukT, wuv=wuv, wout=f(w_out)[0], wq=f(w_q_cross)[0], wkv=f(w_kv_cross)[0],
                  wo=f(w_o_cross)[0], wup=f(w_up)[0], wdn=f(w_down)[0], gall=gall, gF=gF, **consts)
    in_maps = []
    for c in range(NCORES):
        m = dict(shared)
        m["x"] = x[c * NB:(c + 1) * NB]
        m["mem"] = mem[c * NB:(c + 1) * NB]
        in_maps.append(m)
    res = run_bass_kernel_spmd(nc, in_maps, core_ids=list(range(NCORES)))
    return np.concatenate([res.results[c]["out"] for c in range(NCORES)], axis=0).astype(np.float32)
```

```python
import math
from contextlib import ExitStack
import numpy as np
import concourse.bass as bass
import concourse.mybir as mybir
from concourse.bass_utils import run_bass_kernel_spmd

F32 = mybir.dt.float32
BF16 = mybir.dt.bfloat16
AF = mybir.ActivationFunctionType
ALU = mybir.AluOpType
AX = mybir.AxisListType

NCORES = 8
NB = 4
S = 2048
D = 1024
BIS_ITERS = 18
NEG = -1.0e30
STRIP_W = 2816


class Res:
    __slots__ = ("name", "writer", "readers", "excl")

    def __init__(self, name):
        self.name = name
        self.writer = None
        self.readers = []
        self.excl = (name[0] == "psum")


class ResReg:
    def __init__(self):
        self.d = {}

    def __call__(self, *key):
        r = self.d.get(key)
        if r is None:
            r = self.d[key] = Res(key)
        return r


class Eng:
    def __init__(self, name, sem, is_pe=False):
        self.name, self.sem, self.is_pe = name, sem, is_pe
        self.count = 0
        self.ops = []
        self.waited = {}


class Prog:
    def __init__(self, nc, sems, dma_sem_pool):
        self.nc = nc
        self.E = {n: Eng(n, sems[n], is_pe=(n == "pe")) for n in ("pe", "act", "dve", "pool", "sp")}
        self.dma_sems = {}
        self.sem_pool = dma_sem_pool
        self.nwaits = 0
        self.nops = 0

    def _deps(self, reads, writes):
        deps = []
        for r in reads:
            if r.writer is not None:
                deps.append(r.writer)
            if r.excl:
                deps.extend(r.readers)
        for w in writes:
            if w.writer is not None:
                deps.append(w.writer)
            deps.extend(w.readers)
        return deps

    def _waits(self, eng, deps):
        need = {}
        for d in deps:
            key = (d[0], d[1])
            if need.get(key, 0) < d[2]:
                need[key] = d[2]
        waits = []
        for key, v in need.items():
            if key[0] == "e" and key[1] == eng.name and eng.is_pe:
                continue
            if eng.waited.get(key, 0) >= v:
                continue
            eng.waited[key] = v
            if key[0] == "e":
                waits.append((self.E[key[1]].sem, v))
            else:
                waits.append((self.dma_sems[key[1]][0], v * 16))
        self.nwaits += len(waits)
        return waits

    def _commit(self, tok, reads, writes):
        for r in reads:
            if r.excl:
                r.writer = tok
                r.readers = []
            else:
                r.readers.append(tok)
        for w in writes:
            w.writer = tok
            w.readers = []

    def op(self, engname, fns, reads=(), writes=()):
        eng = self.E[engname]
        if isinstance(fns, tuple):
            fns = [fns]
        waits = self._waits(eng, self._deps(reads, writes))
        eng.count += 1
        tok = ("e", engname, eng.count)
        eng.ops.append((waits, fns, ("e", eng.sem)))
        self._commit(tok, reads, writes)
        self.nops += len(fns)
        return tok

    def dma(self, queue, fn, semkey, reads=(), writes=()):
        eng = self.E[queue]
        if semkey not in self.dma_sems:
            self.dma_sems[semkey] = [self.sem_pool.pop(), 0]
        ent = self.dma_sems[semkey]
        deps = self._deps(reads, writes)
        if ent[1] > 0:
            deps.append(("d", semkey, ent[1]))
        waits = self._waits(eng, deps)
        ent[1] += 1
        tok = ("d", semkey, ent[1])
        eng.ops.append((waits, [fn], ("d", ent[0])))
        self._commit(tok, reads, writes)
        self.nops += 1
        return tok

    def wait_tokens(self, engname, toks):
        eng = self.E[engname]
        waits = self._waits(eng, list(toks))
        if waits:
            eng.ops.append((waits, [], None))

    def barrier(self):
        toks = []
        for e in self.E.values():
            if e.count > 0:
                toks.append(("e", e.name, e.count))
        for k, (s, c) in self.dma_sems.items():
            if c > 0:
                toks.append(("d", k, c))
        for e in self.E.values():
            self.wait_tokens(e.name, [t for t in toks if not (t[0] == "e" and t[1] == e.name and e.is_pe)])

    def new_epoch(self, sems, reg):
        for e in self.E.values():
            e.sem = sems[e.name]
            e.count = 0
            e.waited = {k: v for k, v in e.waited.items() if k[0] == "d"}
        for r in reg.d.values():
            if r.writer is not None and r.writer[0] == "e":
                r.writer = None
            r.readers = [t for t in r.readers if t[0] != "e"]

    def emit(self, block):
        def run(eng, h):
            for waits, fns, inc in eng.ops:
                for sem, v in waits:
                    h.wait_ge(sem, v)
                ins = None
                for f in fns:
                    ins = getattr(h, f[0])(*f[1], **f[2])
                if inc is not None and ins is not None:
                    ins.then_inc(inc[1], 1 if inc[0] == "e" else 16)

        block.tensor(lambda h: run(self.E["pe"], h))
        block.scalar(lambda h: run(self.E["act"], h))
        block.vector(lambda h: run(self.E["dve"], h))
        block.gpsimd(lambda h: run(self.E["pool"], h))
        block.sync(lambda h: run(self.E["sp"], h))


def I(name, *a, **k):
    return (name, a, k)


class Rot:
    def __init__(self, items):
        self.items, self.i = items, 0

    def next(self):
        it = self.items[self.i % len(self.items)]
        self.i += 1
        return it


O_QN, O_QR, O_CKV, O_KR, O_QI, O_KI, O_WI, O_QKV = 0, 512, 768, 896, 928, 1440, 1504, 1512


def _win_plan():
    cols = []
    slabs = []

    def slab(chunks_cols):
        off = len(cols)
        chunks = []
        for kind, idx, cc in chunks_cols:
            chunks.append(dict(kind=kind, idx=idx, coff=len(cols) - off, M=len(cc)))
            cols.extend(cc)
        slabs.append((off, len(cols) - off, chunks))

    def qa(h):
        return list(range(O_QN + 64 * h, O_QN + 64 * h + 64)) + list(range(O_QR + 32 * h, O_QR + 32 * h + 32))

    r = lambda a, n: list(range(a, a + n))
    slab([("QA", h, qa(h)) for h in range(0, 4)])
    slab([("QA", h, qa(h)) for h in range(4, 8)] + [("KR", 0, r(O_KI, 64) + r(O_KR, 32))])
    slab([("QI", j, r(O_QI + 128 * j, 128)) for j in range(4)])
    slab([("QB", j, r(O_QKV + 128 * j, 128)) for j in range(4)])
    slab([("KB", j, r(O_QKV + 512 + 128 * j, 128)) for j in range(4)])
    slab([("KI", 0, r(O_KI, 64) + r(O_KI, 64)), ("CKV", 0, r(O_CKV, 128))])
    slab([("VB", 0, r(O_QKV + 1024, 512))])
    slab([("WI", 0, r(O_WI, 8))])
    return np.array(cols, dtype=np.int64), slabs


def _consts():
    c = {}
    c["ident"] = np.eye(128, dtype=np.float32)
    tri = np.zeros((128, 128), np.float32)
    tri[np.triu_indices(128, 1)] = NEG
    c["tri"] = tri
    i = np.arange(128)[:, None]
    cc = np.arange(STRIP_W)[None, :]
    dl = cc - i - 384
    m = ((dl >= 0) & (dl <= 128)).astype(np.float32) + ((dl >= 0) & (dl % 4 == 0) & (dl <= 512)) + \
        ((dl >= 0) & (dl % 16 == 0) & (dl <= 2048))
    c["strip"] = m.astype(np.float32)
    p32 = np.zeros((128, 128), np.float32)
    for k in range(16):
        p32[64 + k, 80 + k] = 1
        p32[80 + k, 64 + k] = 1
    p64 = np.zeros((128, 128), np.float32)
    for hb in (0, 64):
        for k in range(32):
            p64[hb + k, hb + 32 + k] = 1
            p64[hb + 32 + k, hb + k] = 1
    c["p32"], c["p64"] = p32, p64
    c["cvec"] = np.tile((2.0 ** -(np.arange(BIS_ITERS) + 1.0))[None, :], (128, 1)).astype(np.float32)
    pos = np.arange(S, dtype=np.float32)[None, :]
    inv32 = (10000.0 ** (-np.arange(0, 32, 2, dtype=np.float32) / 32)).astype(np.float32)
    inv64 = (10000.0 ** (-np.arange(0, 64, 2, dtype=np.float32) / 64)).astype(np.float32)
    cos32 = np.ones((128, S), np.float32)
    sin32 = np.zeros((128, S), np.float32)
    a32 = inv32[:, None] * pos
    cos32[64:80], cos32[80:96] = np.cos(a32), np.cos(a32)
    sin32[64:80], sin32[80:96] = -np.sin(a32), np.sin(a32)
    a64 = inv64[:, None] * pos
    cos64 = np.zeros((128, S), np.float32)
    sin64 = np.zeros((128, S), np.float32)
    for hb in (0, 64):
        cos64[hb:hb + 32], cos64[hb + 32:hb + 64] = np.cos(a64), np.cos(a64)
        sin64[hb:hb + 32], sin64[hb + 32:hb + 64] = -np.sin(a64), np.sin(a64)
    c["cos32"], c["sin32"], c["cos64"], c["sin64"] = cos32, sin32, cos64, sin64
    return c


class _Stop(Exception):
    pass


def build_nc(nb=NB, stop=None, dbg_step=0):
    nc = bass.Bass("TRN2", target_bir_lowering=False)
    win_cols, slabs = _win_plan()
    NCOL = len(win_cols)

    def din(name, shape, dt=F32):
        return nc.dram_tensor(name, list(shape), dt, kind="ExternalInput").ap()

    x_d = din("x", [nb, S, D])
    mem_d = din("mem", [nb, 256, D])
    win_d = din("win", [D, NCOL])
    wuk_d = din("wukT", [128, 512])
    wuv_d = din("wuv", [128, 512])
    wout_d = din("wout", [D, D])
    wq_d = din("wq", [D, D])
    wkv_d = din("wkv", [D, 2 * D])
    wo_d = din("wo", [D, D])
    wup_d = din("wup", [D, 4 * D])
    wdn_d = din("wdn", [4 * D, D])
    gall_d = din("gall", [128, 33])
    gF_d = din("gF", [128, D])
    c_ident = din("ident", [128, 128])
    c_tri = din("tri", [128, 128])
    c_strip = din("strip", [128, STRIP_W])
    c_p32 = din("p32", [128, 128])
    c_p64 = din("p64", [128, 128])
    c_cvec = din("cvec", [128, BIS_ITERS])
    c_tab = {k: din(k, [128, S]) for k in ("cos32", "sin32", "cos64", "sin64")}
    out_d = nc.dram_tensor("out", [nb, S, D], F32, kind="ExternalOutput").ap()
    qa_s = nc.dram_tensor("qa_s", [8, 96, S], BF16, kind="ExternalOutput").ap()
    qi_s = nc.dram_tensor("qi_s", [4, 128, S], BF16, kind="ExternalOutput").ap()
    qb_s = nc.dram_tensor("qb_s", [4, 128, S], BF16, kind="ExternalOutput").ap()

    with ExitStack() as es:
        def sb(name, shape, dt):
            return es.enter_context(nc.sbuf_tensor("sb_" + name, list(shape), dt))

        def sem(name):
            return es.enter_context(nc.semaphore(name))

        sems = {n: sem("s_" + n) for n in ("pe", "act", "dve", "pool", "sp")}
        P = Prog(nc, sems, [sem(f"dq{i}") for i in range(40)])
        R = ResReg()

        HT = sb("HT", [128, 8, S], BF16)
        identb = sb("identb", [128, 128], BF16)
        onesb = sb("onesb", [128, 128], BF16)
        tri = sb("tri", [128, 128], F32)
        strip = sb("strip", [128, STRIP_W], BF16)
        p32 = sb("p32", [128, 128], BF16)
        p64 = sb("p64", [128, 128], BF16)
        cvec = sb("cvec", [128, BIS_ITERS], F32)
        gall = sb("gall", [128, 33], F32)
        wukT = sb("wukT", [128, 512], BF16)
        wuv = sb("wuv", [128, 512], BF16)
        weff = sb("weff", [128, 16, 8], F32)
        stat = sb("stat", [128, 64], F32)
        ARENA_B = 165 * 1024
        arena = sb("arena", [128, ARENA_B // 2], BF16)
        psum = [es.enter_context(nc.psum_tensor(f"ps{i}", [128, 512], F32)) for i in range(8)]
        PB = [(psum[i], R("psum", i)) for i in range(8)]

        class Carver:
            def __init__(self):
                self.off = 0

            def take(self, shape, dt):
                esz = 4 if dt == F32 else 2
                n = int(np.prod(shape[1:]))
                nbytes = n * esz
                nbytes_al = (nbytes + 63) // 64 * 64
                a = arena[:, self.off // 2: (self.off + nbytes) // 2]
                self.off += nbytes_al
                assert self.off <= ARENA_B, ("arena overflow", self.off)
                if dt == F32:
                    a = a.bitcast(F32)
                if len(shape) == 3:
                    a = a.rearrange("p (a b) -> p a b", a=shape[1])
                elif len(shape) == 4:
                    a = a.rearrange("p (a b c) -> p a b c", a=shape[1], b=shape[2])
                return a

        def bfv(ps_ap):
            return ps_ap.bitcast(BF16)

        def ld(dst, src, key, queue="sp"):
            P.dma(queue, I("dma_start", out=dst, in_=src), key, writes=[R(key)])

        ld(identb[:], c_ident[:, :], "identb", "pool")
        P.dma("pool", I("dma_start", out=strip[:, 0:1408], in_=c_strip[:, 0:1408]), "strip", writes=[R("strip")])
        P.dma("pool", I("dma_start", out=strip[:, 1408:STRIP_W], in_=c_strip[:, 1408:STRIP_W]), "strip2", writes=[R("strip2")])
        ld(p32[:], c_p32[:, :], "p32", "pool")
        ld(p64[:], c_p64[:, :], "p64", "pool")
        ld(wukT[:], wuk_d[:, :], "wukT", "pool")
        ld(wuv[:], wuv_d[:, :], "wuv", "pool")
        ld(tri[:], c_tri[:, :], "tri")
        ld(cvec[:], c_cvec[:, :], "cvec")
        ld(gall[:], gall_d[:, :], "gall")
        P.op("dve", I("memset", onesb[:], 1.0), writes=[R("onesb")])
        CONST_R = [R(k) for k in ("identb", "strip", "strip2", "p32", "p64", "wukT", "wuv", "tri", "cvec", "gall", "onesb")]
        P.barrier()

        out_toks = []

        def evac(i, dst, src, reads, writes):
            if i % 2 == 0:
                P.op("act", I("activation", out=dst, in_=src, func=AF.Copy), reads=reads, writes=writes)
            else:
                P.op("dve", I("tensor_copy", out=dst, in_=src), reads=reads, writes=writes)

        def norm_tile(src, src_res, gcol, i, junk, junk_r, xn, xn_r, tbank, dstT, dst_res, ssc, part=None):
            ss = stat[:, ssc:ssc + 1]
            rs = stat[:, ssc + 1:ssc + 2]
            ss_r, rs_r = R("stat", ssc), R("stat", ssc + 1)
            if part != "b":
                P.op("act", I("activation", out=junk, in_=src, func=AF.Square, accum_out=ss),
                     reads=[src_res], writes=[junk_r, ss_r])
                P.op("act", I("activation", out=rs, in_=ss, func=AF.Sqrt, scale=1.0 / D, bias=1e-6),
                     reads=[ss_r], writes=[rs_r])
                P.op("dve", I("reciprocal", out=rs, in_=rs), reads=[rs_r], writes=[rs_r])
                P.op("act", I("activation", out=xn, in_=src, func=AF.Copy, scale=rs),
                     reads=[src_res, rs_r], writes=[xn_r])
            if part == "a":
                return
            tb_ap, tb_r = tbank
            tv = bfv(tb_ap[:, :]).rearrange("p (a b) -> p a b", a=8)
            P.op("pe", [(I("transpose", out=tv[:, c, :], in_=xn[:, c * 128:(c + 1) * 128], identity=identb[:]))
                        for c in range(8)], reads=[xn_r, R("identb")], writes=[tb_r])
            P.op("dve", I("tensor_tensor", out=dstT, in0=tv,
                                                   in1=gall[:, gcol:gcol + 8].unsqueeze(2).to_broadcast([128, 8, 128]),
                                                   op=ALU.mult),
                 reads=[tb_r, R("gall")], writes=[dst_res])

        def load_w(dst, src_rows_cols, key, res):
            P.dma("pool", I("dma_start", out=dst, in_=src_rows_cols), key, writes=[res])

        def chk(tag):
            if stop == tag:
                raise _Stop()

        try:
          for b in range(nb):
              P.barrier()
              P.new_epoch({n: sem(f"s{b}_" + n) for n in ("pe", "act", "dve", "pool", "sp")}, R)
              cv = Carver()
              Kp = cv.take([128, 8, S], BF16)
              Vp = cv.take([128, 16, 768], BF16)
              kidx = cv.take([128, S], BF16)
              kbT = cv.take([128, 4, S], BF16)
              Vb = cv.take([128, 16, 768], BF16)
              keys_end = cv.off
              xs = [cv.take([128, D], F32) for _ in range(4)]
              junk = cv.take([128, D], BF16)
              xn = [cv.take([128, D], BF16) for _ in range(4)]
              cv.off = keys_end
              tabc = cv.take([128, S], F32)
              tabs_ = cv.take([128, S], F32)
              wsl = [cv.take([128, 8, 512], BF16) for _ in range(2)]
              qbuf = [cv.take([128, 512], BF16) for _ in range(2)]
              t1b = [cv.take([128, 512], F32) for _ in range(2)]
              t2b = [cv.take([128, 512], F32) for _ in range(2)]
              stg = [cv.take([128, 512], BF16) for _ in range(4)]
              ckf = cv.take([128, 512], F32)
              sqb = cv.take([128, 512], BF16)
              rtb = cv.take([128, 512], F32)
              ckvT = cv.take([128, S], BF16)

              for (T, nm) in ((Vp, "Vp"), (Vb, "Vb")):
                  tv4 = T.rearrange("p k (a c) -> p k a c", a=4)
                  P.op("pool", I("memset", tv4[:, :, :, 64:128], 1.0),
                       writes=[R(nm, kb) for kb in range(16)])
              for i in range(16):
                  P.dma("sp", I("dma_start", out=xs[i % 4][:, :], in_=x_d[b, i * 128:(i + 1) * 128, :]),
                        f"xs{i % 4}", writes=[R("xs", i % 4)])
                  norm_tile(xs[i % 4][:, :], R("xs", i % 4), 0, i, junk[:, :], R("junk"), xn[i % 4][:, :], R("xn", i % 4),
                            PB[7 - (i % 2)], HT[:, :, i * 128:(i + 1) * 128], R("HT", i // 4), 2 * (i % 4))
              P.barrier()
              chk("norm1")
              cur_tab = [None]
              mmb = Rot([PB[0], PB[1], PB[2], PB[7]])
              ppb = Rot([PB[3], PB[4]])
              msb = Rot([PB[5], PB[6]])
              ecnt = [0]
              for si, (soff, sw, chunks) in enumerate(slabs):
                  if si > 0:
                      chk(f"slab{si - 1}")
                  ws, ws_r = wsl[si % 2], R("wsl", si % 2)

                  def _ldslab(sj):
                      so_, sw_, _ = slabs[sj]
                      load_w(wsl[sj % 2][:, :, 0:sw_], win_d[:, so_:so_ + sw_].rearrange("(c p) n -> p c n", p=128),
                             f"wsl{sj % 2}", R("wsl", sj % 2))
                  if si == 0:
                      _ldslab(0)
                  if si + 1 < len(slabs):
                      _ldslab(si + 1)
                  for ch in chunks:
                      kind, idx, coff, M = ch["kind"], ch["idx"], ch["coff"], ch["M"]
                      if kind in ("VB", "WI"):
                          for tb in range(16):
                              bk, bk_r = mmb.next()
                              P.op("pe", [(I("matmul",
                                  bk[:, 0:M], lhsT=HT[:, c, tb * 128:(tb + 1) * 128], rhs=ws[:, c, coff:coff + M],
                                  start=(c == 0), stop=(c == 7))) for c in range(8)],
                                  reads=[ws_r, R("HT", tb // 4)], writes=[bk_r])
                              if kind == "WI":
                                  P.op("act", I("activation",
                                      out=weff[:, tb, :], in_=bk[:, 0:8], func=AF.Copy, scale=float(8 ** -0.5 * 64 ** -0.5)),
                                      reads=[bk_r], writes=[R("weff", tb)])
                              else:
                                  src4 = bk[:, :].rearrange("p (a c) -> p a c", a=4)
                                  dst4 = Vb[:, tb, :].rearrange("p (a c) -> p a c", a=4)
                                  P.op("act", I("activation", out=dst4[:, :, 0:64], in_=src4[:, :, 0:64], func=AF.Copy),
                                       reads=[bk_r], writes=[R("Vb", tb)])
                                  P.op("dve", I("tensor_copy", out=dst4[:, :, 128:192], in_=src4[:, :, 64:128]),
                                       reads=[bk_r], writes=[R("Vb", tb)])
                          continue
                      for tt in range(4):
                          tsl = slice(tt * 512, (tt + 1) * 512)
                          bk, bk_r = mmb.next()
                          P.op("pe", [(I("matmul",
                              bk[0:M, :], lhsT=ws[:, c, coff:coff + M], rhs=HT[:, c, tsl],
                              start=(c == 0), stop=(c == 7))) for c in range(8)],
                              reads=[ws_r, R("HT", tt)], writes=[bk_r])
                          if dbg_step == 1:
                              raise _Stop()
                          if kind == "CKV":
                              P.op("act", I("activation", out=ckf[:, :], in_=bk[:, :], func=AF.Copy),
                                   reads=[bk_r], writes=[R("ckf")])
                              P.op("act", I("activation", out=sqb[:, :], in_=bk[:, :], func=AF.Square),
                                   reads=[bk_r], writes=[R("sqb")])
                              sm, sm_r = msb.next()
                              P.op("pe", I("matmul", sm[:, :], lhsT=onesb[:, :], rhs=sqb[:, :], start=True, stop=True),
                                   reads=[R("sqb"), R("onesb")], writes=[sm_r])
                              P.op("act", I("activation", out=rtb[:, :], in_=sm[:, :], func=AF.Sqrt,
                                                                          scale=1.0 / 128, bias=1e-6),
                                   reads=[sm_r], writes=[R("rtb")])
                              P.op("dve", I("reciprocal", out=rtb[:, :], in_=rtb[:, :]), reads=[R("rtb")], writes=[R("rtb")])
                              P.op("dve", I("scalar_tensor_tensor",
                                  out=ckvT[:, tsl], in0=ckf[:, :], scalar=gall[:, 32:33], in1=rtb[:, :],
                                  op0=ALU.mult, op1=ALU.mult),
                                  reads=[R("ckf"), R("rtb"), R("gall")], writes=[R("ckvT", tt)])
                              for hh in range(8):
                                  kb_, kb_r = msb.next()
                                  P.op("pe", I("matmul",
                                      kb_[0:64, :], lhsT=wukT[:, hh * 64:(hh + 1) * 64], rhs=ckvT[:, tsl], start=True, stop=True),
                                      reads=[R("ckvT", tt), R("wukT")], writes=[kb_r])
                                  ecnt[0] += 1
                                  evac(ecnt[0], Kp[0:64, hh, tsl], kb_[0:64, :], [kb_r], [R("Kp", hh, tt)])
                              for j in range(4):
                                  kb = tt * 4 + j
                                  vb_, vb_r = msb.next()
                                  P.op("pe", I("matmul",
                                      vb_[:, :], lhsT=ckvT[:, kb * 128:(kb + 1) * 128], rhs=wuv[:, :], start=True, stop=True),
                                      reads=[R("ckvT", tt), R("wuv")], writes=[vb_r])
                                  src4 = vb_[:, :].rearrange("p (a c) -> p a c", a=4)
                                  dst4 = Vp[:, kb, :].rearrange("p (a c) -> p a c", a=4)
                                  P.op("act", I("activation", out=dst4[:, :, 0:64], in_=src4[:, :, 0:64], func=AF.Copy),
                                       reads=[vb_r], writes=[R("Vp", kb)])
                                  P.op("dve", I("tensor_copy", out=dst4[:, :, 128:192], in_=src4[:, :, 64:128]),
                                       reads=[vb_r], writes=[R("Vp", kb)])
                              continue
                          t32 = kind in ("QA", "KR")
                          tk = "32" if t32 else "64"
                          if cur_tab[0] != tk:
                              cur_tab[0] = tk
                              P.dma("sp", I("dma_start", out=tabc[:, :], in_=c_tab["cos" + tk][:, :]), "tabc", writes=[R("tabc")])
                              P.dma("sp", I("dma_start", out=tabs_[:, :], in_=c_tab["sin" + tk][:, :]), "tabs", writes=[R("tabs")])
                          cosT, sinT = tabc, tabs_
                          cos_r, sin_r = R("tabc"), R("tabs")
                          pm, pm_r = (p32, R("p32")) if t32 else (p64, R("p64"))
                          u = ecnt[0] = ecnt[0] + 1
                          qb_, qb_r = qbuf[u % 2], R("qbuf", u % 2)
                          t1, t1_r = t1b[u % 2], R("t1b", u % 2)
                          t2, t2_r = t2b[u % 2], R("t2b", u % 2)
                          P.op("act", I("activation", out=qb_[0:M, :], in_=bk[0:M, :], func=AF.Copy),
                               reads=[bk_r], writes=[qb_r])
                          if dbg_step == 2:
                              raise _Stop()
                          pb_, pb_r = ppb.next()
                          P.op("pe", I("matmul",
                              pb_[0:M, :], lhsT=pm[0:M, 0:M], rhs=qb_[0:M, :], start=True, stop=True),
                              reads=[qb_r, pm_r], writes=[pb_r])
                          if dbg_step == 3:
                              raise _Stop()
                          import os as _os
                          _v = _os.environ.get("DBG_VAR", "")
                          if _v == "v1":
                              P.op("dve", I("tensor_copy", out=t1[0:M, :], in_=bk[0:M, :]), reads=[bk_r], writes=[t1_r])
                          elif _v == "v5":
                              P.op("dve", I("tensor_copy", out=t1[0:M, :], in_=bk[0:M, :]), reads=[bk_r, qb_r, pb_r], writes=[t1_r])
                          elif _v == "v2":
                              P.op("dve", I("tensor_tensor", out=t1[0:M, :], in0=bk[0:M, :], in1=t2[0:M, :], op=ALU.mult),
                                   reads=[bk_r], writes=[t1_r])
                          elif _v == "v3":
                              P.op("dve", I("tensor_copy", out=t1[0:M, :], in_=cosT[0:M, tsl]), reads=[cos_r], writes=[t1_r])
                          elif _v == "v4":
                              P.op("dve", I("tensor_tensor", out=t1[0:M, :], in0=bk[0:M, :], in1=cosT[0:M, tsl], op=ALU.mult),
                                   reads=[bk_r], writes=[t1_r])
                          else:
                              P.op("dve", I("tensor_tensor",
                                  out=t1[0:M, :], in0=bk[0:M, :], in1=cosT[0:M, tsl], op=ALU.mult),
                                  reads=[bk_r, cos_r], writes=[t1_r])
                          if dbg_step == 4:
                              raise _Stop()
                          P.op("dve", I("tensor_tensor",
                              out=t2[0:M, :], in0=pb_[0:M, :], in1=sinT[0:M, tsl], op=ALU.mult),
                              reads=[pb_r, sin_r], writes=[t2_r])
                          if dbg_step == 5:
                              raise _Stop()
                          if kind == "KR":
                              for hh in range(8):
                                  P.op("pool", I("tensor_tensor",
                                      out=Kp[64:96, hh, tsl], in0=t1[64:96, :], in1=t2[64:96, :], op=ALU.add),
                                      reads=[t1_r, t2_r], writes=[R("Kp2", hh, tt)])
                          elif kind == "KI":
                              P.op("pool", I("tensor_tensor",
                                  out=kidx[:, tsl], in0=t1[:, :], in1=t2[:, :], op=ALU.add),
                                  reads=[t1_r, t2_r], writes=[R("kidx", tt)])
                          elif kind == "KB":
                              P.op("pool", I("tensor_tensor",
                                  out=kbT[:, idx, tsl], in0=t1[:, :], in1=t2[:, :], op=ALU.add),
                                  reads=[t1_r, t2_r], writes=[R("kbT", idx, tt)])
                          else:
                              sg, sg_r = stg[u % 4], R("stg", u % 4)
                              P.op("pool", I("tensor_tensor",
                                  out=sg[0:M, :], in0=t1[0:M, :], in1=t2[0:M, :], op=ALU.add),
                                  reads=[t1_r, t2_r], writes=[sg_r])
                              if dbg_step == 6:
                                  raise _Stop()
                              dst = {"QA": qa_s, "QI": qi_s, "QB": qb_s}[kind]
                              P.dma("sp", I("dma_start",
                                  out=dst[idx, 0:M, tsl], in_=sg[0:M, :]), f"stg{u % 4}",
                                  reads=[sg_r], writes=[R("scr", kind, idx, tt)])
              P.barrier()

              chk("proj")
              cv = Carver()
              cv.off = keys_end
              qaq = cv.take([128, 4, 512], BF16)
              qiq = cv.take([128, 4, 512], BF16)
              qbq = cv.take([128, 4, 512], BF16)
              idxb = [cv.take([128, S], F32) for _ in range(2)]
              Rb = [cv.take([128, 512], BF16) for _ in range(3)]
              diag = cv.take([128, 8, 128], BF16)
              Mk = cv.take([128, S], BF16)
              maskT = cv.take([128, 16, 512], BF16)
              Eb = [cv.take([128, 512], BF16) for _ in range(3)]
              Pmb = [cv.take([128, 512], BF16) for _ in range(6)]
              dsh = [cv.take([128, 512], F32)] * 2
              bis = cv.take([128, 32], F32)
              steps = cv.take([128, BIS_ITERS], F32)

              Lb = Rot([PB[0], PB[1]])
              accb = PB[2]
              Tb = PB[2]
              Ob = Rot([PB[6], PB[7]])
              sc_a = float((64 + 32) ** -0.5)
              sc_b = 0.125
              ucount = [0]

              seqc = [0]
              inflight = []
              obank = {}
              Sbh = [None]
              Sb_small = Rot([PB[3], PB[4], PB[5]])
              Sb_big = Rot([PB[3], PB[4], PB[5], PB[0], PB[1]])

              def load_qaq(hf, qt_):
                  qs_ = slice(qt_ * 512, (qt_ + 1) * 512)
                  P.dma("sp", I("dma_start", out=qaq[0:96, :, :], in_=qa_s[4 * hf:4 * hf + 4, :, qs_].rearrange("a r t -> r a t")),
                        "qaq", reads=[R("scr", "QA", i, qt_) for i in range(8)], writes=[R("qaq")])

              def emitQK(un):
                  g, hh, kb, qt_, seq, meng = un
                  c0 = max(0, kb - 4 * qt_) * 128
                  sbk, sbk_r = Sbh[0].next()
                  pr = (hh % 2) * 64
                  if g == "A":
                      if hh == 4 and kb == 0:
                          load_qaq(1, qt_)
                      P.op("pe", I("matmul", sbk[:, c0:512], lhsT=Kp[0:96, hh, kb * 128:(kb + 1) * 128],
                                   rhs=qaq[0:96, hh % 4, c0:512], start=True, stop=True),
                           reads=[R("Kp", hh, kb // 4), R("Kp2", hh, kb // 4), R("qaq")], writes=[sbk_r])
                  else:
                      P.op("pe", I("matmul", sbk[:, c0:512], lhsT=kbT[pr:pr + 64, hh // 2, kb * 128:(kb + 1) * 128],
                                   rhs=qbq[pr:pr + 64, hh // 2, c0:512], start=True, stop=True),
                           reads=[R("kbT", hh // 2, kb // 4), R("qbq")], writes=[sbk_r])
                  e, e_r = Eb[seq % 3], R("Eb", seq % 3)
                  P.op("act", I("activation", out=e[:, c0:512], in_=sbk[:, c0:512], func=AF.Exp,
                                scale=(sc_a if g == "A" else sc_b)),
                       reads=[sbk_r], writes=[e_r])
                  pmt, pmt_r = Pmb[seq % 6], R("Pmb", seq % 6)
                  if g == "A":
                      msk, msk_r = maskT[:, kb, c0:512], R("maskT")
                  else:
                      o_ = 128 * (4 * qt_ - kb) + 384
                      msk, msk_r = strip[:, o_ + c0:o_ + 512], R("strip")
                  P.op(meng, I("tensor_tensor", out=pmt[:, c0:512], in0=e[:, c0:512], in1=msk, op=ALU.mult),
                       reads=[e_r, msk_r], writes=[pmt_r])

              def emitPV(un):
                  g, hh, kb, qt_, seq, meng = un
                  NK_ = 4 * qt_ + 4
                  qs_ = slice(qt_ * 512, (qt_ + 1) * 512)
                  c0 = max(0, kb - 4 * qt_) * 128
                  if kb == 0:
                      obank[(g, hh)] = Ob.next()
                  ob, ob_r = obank[(g, hh)]
                  pmt, pmt_r = Pmb[seq % 6], R("Pmb", seq % 6)
                  V = Vp if g == "A" else Vb
                  vo = (hh // 2) * 192 + (hh % 2) * 64
                  P.op("pe", I("matmul", ob[:, c0:512], lhsT=V[:, kb, vo:vo + 128], rhs=pmt[:, c0:512],
                               start=(kb == 0), stop=(kb == NK_ - 1)),
                       reads=[pmt_r, R("Vp" if g == "A" else "Vb", kb)], writes=[ob_r])
                  if kb == NK_ - 1:
                      ev = (hh % 2 == 0)
                      np_, dp_ = (slice(0, 64), slice(64, 128)) if ev else (slice(64, 128), slice(0, 64))
                      d, d_r = dsh[0], R("dsh", 0)
                      P.op("act", I("activation", out=d[np_, :], in_=ob[dp_, :], func=AF.Ln),
                           reads=[ob_r], writes=[d_r])
                      P.op("act", I("activation", out=d[np_, :], in_=d[np_, :], func=AF.Exp, scale=-1.0), reads=[d_r], writes=[d_r])
                      chunk = (hh // 2) + (0 if g == "A" else 4)
                      P.op("dve", I("tensor_tensor", out=HT[np_, chunk, qs_], in0=ob[np_, :], in1=d[np_, :], op=ALU.mult),
                           reads=[ob_r, d_r], writes=[R("HT", qt_)])

              def feed(g, hh, kb, qt_, meng, LA):
                  un = (g, hh, kb, qt_, seqc[0], meng)
                  seqc[0] += 1
                  emitQK(un)
                  inflight.append(un)
                  while len(inflight) > LA:
                      emitPV(inflight.pop(0))

              def flush():
                  while inflight:
                      emitPV(inflight.pop(0))

              for qt in range(4):
                  qsl = slice(qt * 512, (qt + 1) * 512)
                  NK = 4 * qt + 4
                  load_qaq(0, qt)
                  P.dma("sp", I("dma_start", out=qiq[:, :, :], in_=qi_s[:, :, qsl].rearrange("a r t -> r a t")),
                        "qiq", reads=[R("scr", "QI", i, qt) for i in range(4)], writes=[R("qiq")])
                  P.dma("sp", I("dma_start", out=qbq[:, :, :], in_=qb_s[:, :, qsl].rearrange("a r t -> r a t")),
                        "qbq", reads=[R("scr", "QB", i, qt) for i in range(4)], writes=[R("qbq")])
                  bunits = [(hh, kb) for hh in range(8) for kb in range(NK)]
                  per = len(bunits) // 4

                  def indexer(tb):
                      tbl = tb - 4 * qt
                      lo = tbl * 128
                      n = 128 * (tb + 1)
                      idx_sb, idx_r = idxb[tb % 2], R("idx", tb % 2)
                      for hh in range(8):
                          P.op("pool", I("tensor_scalar",
                              out=diag[:, hh, :], in0=identb[:, :], scalar1=weff[:, tb, hh:hh + 1], scalar2=1.0,
                              op0=ALU.mult, op1=ALU.mult),
                              reads=[R("identb"), R("weff", tb)], writes=[R("diag", hh)])
                      for st in range(qt + 1):
                          wd = min(512, n - 512 * st)
                          acc, acc_r = accb

                          def emitL(hh):
                              lb, lb_r = Lb.next()
                              pr = (hh % 2) * 64
                              P.op("pe", I("matmul",
                                  lb[:, 0:wd], lhsT=qiq[pr:pr + 64, hh // 2, lo:lo + 128],
                                  rhs=kidx[pr:pr + 64, st * 512:st * 512 + wd], start=True, stop=True),
                                  reads=[R("qiq"), R("kidx", st)], writes=[lb_r])
                              rb, rb_r = Rb[hh % 3], R("Rb", hh % 3)
                              P.op("act", I("activation", out=rb[:, 0:wd], in_=lb[:, 0:wd], func=AF.Relu),
                                   reads=[lb_r], writes=[rb_r])

                          def emitA(hh):
                              rb, rb_r = Rb[hh % 3], R("Rb", hh % 3)
                              P.op("pe", I("matmul",
                                  acc[:, 0:wd], lhsT=diag[:, hh, :], rhs=rb[:, 0:wd], start=(hh == 0), stop=(hh == 7)),
                                  reads=[rb_r, R("diag", hh)], writes=[acc_r])

                          emitL(0)
                          emitL(1)
                          for hh in range(8):
                              if hh + 2 < 8:
                                  emitL(hh + 2)
                              emitA(hh)
                          c0 = st * 512
                          P.op("act", I("activation", out=idx_sb[:, c0:c0 + wd], in_=acc[:, 0:wd], func=AF.Copy),
                               reads=[acc_r], writes=[idx_r])
                          if st == qt:
                              P.op("pool", I("tensor_tensor",
                                  out=idx_sb[:, c0 + wd - 128:c0 + wd], in0=idx_sb[:, c0 + wd - 128:c0 + wd], in1=tri[:, :],
                                  op=ALU.add), reads=[idx_r, R("tri")], writes=[idx_r])

                  def bisect(tb):
                      n = 128 * (tb + 1)
                      idx_sb, idx_r = idxb[tb % 2], R("idx", tb % 2)
                      thr = bis[:, 0:1]
                      if tb >= 2:
                          mx, mn, W, cnt, a_ = bis[:, 1:2], bis[:, 2:3], bis[:, 3:4], bis[:, 4:5], bis[:, 5:6]
                          RB = R("bis")
                          P.op("dve", I("tensor_reduce", out=mx, in_=idx_sb[:, 0:n], op=ALU.max, axis=AX.X),
                               reads=[idx_r], writes=[RB])
                          P.op("dve", I("tensor_reduce", out=mn, in_=idx_sb[:, 0:n - 128], op=ALU.min, axis=AX.X),
                               reads=[idx_r], writes=[RB])
                          P.op("dve", I("tensor_tensor", out=W, in0=mx, in1=mn, op=ALU.subtract), reads=[RB], writes=[RB])
                          P.op("dve", I("tensor_tensor", out=thr, in0=mx, in1=mn, op=ALU.add), reads=[RB], writes=[RB])
                          P.op("dve", I("tensor_scalar", out=thr, in0=thr, scalar1=0.5, scalar2=None, op0=ALU.mult),
                               reads=[RB], writes=[RB])
                          P.op("dve", I("tensor_scalar", out=steps[:, :], in0=cvec[:, :], scalar1=W, scalar2=None, op0=ALU.mult),
                               reads=[RB, R("cvec")], writes=[RB])
                          for it in range(BIS_ITERS):
                              P.op("dve", I("tensor_scalar",
                                  out=Mk[:, 0:n], in0=idx_sb[:, 0:n], scalar1=thr, scalar2=None,
                                  op0=ALU.is_ge, op1=ALU.add, accum_out=cnt),
                                  reads=[idx_r, RB], writes=[R("Mk"), RB])
                              P.op("dve", I("tensor_scalar", out=a_, in0=cnt, scalar1=255.5, scalar2=0.5,
                                            op0=ALU.is_ge, op1=ALU.subtract),
                                   reads=[RB], writes=[RB])
                              P.op("dve", I("scalar_tensor_tensor",
                                  out=thr, in0=a_, scalar=steps[:, it:it + 1], in1=thr, op0=ALU.mult, op1=ALU.add),
                                  reads=[RB], writes=[RB])
                          P.op("dve", I("tensor_tensor", out=thr, in0=thr, in1=steps[:, BIS_ITERS - 1:BIS_ITERS], op=ALU.subtract),
                               reads=[RB], writes=[RB])
                          P.op("dve", I("tensor_scalar", out=Mk[:, 0:n], in0=idx_sb[:, 0:n], scalar1=thr, scalar2=None,
                                        op0=ALU.is_ge),
                               reads=[idx_r, RB], writes=[R("Mk")])
                      else:
                          P.op("dve", I("tensor_scalar", out=Mk[:, 0:n], in0=idx_sb[:, 0:n], scalar1=-1.0e29, scalar2=None,
                                        op0=ALU.is_ge),
                               reads=[idx_r], writes=[R("Mk")])

                  def transposes(tb):
                      lo = (tb - 4 * qt) * 128
                      tbk, tbk_r = Tb
                      tv = bfv(tbk[:, :]).rearrange("p (a b) -> p a b", a=8)
                      for g0 in range(0, tb + 1, 8):
                          g1 = min(tb + 1, g0 + 8)
                          P.op("pe", [(I("transpose", out=tv[:, kb - g0, :], in_=Mk[:, kb * 128:(kb + 1) * 128],
                                         identity=identb[:, :])) for kb in range(g0, g1)],
                               reads=[R("Mk"), R("identb")], writes=[tbk_r])
                          ucount[0] += 1
                          evac(ucount[0], maskT[:, g0:g1, lo:lo + 128], tv[:, 0:g1 - g0, :], [tbk_r], [R("maskT")])

                  indexer(4 * qt)
                  for tbl in range(4):
                      tb = 4 * qt + tbl
                      bisect(tb)
                      if tbl < 3:
                          indexer(tb + 1)
                      Sbh[0] = Sb_small
                      for (hh, kb) in bunits[tbl * per:(tbl + 1) * per]:
                          feed("B", hh, kb, qt, "pool", 3)
                      transposes(tb)

                  if qt == 0:
                      chk("idx0")
                  Sbh[0] = Sb_big
                  for hh in range(8):
                      for kb in range(NK):
                          feed("A", hh, kb, qt, ("pool" if seqc[0] % 5 == 0 else "dve"), 5)
                  flush()
              P.barrier()

              chk("attn")
              cv = Carver()
              XR = cv.take([128, 16, D], F32)
              x_end = cv.off
              wA = [cv.take([128, 8, 512], BF16) for _ in range(2)]
              junk = cv.take([128, D], BF16)
              xn = [cv.take([128, D], BF16) for _ in range(4)]
              mmb = Rot([PB[0], PB[1], PB[2], PB[3]])
              for q4 in range(4):
                  P.dma("sp", I("dma_start",
                      out=XR[:, q4 * 4:(q4 + 1) * 4, :], in_=x_d[b, q4 * 512:(q4 + 1) * 512, :].rearrange("(n p) d -> p n d", p=128)),
                      f"xr{q4}", writes=[R("XR", t) for t in range(q4 * 4, q4 * 4 + 4)])

              def out_proj(w_d, srcT, src_res_fn, gcol):
                  for half in range(2):
                      load_w(wA[half][:, :, :], w_d[:, half * 512:(half + 1) * 512].rearrange("(c p) n -> p c n", p=128),
                             f"wA{half}", R("wA", half))
                  for tb in range(16):
                      for half in range(2):
                          ws, ws_r = wA[half], R("wA", half)
                          bk, bk_r = mmb.next()
                          P.op("pe", [(I("matmul",
                              bk[:, :], lhsT=srcT[:, c, tb * 128:(tb + 1) * 128], rhs=ws[:, c, :],
                              start=(c == 0), stop=(c == 7))) for c in range(8)],
                              reads=[ws_r, src_res_fn(tb)], writes=[bk_r])
                          P.op("dve", I("tensor_tensor",
                              out=XR[:, tb, half * 512:(half + 1) * 512], in0=bk[:, :], in1=XR[:, tb, half * 512:(half + 1) * 512],
                              op=ALU.add), reads=[bk_r, R("XR", tb)], writes=[R("XR", tb)])
                      def _nrm(t_, part):
                          norm_tile(XR[:, t_, :], R("XR", t_), gcol, t_, junk[:, :], R("junk"), xn[t_ % 4][:, :], R("xn", t_ % 4),
                                    PB[7 - (t_ % 2)], HT[:, :, t_ * 128:(t_ + 1) * 128], R("HTb", t_), 2 * (t_ % 4), part=part)
                      _nrm(tb, "a")
                      if tb >= 2:
                          _nrm(tb - 2, "b")
                  _nrm(14, "b")
                  _nrm(15, "b")

              out_proj(wout_d, HT, lambda tb: R("HTb", tb), 8)
              P.barrier()

              chk("wout")
              cv = Carver()
              cv.off = x_end
              OC = cv.take([128, 8, S], BF16)
              wA = [cv.take([128, 8, 512], BF16) for _ in range(2)]
              junk = cv.take([128, D], BF16)
              xn = [cv.take([128, D], BF16) for _ in range(4)]
              ms = [cv.take([128, D], F32)] * 2
              mT = cv.take([128, 8, 256], BF16)
              KcT = cv.take([128, 8, 256], BF16)
              Vc = cv.take([128, 2, D], BF16)
              qcT = cv.take([128, 8, 512], BF16)
              Ec = [cv.take([128, 512], BF16) for _ in range(4)]
              rdn = [cv.take([128, 512], F32)] * 2
              for i in range(2):
                  P.dma("sp", I("dma_start", out=ms[i][:, :], in_=mem_d[b, i * 128:(i + 1) * 128, :]), "ms0",
                        writes=[R("ms", 0)])
                  norm_tile(ms[i][:, :], R("ms", 0), 16, i, junk[:, :], R("junk"), xn[i % 4][:, :], R("xn", i % 4),
                            PB[7 - (i % 2)], mT[:, :, i * 128:(i + 1) * 128], R("mT"), 2 * (i % 4))
              mmb = Rot([PB[0], PB[1], PB[2]])
              ec = 0
              for half in range(2):
                  ws, ws_r = wA[half], R("wA", half)
                  load_w(ws[:, :, :], wkv_d[:, half * 512:(half + 1) * 512].rearrange("(c p) n -> p c n", p=128), f"wA{half}", ws_r)
                  for j4 in range(4):
                      j = half * 4 + j4
                      bk, bk_r = mmb.next()
                      P.op("pe", [(I("matmul",
                          bk[:, 0:256], lhsT=ws[:, c, j4 * 128:(j4 + 1) * 128], rhs=mT[:, c, :], start=(c == 0), stop=(c == 7)))
                          for c in range(8)], reads=[ws_r, R("mT")], writes=[bk_r])
                      ec += 1
                      evac(ec, KcT[:, j, :], bk[:, 0:256], [bk_r], [R("KcT", j)])
              for half in range(2):
                  ws, ws_r = wA[half], R("wA", half)
                  load_w(ws[:, :, :], wkv_d[:, D + half * 512:D + (half + 1) * 512].rearrange("(c p) n -> p c n", p=128),
                         f"wA{half}", ws_r)
                  for mc in range(2):
                      bk, bk_r = mmb.next()
                      P.op("pe", [(I("matmul",
                          bk[:, :], lhsT=mT[:, c, mc * 128:(mc + 1) * 128], rhs=ws[:, c, :], start=(c == 0), stop=(c == 7)))
                          for c in range(8)], reads=[ws_r, R("mT")], writes=[bk_r])
                      ec += 1
                      evac(ec, Vc[:, mc, half * 512:(half + 1) * 512], bk[:, :], [bk_r], [R("Vc", mc)])
              for half in range(2):
                  load_w(wA[half][:, :, :], wq_d[:, half * 512:(half + 1) * 512].rearrange("(c p) n -> p c n", p=128),
                         f"wA{half}", R("wA", half))
              sbk2 = Rot([PB[3], PB[4]])
              obk = Rot([PB[5], PB[6]])
              dbk = PB[7]
              for tt in range(4):
                  tsl = slice(tt * 512, (tt + 1) * 512)
                  for j in range(8):
                      bk, bk_r = mmb.next()
                      P.op("pe", [(I("matmul",
                          bk[:, :], lhsT=wA[j // 4][:, c, (j % 4) * 128:(j % 4 + 1) * 128], rhs=HT[:, c, tsl],
                          start=(c == 0), stop=(c == 7))) for c in range(8)],
                          reads=[R("wA", j // 4), R("HT", tt)], writes=[bk_r])
                      ec += 1
                      evac(ec, qcT[:, j, :], bk[:, :], [bk_r], [R("qcT", j)])
                  for hh in range(4):
                      for mc in range(2):
                          sk, sk_r = sbk2.next()
                          P.op("pe", [(I("matmul",
                              sk[:, :], lhsT=KcT[:, 2 * hh + jj, mc * 128:(mc + 1) * 128], rhs=qcT[:, 2 * hh + jj, :],
                              start=(jj == 0), stop=(jj == 1))) for jj in range(2)],
                              reads=[R("KcT", 2 * hh), R("KcT", 2 * hh + 1), R("qcT", 2 * hh), R("qcT", 2 * hh + 1)], writes=[sk_r])
                          e, e_r = Ec[(hh % 2) * 2 + mc], R("Ec", (hh % 2) * 2 + mc)
                          P.op("act", I("activation", out=e[:, :], in_=sk[:, :], func=AF.Exp, scale=1.0 / 16),
                               reads=[sk_r], writes=[e_r])
                      e0, e1 = Ec[(hh % 2) * 2], Ec[(hh % 2) * 2 + 1]
                      er = [R("Ec", (hh % 2) * 2), R("Ec", (hh % 2) * 2 + 1)]
                      db, db_r = dbk
                      P.op("pe", [(I("matmul", db[:, :], lhsT=onesb[:, :], rhs=e[:, :],
                                                                  start=(mc == 0), stop=(mc == 1))) for mc, e in ((0, e0), (1, e1))],
                           reads=er + [R("onesb")], writes=[db_r])
                      rd, rd_r = rdn[0], R("rdn", 0)
                      P.op("dve", I("reciprocal", out=rd[:, :], in_=db[:, :]), reads=[db_r], writes=[rd_r])
                      for jj in range(2):
                          ob, ob_r = obk.next()
                          P.op("pe", [(I("matmul",
                              ob[:, :], lhsT=Vc[:, mc, (2 * hh + jj) * 128:(2 * hh + jj + 1) * 128], rhs=e[:, :],
                              start=(mc == 0), stop=(mc == 1))) for mc, e in ((0, e0), (1, e1))],
                              reads=er + [R("Vc", 0), R("Vc", 1)], writes=[ob_r])
                          P.op("dve", I("tensor_tensor",
                              out=OC[:, 2 * hh + jj, tsl], in0=ob[:, :], in1=rd[:, :], op=ALU.mult),
                              reads=[ob_r, rd_r], writes=[R("OC", tt)])
              mmb = Rot([PB[0], PB[1], PB[2], PB[3]])
              P.barrier()

              out_proj(wo_d, OC, lambda tb: R("OC", tb // 4), 24)
              P.barrier()

              chk("cross")
              cv = Carver()
              cv.off = x_end
              junk = cv.take([128, D], BF16)
              xn = [cv.take([128, D], BF16) for _ in range(4)]
              wU = [cv.take([128, 8, 512], BF16) for _ in range(2)]
              wD = [cv.take([128, 4, D], BF16) for _ in range(2)]
              hid = [cv.take([128, 4, 512], BF16) for _ in range(2)]
              rl = [cv.take([128, 512], BF16) for _ in range(2)]
              gF = cv.take([128, D], F32)
              ot = [cv.take([128, D], F32) for _ in range(2)]
              P.dma("sp", I("dma_start", out=gF[:, :], in_=gF_d[:, :]), "gF", writes=[R("gF")])
              def final_norm(i):
                  src = XR[:, i, :]
                  ssc = 8 + 2 * (i % 4)
                  ss, rs = stat[:, ssc:ssc + 1], stat[:, ssc + 1:ssc + 2]
                  ss_r, rs_r = R("stat", ssc), R("stat", ssc + 1)
                  o_, o_r = ot[i % 2], R("ot", i % 2)
                  P.op("act", I("activation", out=junk[:, :], in_=src, func=AF.Square, accum_out=ss),
                       reads=[R("XR", i)], writes=[R("junk"), ss_r])
                  P.op("act", I("activation", out=rs, in_=ss, func=AF.Sqrt, scale=1.0 / D, bias=1e-6),
                       reads=[ss_r], writes=[rs_r])
                  P.op("dve", I("reciprocal", out=rs, in_=rs), reads=[rs_r], writes=[rs_r])
                  P.op("dve", I("scalar_tensor_tensor",
                      out=o_[:, :], in0=src, scalar=rs, in1=gF[:, :], op0=ALU.mult, op1=ALU.mult),
                      reads=[R("XR", i), rs_r, R("gF")], writes=[o_r])
                  out_toks.append(P.dma("sp", I("dma_start", out=out_d[b, i * 128:(i + 1) * 128, :], in_=o_[:, :]),
                                        f"ot{i % 2}", reads=[o_r]))

              upb = Rot([PB[0], PB[1], PB[2]])
              dnb = Rot([PB[3], PB[4], PB[5], PB[6]])
              cnt = 0
              for fg in range(8):
                  wu, wu_r = wU[fg % 2], R("wU", fg % 2)
                  wd_, wd_r = wD[fg % 2], R("wD", fg % 2)
                  def _ldmlp(fj):
                      load_w(wU[fj % 2][:, :, :], wup_d[:, fj * 512:(fj + 1) * 512].rearrange("(c p) n -> p c n", p=128),
                             f"wU{fj % 2}", R("wU", fj % 2))
                      load_w(wD[fj % 2][:, :, :], wdn_d[fj * 512:(fj + 1) * 512, :].rearrange("(c p) n -> p c n", p=128),
                             f"wD{fj % 2}", R("wD", fj % 2))
                  if fg == 0:
                      _ldmlp(0)
                  if fg + 1 < 8:
                      _ldmlp(fg + 1)
                  for tt in range(4):
                      tsl = slice(tt * 512, (tt + 1) * 512)
                      cnt += 1
                      hd, hd_r = hid[cnt % 2], R("hid", cnt % 2)
                      for fc in range(4):
                          bk, bk_r = upb.next()
                          P.op("pe", [(I("matmul",
                              bk[:, :], lhsT=wu[:, c, fc * 128:(fc + 1) * 128], rhs=HT[:, c, tsl],
                              start=(c == 0), stop=(c == 7))) for c in range(8)],
                              reads=[wu_r, R("HT", tt)], writes=[bk_r])
                          r_, r_r = rl[fc % 2], R("rl", fc % 2)
                          P.op("act", I("activation", out=r_[:, :], in_=bk[:, :], func=AF.Relu),
                               reads=[bk_r], writes=[r_r])
                          P.op("pool", I("tensor_tensor", out=hd[:, fc, :], in0=r_[:, :], in1=r_[:, :],
                                                                                         op=ALU.mult),
                               reads=[r_r], writes=[hd_r])
                      for t4 in range(4):
                          tb = tt * 4 + t4
                          for half in range(2):
                              bk, bk_r = dnb.next()
                              P.op("pe", [(I("matmul",
                                  bk[:, :], lhsT=hd[:, fc, t4 * 128:(t4 + 1) * 128], rhs=wd_[:, fc, half * 512:(half + 1) * 512],
                                  start=(fc == 0), stop=(fc == 3))) for fc in range(4)],
                                  reads=[hd_r, wd_r], writes=[bk_r])
                              P.op("dve", I("tensor_tensor",
                                  out=XR[:, tb, half * 512:(half + 1) * 512], in0=bk[:, :], in1=XR[:, tb, half * 512:(half + 1) * 512],
                                  op=ALU.add), reads=[bk_r, R("XR", tb)], writes=[R("XR", tb)])
                          if fg == 7:
                              final_norm(tb)
              P.barrier()

        except _Stop:
            P.barrier()
        P.wait_tokens("sp", out_toks)
        with nc.Block() as block:
            P.emit(block)
    nc._mk_stats = (P.nops, P.nwaits)
    return nc


_NC_CACHE = {}


def kernel(x, mem, norm_mix_g, w_in, kv_norm_g, w_uk, w_uv, w_out, norm_cross_g, norm_mem_g,
           w_q_cross, w_kv_cross, w_o_cross, norm_mlp_g, w_up, w_down, norm_final_g):
    f = lambda a: np.ascontiguousarray(np.asarray(a, dtype=np.float32))
    x, mem = f(x), f(mem)
    win_cols, _ = _win_plan()
    win = f(f(w_in)[0][:, win_cols])
    wukT = f(np.transpose(f(w_uk)[0], (2, 0, 1)).reshape(128, 512))
    wuv = f(np.transpose(f(w_uv)[0], (1, 0, 2)).reshape(128, 512))
    gcol = lambda g: f(g).reshape(8, 128).T
    gall = f(np.concatenate([gcol(norm_mix_g), gcol(norm_cross_g), gcol(norm_mem_g), gcol(norm_mlp_g),
                             f(kv_norm_g).reshape(128, 1)], axis=1))
    gF = f(np.broadcast_to(f(norm_final_g).reshape(1, D), (128, D)))
    consts = _consts()
    if "nc" not in _NC_CACHE:
        _NC_CACHE["nc"] = build_nc(NB)
    nc = _NC_CACHE["nc"]
    shared = dict(win=win, wukT=wukT, wuv=wuv, wout=f(w_out)[0], wq=f(w_q_cross)[0], wkv=f(w_kv_cross)[0],
                  wo=f(w_o_cross)[0], wup=f(w_up)[0], wdn=f(w_down)[0], gall=gall, gF=gF, **consts)
    in_maps = []
    for c in range(NCORES):
        m = dict(shared)
        m["x"] = x[c * NB:(c + 1) * NB]
        m["mem"] = mem[c * NB:(c + 1) * NB]
        in_maps.append(m)
    res = run_bass_kernel_spmd(nc, in_maps, core_ids=list(range(NCORES)))
    return np.concatenate([res.results[c]["out"] for c in range(NCORES)], axis=0).astype(np.float32)
```

```python
import math
from contextlib import ExitStack
import numpy as np
import concourse.bass as bass
import concourse.mybir as mybir
from concourse.bass_utils import run_bass_kernel_spmd

F32 = mybir.dt.float32
BF16 = mybir.dt.bfloat16
AF = mybir.ActivationFunctionType
ALU = mybir.AluOpType
AX = mybir.AxisListType

NCORES = 8
NB = 4
S = 2048
D = 1024
BIS_ITERS = 18
NEG = -1.0e30
STRIP_W = 2816


class Res:
    __slots__ = ("name", "writer", "readers", "excl")

    def __init__(self, name):
        self.name = name
        self.writer = None
        self.readers = []
        self.excl = (name[0] == "psum")


class ResReg:
    def __init__(self):
        self.d = {}

    def __call__(self, *key):
        r = self.d.get(key)
        if r is None:
            r = self.d[key] = Res(key)
        return r


class Eng:
    def __init__(self, name, sem, is_pe=False):
        self.name, self.sem, self.is_pe = name, sem, is_pe
        self.count = 0
        self.ops = []
        self.waited = {}


class Prog:
    def __init__(self, nc, sems, dma_sem_pool):
        self.nc = nc
        self.E = {n: Eng(n, sems[n], is_pe=(n == "pe")) for n in ("pe", "act", "dve", "pool", "sp")}
        self.dma_sems = {}
        self.sem_pool = dma_sem_pool
        self.nwaits = 0
        self.nops = 0

    def _deps(self, reads, writes):
        deps = []
        for r in reads:
            if r.writer is not None:
                deps.append(r.writer)
            if r.excl:
                deps.extend(r.readers)
        for w in writes:
            if w.writer is not None:
                deps.append(w.writer)
            deps.extend(w.readers)
        return deps

    def _waits(self, eng, deps):
        need = {}
        for d in deps:
            key = (d[0], d[1])
            if need.get(key, 0) < d[2]:
                need[key] = d[2]
        waits = []
        for key, v in need.items():
            if key[0] == "e" and key[1] == eng.name and eng.is_pe:
                continue
            if eng.waited.get(key, 0) >= v:
                continue
            eng.waited[key] = v
            if key[0] == "e":
                waits.append((self.E[key[1]].sem, v))
            else:
                waits.append((self.dma_sems[key[1]][0], v * 16))
        self.nwaits += len(waits)
        return waits

    def _commit(self, tok, reads, writes):
        for r in reads:
            if r.excl:
                r.writer = tok
                r.readers = []
            else:
                r.readers.append(tok)
        for w in writes:
            w.writer = tok
            w.readers = []

    def op(self, engname, fns, reads=(), writes=()):
        eng = self.E[engname]
        if isinstance(fns, tuple):
            fns = [fns]
        waits = self._waits(eng, self._deps(reads, writes))
        eng.count += 1
        tok = ("e", engname, eng.count)
        eng.ops.append((waits, fns, ("e", eng.sem)))
        self._commit(tok, reads, writes)
        self.nops += len(fns)
        return tok

    def dma(self, queue, fn, semkey, reads=(), writes=()):
        eng = self.E[queue]
        if semkey not in self.dma_sems:
            self.dma_sems[semkey] = [self.sem_pool.pop(), 0]
        ent = self.dma_sems[semkey]
        deps = self._deps(reads, writes)
        if ent[1] > 0:
            deps.append(("d", semkey, ent[1]))
        waits = self._waits(eng, deps)
        ent[1] += 1
        tok = ("d", semkey, ent[1])
        eng.ops.append((waits, [fn], ("d", ent[0])))
        self._commit(tok, reads, writes)
        self.nops += 1
        return tok

    def wait_tokens(self, engname, toks):
        eng = self.E[engname]
        waits = self._waits(eng, list(toks))
        if waits:
            eng.ops.append((waits, [], None))

    def barrier(self):
        toks = []
        for e in self.E.values():
            if e.count > 0:
                toks.append(("e", e.name, e.count))
        for k, (s, c) in self.dma_sems.items():
            if c > 0:
                toks.append(("d", k, c))
        for e in self.E.values():
            self.wait_tokens(e.name, [t for t in toks if not (t[0] == "e" and t[1] == e.name and e.is_pe)])

    def new_epoch(self, sems, reg):
        for e in self.E.values():
            e.sem = sems[e.name]
            e.count = 0
            e.waited = {k: v for k, v in e.waited.items() if k[0] == "d"}
        for r in reg.d.values():
            if r.writer is not None and r.writer[0] == "e":
                r.writer = None
            r.readers = [t for t in r.readers if t[0] != "e"]

    def emit(self, block):
        def run(eng, h):
            for waits, fns, inc in eng.ops:
                for sem, v in waits:
                    h.wait_ge(sem, v)
                ins = None
                for f in fns:
                    ins = getattr(h, f[0])(*f[1], **f[2])
                if inc is not None and ins is not None:
                    ins.then_inc(inc[1], 1 if inc[0] == "e" else 16)

        block.tensor(lambda h: run(self.E["pe"], h))
        block.scalar(lambda h: run(self.E["act"], h))
        block.vector(lambda h: run(self.E["dve"], h))
        block.gpsimd(lambda h: run(self.E["pool"], h))
        block.sync(lambda h: run(self.E["sp"], h))


def I(name, *a, **k):
    return (name, a, k)


class Rot:
    def __init__(self, items):
        self.items, self.i = items, 0

    def next(self):
        it = self.items[self.i % len(self.items)]
        self.i += 1
        return it


O_QN, O_QR, O_CKV, O_KR, O_QI, O_KI, O_WI, O_QKV = 0, 512, 768, 896, 928, 1440, 1504, 1512


def _win_plan():
    cols = []
    slabs = []

    def slab(chunks_cols):
        off = len(cols)
        chunks = []
        for kind, idx, cc in chunks_cols:
            chunks.append(dict(kind=kind, idx=idx, coff=len(cols) - off, M=len(cc)))
            cols.extend(cc)
        slabs.append((off, len(cols) - off, chunks))

    def qa(h):
        return list(range(O_QN + 64 * h, O_QN + 64 * h + 64)) + list(range(O_QR + 32 * h, O_QR + 32 * h + 32))

    r = lambda a, n: list(range(a, a + n))
    slab([("QA", h, qa(h)) for h in range(0, 4)])
    slab([("QA", h, qa(h)) for h in range(4, 8)] + [("KR", 0, r(O_KI, 64) + r(O_KR, 32))])
    slab([("QI", j, r(O_QI + 128 * j, 128)) for j in range(4)])
    slab([("QB", j, r(O_QKV + 128 * j, 128)) for j in range(4)])
    slab([("KB", j, r(O_QKV + 512 + 128 * j, 128)) for j in range(4)])
    slab([("KI", 0, r(O_KI, 64) + r(O_KI, 64)), ("CKV", 0, r(O_CKV, 128))])
    slab([("VB", 0, r(O_QKV + 1024, 512))])
    slab([("WI", 0, r(O_WI, 8))])
    return np.array(cols, dtype=np.int64), slabs


def _consts():
    c = {}
    c["ident"] = np.eye(128, dtype=np.float32)
    tri = np.zeros((128, 128), np.float32)
    tri[np.triu_indices(128, 1)] = NEG
    c["tri"] = tri
    i = np.arange(128)[:, None]
    cc = np.arange(STRIP_W)[None, :]
    dl = cc - i - 384
    m = ((dl >= 0) & (dl <= 128)).astype(np.float32) + ((dl >= 0) & (dl % 4 == 0) & (dl <= 512)) + \
        ((dl >= 0) & (dl % 16 == 0) & (dl <= 2048))
    c["strip"] = m.astype(np.float32)
    p32 = np.zeros((128, 128), np.float32)
    for k in range(16):
        p32[64 + k, 80 + k] = 1
        p32[80 + k, 64 + k] = 1
    p64 = np.zeros((128, 128), np.float32)
    for hb in (0, 64):
        for k in range(32):
            p64[hb + k, hb + 32 + k] = 1
            p64[hb + 32 + k, hb + k] = 1
    c["p32"], c["p64"] = p32, p64
    c["cvec"] = np.tile((2.0 ** -(np.arange(BIS_ITERS) + 1.0))[None, :], (128, 1)).astype(np.float32)
    pos = np.arange(S, dtype=np.float32)[None, :]
    inv32 = (10000.0 ** (-np.arange(0, 32, 2, dtype=np.float32) / 32)).astype(np.float32)
    inv64 = (10000.0 ** (-np.arange(0, 64, 2, dtype=np.float32) / 64)).astype(np.float32)
    cos32 = np.ones((128, S), np.float32)
    sin32 = np.zeros((128, S), np.float32)
    a32 = inv32[:, None] * pos
    cos32[64:80], cos32[80:96] = np.cos(a32), np.cos(a32)
    sin32[64:80], sin32[80:96] = -np.sin(a32), np.sin(a32)
    a64 = inv64[:, None] * pos
    cos64 = np.zeros((128, S), np.float32)
    sin64 = np.zeros((128, S), np.float32)
    for hb in (0, 64):
        cos64[hb:hb + 32], cos64[hb + 32:hb + 64] = np.cos(a64), np.cos(a64)
        sin64[hb:hb + 32], sin64[hb + 32:hb + 64] = -np.sin(a64), np.sin(a64)
    c["cos32"], c["sin32"], c["cos64"], c["sin64"] = cos32, sin32, cos64, sin64
    return c


class _Stop(Exception):
    pass


def build_nc(nb=NB, stop=None, dbg_step=0):
    nc = bass.Bass("TRN2", target_bir_lowering=False)
    win_cols, slabs = _win_plan()
    NCOL = len(win_cols)

    def din(name, shape, dt=F32):
        return nc.dram_tensor(name, list(shape), dt, kind="ExternalInput").ap()

    x_d = din("x", [nb, S, D])
    mem_d = din("mem", [nb, 256, D])
    win_d = din("win", [D, NCOL])
    wuk_d = din("wukT", [128, 512])
    wuv_d = din("wuv", [128, 512])
    wout_d = din("wout", [D, D])
    wq_d = din("wq", [D, D])
    wkv_d = din("wkv", [D, 2 * D])
    wo_d = din("wo", [D, D])
    wup_d = din("wup", [D, 4 * D])
    wdn_d = din("wdn", [4 * D, D])
    gall_d = din("gall", [128, 33])
    gF_d = din("gF", [128, D])
    c_ident = din("ident", [128, 128])
    c_tri = din("tri", [128, 128])
    c_strip = din("strip", [128, STRIP_W])
    c_p32 = din("p32", [128, 128])
    c_p64 = din("p64", [128, 128])
    c_cvec = din("cvec", [128, BIS_ITERS])
    c_tab = {k: din(k, [128, S]) for k in ("cos32", "sin32", "cos64", "sin64")}
    out_d = nc.dram_tensor("out", [nb, S, D], F32, kind="ExternalOutput").ap()
    qa_s = nc.dram_tensor("qa_s", [8, 96, S], BF16, kind="ExternalOutput").ap()
    qi_s = nc.dram_tensor("qi_s", [4, 128, S], BF16, kind="ExternalOutput").ap()
    qb_s = nc.dram_tensor("qb_s", [4, 128, S], BF16, kind="ExternalOutput").ap()

    with ExitStack() as es:
        def sb(name, shape, dt):
            return es.enter_context(nc.sbuf_tensor("sb_" + name, list(shape), dt))

        def sem(name):
            return es.enter_context(nc.semaphore(name))

        sems = {n: sem("s_" + n) for n in ("pe", "act", "dve", "pool", "sp")}
        P = Prog(nc, sems, [sem(f"dq{i}") for i in range(40)])
        R = ResReg()

        HT = sb("HT", [128, 8, S], BF16)
        identb = sb("identb", [128, 128], BF16)
        onesb = sb("onesb", [128, 128], BF16)
        tri = sb("tri", [128, 128], F32)
        strip = sb("strip", [128, STRIP_W], BF16)
        p32 = sb("p32", [128, 128], BF16)
        p64 = sb("p64", [128, 128], BF16)
        cvec = sb("cvec", [128, BIS_ITERS], F32)
        gall = sb("gall", [128, 33], F32)
        wukT = sb("wukT", [128, 512], BF16)
        wuv = sb("wuv", [128, 512], BF16)
        weff = sb("weff", [128, 16, 8], F32)
        stat = sb("stat", [128, 64], F32)
        ARENA_B = 165 * 1024
        arena = sb("arena", [128, ARENA_B // 2], BF16)
        psum = [es.enter_context(nc.psum_tensor(f"ps{i}", [128, 512], F32)) for i in range(8)]
        PB = [(psum[i], R("psum", i)) for i in range(8)]

        class Carver:
            def __init__(self):
                self.off = 0

            def take(self, shape, dt):
                esz = 4 if dt == F32 else 2
                n = int(np.prod(shape[1:]))
                nbytes = n * esz
                nbytes_al = (nbytes + 63) // 64 * 64
                a = arena[:, self.off // 2: (self.off + nbytes) // 2]
                self.off += nbytes_al
                assert self.off <= ARENA_B, ("arena overflow", self.off)
                if dt == F32:
                    a = a.bitcast(F32)
                if len(shape) == 3:
                    a = a.rearrange("p (a b) -> p a b", a=shape[1])
                elif len(shape) == 4:
                    a = a.rearrange("p (a b c) -> p a b c", a=shape[1], b=shape[2])
                return a

        def bfv(ps_ap):
            return ps_ap.bitcast(BF16)

        def ld(dst, src, key, queue="sp"):
            P.dma(queue, I("dma_start", out=dst, in_=src), key, writes=[R(key)])

        ld(identb[:], c_ident[:, :], "identb", "pool")
        P.dma("pool", I("dma_start", out=strip[:, 0:1408], in_=c_strip[:, 0:1408]), "strip", writes=[R("strip")])
        P.dma("pool", I("dma_start", out=strip[:, 1408:STRIP_W], in_=c_strip[:, 1408:STRIP_W]), "strip2", writes=[R("strip2")])
        ld(p32[:], c_p32[:, :], "p32", "pool")
        ld(p64[:], c_p64[:, :], "p64", "pool")
        ld(wukT[:], wuk_d[:, :], "wukT", "pool")
        ld(wuv[:], wuv_d[:, :], "wuv", "pool")
        ld(tri[:], c_tri[:, :], "tri")
        ld(cvec[:], c_cvec[:, :], "cvec")
        ld(gall[:], gall_d[:, :], "gall")
        P.op("dve", I("memset", onesb[:], 1.0), writes=[R("onesb")])
        CONST_R = [R(k) for k in ("identb", "strip", "strip2", "p32", "p64", "wukT", "wuv", "tri", "cvec", "gall", "onesb")]
        P.barrier()

        out_toks = []

        def evac(i, dst, src, reads, writes):
            if i % 2 == 0:
                P.op("act", I("activation", out=dst, in_=src, func=AF.Copy), reads=reads, writes=writes)
            else:
                P.op("dve", I("tensor_copy", out=dst, in_=src), reads=reads, writes=writes)

        def norm_tile(src, src_res, gcol, i, junk, junk_r, xn, xn_r, tbank, dstT, dst_res, ssc, part=None):
            ss = stat[:, ssc:ssc + 1]
            rs = stat[:, ssc + 1:ssc + 2]
            ss_r, rs_r = R("stat", ssc), R("stat", ssc + 1)
            if part in (None, "a", "a1"):
                P.op("act", I("activation", out=junk, in_=src, func=AF.Square, accum_out=ss),
                     reads=[src_res], writes=[junk_r, ss_r])
                P.op("act", I("activation", out=rs, in_=ss, func=AF.Sqrt, scale=1.0 / D, bias=1e-6),
                     reads=[ss_r], writes=[rs_r])
                P.op("dve", I("reciprocal", out=rs, in_=rs), reads=[rs_r], writes=[rs_r])
            if part in (None, "a", "a2"):
                P.op("act", I("activation", out=xn, in_=src, func=AF.Copy, scale=rs),
                     reads=[src_res, rs_r], writes=[xn_r])
            if part in ("a", "a1", "a2"):
                return
            tb_ap, tb_r = tbank
            tv = bfv(tb_ap[:, :]).rearrange("p (a b) -> p a b", a=8)
            P.op("pe", [(I("transpose", out=tv[:, c, :], in_=xn[:, c * 128:(c + 1) * 128], identity=identb[:]))
                        for c in range(8)], reads=[xn_r, R("identb")], writes=[tb_r])
            P.op("dve", I("tensor_tensor", out=dstT, in0=tv,
                                                   in1=gall[:, gcol:gcol + 8].unsqueeze(2).to_broadcast([128, 8, 128]),
                                                   op=ALU.mult),
                 reads=[tb_r, R("gall")], writes=[dst_res])

        def load_w(dst, src_rows_cols, key, res):
            P.dma("pool", I("dma_start", out=dst, in_=src_rows_cols), key, writes=[res])

        def chk(tag):
            if stop == tag:
                raise _Stop()

        try:
          for b in range(nb):
              P.barrier()
              P.new_epoch({n: sem(f"s{b}_" + n) for n in ("pe", "act", "dve", "pool", "sp")}, R)
              cv = Carver()
              Kp = cv.take([128, 8, S], BF16)
              Vp = cv.take([128, 16, 768], BF16)
              kidx = cv.take([128, S], BF16)
              kbT = cv.take([128, 4, S], BF16)
              Vb = cv.take([128, 16, 768], BF16)
              keys_end = cv.off
              xs = [cv.take([128, D], F32) for _ in range(4)]
              junk = cv.take([128, D], BF16)
              xn = [cv.take([128, D], BF16) for _ in range(4)]
              cv.off = keys_end
              tabc = cv.take([128, S], F32)
              tabs_ = cv.take([128, S], F32)
              wsl = [cv.take([128, 8, 512], BF16) for _ in range(2)]
              qbuf = [cv.take([128, 512], BF16) for _ in range(2)]
              t1b = [cv.take([128, 512], F32) for _ in range(2)]
              t2b = [cv.take([128, 512], F32) for _ in range(2)]
              stg = [cv.take([128, 512], BF16) for _ in range(4)]
              ckf = cv.take([128, 512], F32)
              sqb = cv.take([128, 512], BF16)
              rtb = cv.take([128, 512], F32)
              ckvT = cv.take([128, S], BF16)

              for (T, nm) in ((Vp, "Vp"), (Vb, "Vb")):
                  tv4 = T.rearrange("p k (a c) -> p k a c", a=4)
                  P.op("pool", I("memset", tv4[:, :, :, 64:128], 1.0),
                       writes=[R(nm, kb) for kb in range(16)])
              for i in range(16):
                  P.dma("sp", I("dma_start", out=xs[i % 4][:, :], in_=x_d[b, i * 128:(i + 1) * 128, :]),
                        f"xs{i % 4}", writes=[R("xs", i % 4)])
                  def _n1(j, part):
                      norm_tile(xs[j % 4][:, :], R("xs", j % 4), 0, j, junk[:, :], R("junk"), xn[j % 4][:, :], R("xn", j % 4),
                                PB[7 - (j % 2)], HT[:, :, j * 128:(j + 1) * 128], R("HT", j // 4), 2 * (j % 4), part=part)
                  _n1(i, "a1")
                  if i >= 1:
                      _n1(i - 1, "a2")
                      _n1(i - 1, "b")
              _n1(15, "a2")
              _n1(15, "b")
              P.barrier()
              chk("norm1")
              cur_tab = [None]
              mmb = Rot([PB[0], PB[1], PB[2], PB[7]])
              ppb = Rot([PB[3], PB[4]])
              msb = Rot([PB[5], PB[6]])
              ecnt = [0]
              for si, (soff, sw, chunks) in enumerate(slabs):
                  if si > 0:
                      chk(f"slab{si - 1}")
                  ws, ws_r = wsl[si % 2], R("wsl", si % 2)

                  def _ldslab(sj):
                      so_, sw_, _ = slabs[sj]
                      load_w(wsl[sj % 2][:, :, 0:sw_], win_d[:, so_:so_ + sw_].rearrange("(c p) n -> p c n", p=128),
                             f"wsl{sj % 2}", R("wsl", sj % 2))
                  if si == 0:
                      _ldslab(0)
                  if si + 1 < len(slabs):
                      _ldslab(si + 1)
                  for ch in chunks:
                      kind, idx, coff, M = ch["kind"], ch["idx"], ch["coff"], ch["M"]
                      if kind in ("VB", "WI"):
                          for tb in range(16):
                              bk, bk_r = mmb.next()
                              P.op("pe", [(I("matmul",
                                  bk[:, 0:M], lhsT=HT[:, c, tb * 128:(tb + 1) * 128], rhs=ws[:, c, coff:coff + M],
                                  start=(c == 0), stop=(c == 7))) for c in range(8)],
                                  reads=[ws_r, R("HT", tb // 4)], writes=[bk_r])
                              if kind == "WI":
                                  P.op("act", I("activation",
                                      out=weff[:, tb, :], in_=bk[:, 0:8], func=AF.Copy, scale=float(8 ** -0.5 * 64 ** -0.5)),
                                      reads=[bk_r], writes=[R("weff", tb)])
                              else:
                                  src4 = bk[:, :].rearrange("p (a c) -> p a c", a=4)
                                  dst4 = Vb[:, tb, :].rearrange("p (a c) -> p a c", a=4)
                                  P.op("act", I("activation", out=dst4[:, :, 0:64], in_=src4[:, :, 0:64], func=AF.Copy),
                                       reads=[bk_r], writes=[R("Vb", tb)])
                                  P.op("dve", I("tensor_copy", out=dst4[:, :, 128:192], in_=src4[:, :, 64:128]),
                                       reads=[bk_r], writes=[R("Vb", tb)])
                          continue
                      for tt in range(4):
                          tsl = slice(tt * 512, (tt + 1) * 512)
                          bk, bk_r = mmb.next()
                          P.op("pe", [(I("matmul",
                              bk[0:M, :], lhsT=ws[:, c, coff:coff + M], rhs=HT[:, c, tsl],
                              start=(c == 0), stop=(c == 7))) for c in range(8)],
                              reads=[ws_r, R("HT", tt)], writes=[bk_r])
                          if dbg_step == 1:
                              raise _Stop()
                          if kind == "CKV":
                              P.op("act", I("activation", out=ckf[:, :], in_=bk[:, :], func=AF.Copy),
                                   reads=[bk_r], writes=[R("ckf")])
                              P.op("act", I("activation", out=sqb[:, :], in_=bk[:, :], func=AF.Square),
                                   reads=[bk_r], writes=[R("sqb")])
                              sm, sm_r = msb.next()
                              P.op("pe", I("matmul", sm[:, :], lhsT=onesb[:, :], rhs=sqb[:, :], start=True, stop=True),
                                   reads=[R("sqb"), R("onesb")], writes=[sm_r])
                              P.op("act", I("activation", out=rtb[:, :], in_=sm[:, :], func=AF.Sqrt,
                                                                          scale=1.0 / 128, bias=1e-6),
                                   reads=[sm_r], writes=[R("rtb")])
                              P.op("dve", I("reciprocal", out=rtb[:, :], in_=rtb[:, :]), reads=[R("rtb")], writes=[R("rtb")])
                              P.op("dve", I("scalar_tensor_tensor",
                                  out=ckvT[:, tsl], in0=ckf[:, :], scalar=gall[:, 32:33], in1=rtb[:, :],
                                  op0=ALU.mult, op1=ALU.mult),
                                  reads=[R("ckf"), R("rtb"), R("gall")], writes=[R("ckvT", tt)])
                              for hh in range(8):
                                  kb_, kb_r = msb.next()
                                  P.op("pe", I("matmul",
                                      kb_[0:64, :], lhsT=wukT[:, hh * 64:(hh + 1) * 64], rhs=ckvT[:, tsl], start=True, stop=True),
                                      reads=[R("ckvT", tt), R("wukT")], writes=[kb_r])
                                  ecnt[0] += 1
                                  evac(ecnt[0], Kp[0:64, hh, tsl], kb_[0:64, :], [kb_r], [R("Kp", hh, tt)])
                              for j in range(4):
                                  kb = tt * 4 + j
                                  vb_, vb_r = msb.next()
                                  P.op("pe", I("matmul",
                                      vb_[:, :], lhsT=ckvT[:, kb * 128:(kb + 1) * 128], rhs=wuv[:, :], start=True, stop=True),
                                      reads=[R("ckvT", tt), R("wuv")], writes=[vb_r])
                                  src4 = vb_[:, :].rearrange("p (a c) -> p a c", a=4)
                                  dst4 = Vp[:, kb, :].rearrange("p (a c) -> p a c", a=4)
                                  P.op("act", I("activation", out=dst4[:, :, 0:64], in_=src4[:, :, 0:64], func=AF.Copy),
                                       reads=[vb_r], writes=[R("Vp", kb)])
                                  P.op("dve", I("tensor_copy", out=dst4[:, :, 128:192], in_=src4[:, :, 64:128]),
                                       reads=[vb_r], writes=[R("Vp", kb)])
                              continue
                          t32 = kind in ("QA", "KR")
                          tk = "32" if t32 else "64"
                          if cur_tab[0] != tk:
                              cur_tab[0] = tk
                              P.dma("sp", I("dma_start", out=tabc[:, :], in_=c_tab["cos" + tk][:, :]), "tabc", writes=[R("tabc")])
                              P.dma("sp", I("dma_start", out=tabs_[:, :], in_=c_tab["sin" + tk][:, :]), "tabs", writes=[R("tabs")])
                          cosT, sinT = tabc, tabs_
                          cos_r, sin_r = R("tabc"), R("tabs")
                          pm, pm_r = (p32, R("p32")) if t32 else (p64, R("p64"))
                          u = ecnt[0] = ecnt[0] + 1
                          qb_, qb_r = qbuf[u % 2], R("qbuf", u % 2)
                          t1, t1_r = t1b[u % 2], R("t1b", u % 2)
                          t2, t2_r = t2b[u % 2], R("t2b", u % 2)
                          P.op("act", I("activation", out=qb_[0:M, :], in_=bk[0:M, :], func=AF.Copy),
                               reads=[bk_r], writes=[qb_r])
                          if dbg_step == 2:
                              raise _Stop()
                          pb_, pb_r = ppb.next()
                          P.op("pe", I("matmul",
                              pb_[0:M, :], lhsT=pm[0:M, 0:M], rhs=qb_[0:M, :], start=True, stop=True),
                              reads=[qb_r, pm_r], writes=[pb_r])
                          if dbg_step == 3:
                              raise _Stop()
                          import os as _os
                          _v = _os.environ.get("DBG_VAR", "")
                          if _v == "v1":
                              P.op("dve", I("tensor_copy", out=t1[0:M, :], in_=bk[0:M, :]), reads=[bk_r], writes=[t1_r])
                          elif _v == "v5":
                              P.op("dve", I("tensor_copy", out=t1[0:M, :], in_=bk[0:M, :]), reads=[bk_r, qb_r, pb_r], writes=[t1_r])
                          elif _v == "v2":
                              P.op("dve", I("tensor_tensor", out=t1[0:M, :], in0=bk[0:M, :], in1=t2[0:M, :], op=ALU.mult),
                                   reads=[bk_r], writes=[t1_r])
                          elif _v == "v3":
                              P.op("dve", I("tensor_copy", out=t1[0:M, :], in_=cosT[0:M, tsl]), reads=[cos_r], writes=[t1_r])
                          elif _v == "v4":
                              P.op("dve", I("tensor_tensor", out=t1[0:M, :], in0=bk[0:M, :], in1=cosT[0:M, tsl], op=ALU.mult),
                                   reads=[bk_r], writes=[t1_r])
                          else:
                              P.op("dve", I("tensor_tensor",
                                  out=t1[0:M, :], in0=bk[0:M, :], in1=cosT[0:M, tsl], op=ALU.mult),
                                  reads=[bk_r, cos_r], writes=[t1_r])
                          if dbg_step == 4:
                              raise _Stop()
                          P.op("dve", I("tensor_tensor",
                              out=t2[0:M, :], in0=pb_[0:M, :], in1=sinT[0:M, tsl], op=ALU.mult),
                              reads=[pb_r, sin_r], writes=[t2_r])
                          if dbg_step == 5:
                              raise _Stop()
                          if kind == "KR":
                              for hh in range(8):
                                  P.op("pool", I("tensor_tensor",
                                      out=Kp[64:96, hh, tsl], in0=t1[64:96, :], in1=t2[64:96, :], op=ALU.add),
                                      reads=[t1_r, t2_r], writes=[R("Kp2", hh, tt)])
                          elif kind == "KI":
                              P.op("pool", I("tensor_tensor",
                                  out=kidx[:, tsl], in0=t1[:, :], in1=t2[:, :], op=ALU.add),
                                  reads=[t1_r, t2_r], writes=[R("kidx", tt)])
                          elif kind == "KB":
                              P.op("pool", I("tensor_tensor",
                                  out=kbT[:, idx, tsl], in0=t1[:, :], in1=t2[:, :], op=ALU.add),
                                  reads=[t1_r, t2_r], writes=[R("kbT", idx, tt)])
                          else:
                              sg, sg_r = stg[u % 4], R("stg", u % 4)
                              P.op("pool", I("tensor_tensor",
                                  out=sg[0:M, :], in0=t1[0:M, :], in1=t2[0:M, :], op=ALU.add),
                                  reads=[t1_r, t2_r], writes=[sg_r])
                              if dbg_step == 6:
                                  raise _Stop()
                              dst = {"QA": qa_s, "QI": qi_s, "QB": qb_s}[kind]
                              P.dma("sp", I("dma_start",
                                  out=dst[idx, 0:M, tsl], in_=sg[0:M, :]), f"stg{u % 4}",
                                  reads=[sg_r], writes=[R("scr", kind, idx, tt)])
              P.barrier()

              chk("proj")
              cv = Carver()
              cv.off = keys_end
              qaq = cv.take([128, 4, 512], BF16)
              qiq = cv.take([128, 4, 512], BF16)
              qbq = cv.take([128, 4, 512], BF16)
              idxb = [cv.take([128, S], F32) for _ in range(2)]
              Rb = [cv.take([128, 512], BF16) for _ in range(3)]
              diag = cv.take([128, 8, 128], BF16)
              Mk = cv.take([128, S], BF16)
              maskT = cv.take([128, 16, 512], BF16)
              Eb = [cv.take([128, 512], BF16) for _ in range(3)]
              Pmb = [cv.take([128, 512], BF16) for _ in range(6)]
              dsh = [cv.take([128, 512], F32)] * 2
              bis = cv.take([128, 32], F32)
              steps = cv.take([128, BIS_ITERS], F32)

              Lb = Rot([PB[0], PB[1]])
              accb = PB[2]
              Tb = PB[2]
              Ob = Rot([PB[6], PB[7]])
              sc_a = float((64 + 32) ** -0.5)
              sc_b = 0.125
              ucount = [0]

              seqc = [0]
              inflight = []
              obank = {}
              Sbh = [None]
              Sb_small = Rot([PB[3], PB[4], PB[5]])
              Sb_big = Rot([PB[3], PB[4], PB[5], PB[0], PB[1]])

              def load_qaq(hf, qt_):
                  qs_ = slice(qt_ * 512, (qt_ + 1) * 512)
                  P.dma("sp", I("dma_start", out=qaq[0:96, :, :], in_=qa_s[4 * hf:4 * hf + 4, :, qs_].rearrange("a r t -> r a t")),
                        "qaq", reads=[R("scr", "QA", i, qt_) for i in range(8)], writes=[R("qaq")])

              def emitQK(un):
                  g, hh, kb, qt_, seq, meng = un
                  c0 = max(0, kb - 4 * qt_) * 128
                  sbk, sbk_r = Sbh[0].next()
                  pr = (hh % 2) * 64
                  if g == "A":
                      if hh == 4 and kb == 0:
                          load_qaq(1, qt_)
                      P.op("pe", I("matmul", sbk[:, c0:512], lhsT=Kp[0:96, hh, kb * 128:(kb + 1) * 128],
                                   rhs=qaq[0:96, hh % 4, c0:512], start=True, stop=True),
                           reads=[R("Kp", hh, kb // 4), R("Kp2", hh, kb // 4), R("qaq")], writes=[sbk_r])
                  else:
                      P.op("pe", I("matmul", sbk[:, c0:512], lhsT=kbT[pr:pr + 64, hh // 2, kb * 128:(kb + 1) * 128],
                                   rhs=qbq[pr:pr + 64, hh // 2, c0:512], start=True, stop=True),
                           reads=[R("kbT", hh // 2, kb // 4), R("qbq")], writes=[sbk_r])
                  e, e_r = Eb[seq % 3], R("Eb", seq % 3)
                  P.op("act", I("activation", out=e[:, c0:512], in_=sbk[:, c0:512], func=AF.Exp,
                                scale=(sc_a if g == "A" else sc_b)),
                       reads=[sbk_r], writes=[e_r])
                  pmt, pmt_r = Pmb[seq % 6], R("Pmb", seq % 6)
                  if g == "A":
                      msk, msk_r = maskT[:, kb, c0:512], R("maskT")
                  else:
                      o_ = 128 * (4 * qt_ - kb) + 384
                      msk, msk_r = strip[:, o_ + c0:o_ + 512], R("strip")
                  P.op(meng, I("tensor_tensor", out=pmt[:, c0:512], in0=e[:, c0:512], in1=msk, op=ALU.mult),
                       reads=[e_r, msk_r], writes=[pmt_r])

              def emitPV(un):
                  g, hh, kb, qt_, seq, meng = un
                  NK_ = 4 * qt_ + 4
                  qs_ = slice(qt_ * 512, (qt_ + 1) * 512)
                  c0 = max(0, kb - 4 * qt_) * 128
                  if kb == 0:
                      obank[(g, hh)] = Ob.next()
                  ob, ob_r = obank[(g, hh)]
                  pmt, pmt_r = Pmb[seq % 6], R("Pmb", seq % 6)
                  V = Vp if g == "A" else Vb
                  vo = (hh // 2) * 192 + (hh % 2) * 64
                  P.op("pe", I("matmul", ob[:, c0:512], lhsT=V[:, kb, vo:vo + 128], rhs=pmt[:, c0:512],
                               start=(kb == 0), stop=(kb == NK_ - 1)),
                       reads=[pmt_r, R("Vp" if g == "A" else "Vb", kb)], writes=[ob_r])
                  if kb == NK_ - 1:
                      ev = (hh % 2 == 0)
                      np_, dp_ = (slice(0, 64), slice(64, 128)) if ev else (slice(64, 128), slice(0, 64))
                      d, d_r = dsh[0], R("dsh", 0)
                      P.op("act", I("activation", out=d[np_, :], in_=ob[dp_, :], func=AF.Ln),
                           reads=[ob_r], writes=[d_r])
                      P.op("act", I("activation", out=d[np_, :], in_=d[np_, :], func=AF.Exp, scale=-1.0), reads=[d_r], writes=[d_r])
                      chunk = (hh // 2) + (0 if g == "A" else 4)
                      P.op("dve", I("tensor_tensor", out=HT[np_, chunk, qs_], in0=ob[np_, :], in1=d[np_, :], op=ALU.mult),
                           reads=[ob_r, d_r], writes=[R("HT", qt_)])

              def feed(g, hh, kb, qt_, meng, LA):
                  un = (g, hh, kb, qt_, seqc[0], meng)
                  seqc[0] += 1
                  emitQK(un)
                  inflight.append(un)
                  while len(inflight) > LA:
                      emitPV(inflight.pop(0))

              def flush():
                  while inflight:
                      emitPV(inflight.pop(0))

              for qt in range(4):
                  qsl = slice(qt * 512, (qt + 1) * 512)
                  NK = 4 * qt + 4
                  load_qaq(0, qt)
                  P.dma("sp", I("dma_start", out=qiq[:, :, :], in_=qi_s[:, :, qsl].rearrange("a r t -> r a t")),
                        "qiq", reads=[R("scr", "QI", i, qt) for i in range(4)], writes=[R("qiq")])
                  P.dma("sp", I("dma_start", out=qbq[:, :, :], in_=qb_s[:, :, qsl].rearrange("a r t -> r a t")),
                        "qbq", reads=[R("scr", "QB", i, qt) for i in range(4)], writes=[R("qbq")])
                  bunits = [(hh, kb) for hh in range(8) for kb in range(NK)]
                  per = len(bunits) // 4

                  def indexer(tb):
                      tbl = tb - 4 * qt
                      lo = tbl * 128
                      n = 128 * (tb + 1)
                      idx_sb, idx_r = idxb[tb % 2], R("idx", tb % 2)
                      for hh in range(8):
                          P.op("pool", I("tensor_scalar",
                              out=diag[:, hh, :], in0=identb[:, :], scalar1=weff[:, tb, hh:hh + 1], scalar2=1.0,
                              op0=ALU.mult, op1=ALU.mult),
                              reads=[R("identb"), R("weff", tb)], writes=[R("diag", hh)])
                      for st in range(qt + 1):
                          wd = min(512, n - 512 * st)
                          acc, acc_r = accb

                          def emitL(hh):
                              lb, lb_r = Lb.next()
                              pr = (hh % 2) * 64
                              P.op("pe", I("matmul",
                                  lb[:, 0:wd], lhsT=qiq[pr:pr + 64, hh // 2, lo:lo + 128],
                                  rhs=kidx[pr:pr + 64, st * 512:st * 512 + wd], start=True, stop=True),
                                  reads=[R("qiq"), R("kidx", st)], writes=[lb_r])
                              rb, rb_r = Rb[hh % 3], R("Rb", hh % 3)
                              P.op("act", I("activation", out=rb[:, 0:wd], in_=lb[:, 0:wd], func=AF.Relu),
                                   reads=[lb_r], writes=[rb_r])

                          def emitA(hh):
                              rb, rb_r = Rb[hh % 3], R("Rb", hh % 3)
                              P.op("pe", I("matmul",
                                  acc[:, 0:wd], lhsT=diag[:, hh, :], rhs=rb[:, 0:wd], start=(hh == 0), stop=(hh == 7)),
                                  reads=[rb_r, R("diag", hh)], writes=[acc_r])

                          emitL(0)
                          emitL(1)
                          for hh in range(8):
                              if hh + 2 < 8:
                                  emitL(hh + 2)
                              emitA(hh)
                          c0 = st * 512
                          P.op("act", I("activation", out=idx_sb[:, c0:c0 + wd], in_=acc[:, 0:wd], func=AF.Copy),
                               reads=[acc_r], writes=[idx_r])
                          if st == qt:
                              P.op("pool", I("tensor_tensor",
                                  out=idx_sb[:, c0 + wd - 128:c0 + wd], in0=idx_sb[:, c0 + wd - 128:c0 + wd], in1=tri[:, :],
                                  op=ALU.add), reads=[idx_r, R("tri")], writes=[idx_r])

                  def bisect(tb):
                      n = 128 * (tb + 1)
                      idx_sb, idx_r = idxb[tb % 2], R("idx", tb % 2)
                      thr = bis[:, 0:1]
                      if tb >= 2:
                          mx, mn, W, cnt, a_ = bis[:, 1:2], bis[:, 2:3], bis[:, 3:4], bis[:, 4:5], bis[:, 5:6]
                          RB = R("bis")
                          P.op("dve", I("tensor_reduce", out=mx, in_=idx_sb[:, 0:n], op=ALU.max, axis=AX.X),
                               reads=[idx_r], writes=[RB])
                          P.op("dve", I("tensor_reduce", out=mn, in_=idx_sb[:, 0:n - 128], op=ALU.min, axis=AX.X),
                               reads=[idx_r], writes=[RB])
                          P.op("dve", I("tensor_tensor", out=W, in0=mx, in1=mn, op=ALU.subtract), reads=[RB], writes=[RB])
                          P.op("dve", I("tensor_tensor", out=thr, in0=mx, in1=mn, op=ALU.add), reads=[RB], writes=[RB])
                          P.op("dve", I("tensor_scalar", out=thr, in0=thr, scalar1=0.5, scalar2=None, op0=ALU.mult),
                               reads=[RB], writes=[RB])
                          P.op("dve", I("tensor_scalar", out=steps[:, :], in0=cvec[:, :], scalar1=W, scalar2=None, op0=ALU.mult),
                               reads=[RB, R("cvec")], writes=[RB])
                          for it in range(BIS_ITERS):
                              P.op("dve", I("tensor_scalar",
                                  out=Mk[:, 0:n], in0=idx_sb[:, 0:n], scalar1=thr, scalar2=None,
                                  op0=ALU.is_ge, op1=ALU.add, accum_out=cnt),
                                  reads=[idx_r, RB], writes=[R("Mk"), RB])
                              P.op("dve", I("tensor_scalar", out=a_, in0=cnt, scalar1=255.5, scalar2=0.5,
                                            op0=ALU.is_ge, op1=ALU.subtract),
                                   reads=[RB], writes=[RB])
                              P.op("dve", I("scalar_tensor_tensor",
                                  out=thr, in0=a_, scalar=steps[:, it:it + 1], in1=thr, op0=ALU.mult, op1=ALU.add),
                                  reads=[RB], writes=[RB])
                          P.op("dve", I("tensor_tensor", out=thr, in0=thr, in1=steps[:, BIS_ITERS - 1:BIS_ITERS], op=ALU.subtract),
                               reads=[RB], writes=[RB])
                          P.op("dve", I("tensor_scalar", out=Mk[:, 0:n], in0=idx_sb[:, 0:n], scalar1=thr, scalar2=None,
                                        op0=ALU.is_ge),
                               reads=[idx_r, RB], writes=[R("Mk")])
                      else:
                          P.op("dve", I("tensor_scalar", out=Mk[:, 0:n], in0=idx_sb[:, 0:n], scalar1=-1.0e29, scalar2=None,
                                        op0=ALU.is_ge),
                               reads=[idx_r], writes=[R("Mk")])

                  def transposes(tb):
                      lo = (tb - 4 * qt) * 128
                      tbk, tbk_r = Tb
                      tv = bfv(tbk[:, :]).rearrange("p (a b) -> p a b", a=8)
                      for g0 in range(0, tb + 1, 8):
                          g1 = min(tb + 1, g0 + 8)
                          P.op("pe", [(I("transpose", out=tv[:, kb - g0, :], in_=Mk[:, kb * 128:(kb + 1) * 128],
                                         identity=identb[:, :])) for kb in range(g0, g1)],
                               reads=[R("Mk"), R("identb")], writes=[tbk_r])
                          ucount[0] += 1
                          evac(ucount[0], maskT[:, g0:g1, lo:lo + 128], tv[:, 0:g1 - g0, :], [tbk_r], [R("maskT")])

                  indexer(4 * qt)
                  for tbl in range(4):
                      tb = 4 * qt + tbl
                      bisect(tb)
                      if tbl < 3:
                          indexer(tb + 1)
                      Sbh[0] = Sb_small
                      for (hh, kb) in bunits[tbl * per:(tbl + 1) * per]:
                          feed("B", hh, kb, qt, "pool", 3)
                      transposes(tb)

                  if qt == 0:
                      chk("idx0")
                  Sbh[0] = Sb_big
                  for hh in range(8):
                      for kb in range(NK):
                          feed("A", hh, kb, qt, ("pool" if seqc[0] % 5 == 0 else "dve"), 5)
                  flush()
              P.barrier()

              chk("attn")
              cv = Carver()
              XR = cv.take([128, 16, D], F32)
              x_end = cv.off
              wA = [cv.take([128, 8, 512], BF16) for _ in range(2)]
              junk = cv.take([128, D], BF16)
              xn = [cv.take([128, D], BF16) for _ in range(4)]
              mmb = Rot([PB[0], PB[1], PB[2], PB[3]])
              for q4 in range(4):
                  P.dma("sp", I("dma_start",
                      out=XR[:, q4 * 4:(q4 + 1) * 4, :], in_=x_d[b, q4 * 512:(q4 + 1) * 512, :].rearrange("(n p) d -> p n d", p=128)),
                      f"xr{q4}", writes=[R("XR", t) for t in range(q4 * 4, q4 * 4 + 4)])

              def out_proj(w_d, srcT, src_res_fn, gcol):
                  for half in range(2):
                      load_w(wA[half][:, :, :], w_d[:, half * 512:(half + 1) * 512].rearrange("(c p) n -> p c n", p=128),
                             f"wA{half}", R("wA", half))
                  for tb in range(16):
                      for half in range(2):
                          ws, ws_r = wA[half], R("wA", half)
                          bk, bk_r = mmb.next()
                          P.op("pe", [(I("matmul",
                              bk[:, :], lhsT=srcT[:, c, tb * 128:(tb + 1) * 128], rhs=ws[:, c, :],
                              start=(c == 0), stop=(c == 7))) for c in range(8)],
                              reads=[ws_r, src_res_fn(tb)], writes=[bk_r])
                          P.op("dve", I("tensor_tensor",
                              out=XR[:, tb, half * 512:(half + 1) * 512], in0=bk[:, :], in1=XR[:, tb, half * 512:(half + 1) * 512],
                              op=ALU.add), reads=[bk_r, R("XR", tb)], writes=[R("XR", tb)])
                      def _nrm(t_, part):
                          norm_tile(XR[:, t_, :], R("XR", t_), gcol, t_, junk[:, :], R("junk"), xn[t_ % 4][:, :], R("xn", t_ % 4),
                                    PB[7 - (t_ % 2)], HT[:, :, t_ * 128:(t_ + 1) * 128], R("HTb", t_), 2 * (t_ % 4), part=part)
                      _nrm(tb, "a")
                      if tb >= 2:
                          _nrm(tb - 2, "b")
                  _nrm(14, "b")
                  _nrm(15, "b")

              out_proj(wout_d, HT, lambda tb: R("HTb", tb), 8)
              P.barrier()

              chk("wout")
              cv = Carver()
              cv.off = x_end
              OC = cv.take([128, 8, S], BF16)
              wA = [cv.take([128, 8, 512], BF16) for _ in range(2)]
              junk = cv.take([128, D], BF16)
              xn = [cv.take([128, D], BF16) for _ in range(4)]
              ms = [cv.take([128, D], F32)] * 2
              mT = cv.take([128, 8, 256], BF16)
              KcT = cv.take([128, 8, 256], BF16)
              Vc = cv.take([128, 2, D], BF16)
              qcT = cv.take([128, 8, 512], BF16)
              Ec = [cv.take([128, 512], BF16) for _ in range(4)]
              rdn = [cv.take([128, 512], F32)] * 2
              for i in range(2):
                  P.dma("sp", I("dma_start", out=ms[i][:, :], in_=mem_d[b, i * 128:(i + 1) * 128, :]), "ms0",
                        writes=[R("ms", 0)])
                  norm_tile(ms[i][:, :], R("ms", 0), 16, i, junk[:, :], R("junk"), xn[i % 4][:, :], R("xn", i % 4),
                            PB[7 - (i % 2)], mT[:, :, i * 128:(i + 1) * 128], R("mT"), 2 * (i % 4))
              mmb = Rot([PB[0], PB[1], PB[2]])
              ec = 0
              for half in range(2):
                  ws, ws_r = wA[half], R("wA", half)
                  load_w(ws[:, :, :], wkv_d[:, half * 512:(half + 1) * 512].rearrange("(c p) n -> p c n", p=128), f"wA{half}", ws_r)
                  for j4 in range(4):
                      j = half * 4 + j4
                      bk, bk_r = mmb.next()
                      P.op("pe", [(I("matmul",
                          bk[:, 0:256], lhsT=ws[:, c, j4 * 128:(j4 + 1) * 128], rhs=mT[:, c, :], start=(c == 0), stop=(c == 7)))
                          for c in range(8)], reads=[ws_r, R("mT")], writes=[bk_r])
                      ec += 1
                      evac(ec, KcT[:, j, :], bk[:, 0:256], [bk_r], [R("KcT", j)])
              for half in range(2):
                  ws, ws_r = wA[half], R("wA", half)
                  load_w(ws[:, :, :], wkv_d[:, D + half * 512:D + (half + 1) * 512].rearrange("(c p) n -> p c n", p=128),
                         f"wA{half}", ws_r)
                  for mc in range(2):
                      bk, bk_r = mmb.next()
                      P.op("pe", [(I("matmul",
                          bk[:, :], lhsT=mT[:, c, mc * 128:(mc + 1) * 128], rhs=ws[:, c, :], start=(c == 0), stop=(c == 7)))
                          for c in range(8)], reads=[ws_r, R("mT")], writes=[bk_r])
                      ec += 1
                      evac(ec, Vc[:, mc, half * 512:(half + 1) * 512], bk[:, :], [bk_r], [R("Vc", mc)])
              for half in range(2):
                  load_w(wA[half][:, :, :], wq_d[:, half * 512:(half + 1) * 512].rearrange("(c p) n -> p c n", p=128),
                         f"wA{half}", R("wA", half))
              sbk2 = Rot([PB[3], PB[4]])
              obk = Rot([PB[5], PB[6]])
              dbk = PB[7]
              for tt in range(4):
                  tsl = slice(tt * 512, (tt + 1) * 512)
                  for j in range(8):
                      bk, bk_r = mmb.next()
                      P.op("pe", [(I("matmul",
                          bk[:, :], lhsT=wA[j // 4][:, c, (j % 4) * 128:(j % 4 + 1) * 128], rhs=HT[:, c, tsl],
                          start=(c == 0), stop=(c == 7))) for c in range(8)],
                          reads=[R("wA", j // 4), R("HT", tt)], writes=[bk_r])
                      ec += 1
                      evac(ec, qcT[:, j, :], bk[:, :], [bk_r], [R("qcT", j)])
                  for hh in range(4):
                      for mc in range(2):
                          sk, sk_r = sbk2.next()
                          P.op("pe", [(I("matmul",
                              sk[:, :], lhsT=KcT[:, 2 * hh + jj, mc * 128:(mc + 1) * 128], rhs=qcT[:, 2 * hh + jj, :],
                              start=(jj == 0), stop=(jj == 1))) for jj in range(2)],
                              reads=[R("KcT", 2 * hh), R("KcT", 2 * hh + 1), R("qcT", 2 * hh), R("qcT", 2 * hh + 1)], writes=[sk_r])
                          e, e_r = Ec[(hh % 2) * 2 + mc], R("Ec", (hh % 2) * 2 + mc)
                          P.op("act", I("activation", out=e[:, :], in_=sk[:, :], func=AF.Exp, scale=1.0 / 16),
                               reads=[sk_r], writes=[e_r])
                      e0, e1 = Ec[(hh % 2) * 2], Ec[(hh % 2) * 2 + 1]
                      er = [R("Ec", (hh % 2) * 2), R("Ec", (hh % 2) * 2 + 1)]
                      db, db_r = dbk
                      P.op("pe", [(I("matmul", db[:, :], lhsT=onesb[:, :], rhs=e[:, :],
                                                                  start=(mc == 0), stop=(mc == 1))) for mc, e in ((0, e0), (1, e1))],
                           reads=er + [R("onesb")], writes=[db_r])
                      rd, rd_r = rdn[0], R("rdn", 0)
                      P.op("act", I("activation", out=rd[:, :], in_=db[:, :], func=AF.Ln), reads=[db_r], writes=[rd_r])
                      P.op("act", I("activation", out=rd[:, :], in_=rd[:, :], func=AF.Exp, scale=-1.0), reads=[rd_r], writes=[rd_r])
                      for jj in range(2):
                          ob, ob_r = obk.next()
                          P.op("pe", [(I("matmul",
                              ob[:, :], lhsT=Vc[:, mc, (2 * hh + jj) * 128:(2 * hh + jj + 1) * 128], rhs=e[:, :],
                              start=(mc == 0), stop=(mc == 1))) for mc, e in ((0, e0), (1, e1))],
                              reads=er + [R("Vc", 0), R("Vc", 1)], writes=[ob_r])
                          P.op("dve", I("tensor_tensor",
                              out=OC[:, 2 * hh + jj, tsl], in0=ob[:, :], in1=rd[:, :], op=ALU.mult),
                              reads=[ob_r, rd_r], writes=[R("OC", tt)])
              mmb = Rot([PB[0], PB[1], PB[2], PB[3]])
              P.barrier()

              out_proj(wo_d, OC, lambda tb: R("OC", tb // 4), 24)
              P.barrier()

              chk("cross")
              cv = Carver()
              cv.off = x_end
              junk = cv.take([128, D], BF16)
              xn = [cv.take([128, D], BF16) for _ in range(4)]
              wU = [cv.take([128, 8, 512], BF16) for _ in range(2)]
              wD = [cv.take([128, 4, D], BF16) for _ in range(2)]
              hid = [cv.take([128, 4, 512], BF16) for _ in range(2)]
              rl = [cv.take([128, 512], BF16) for _ in range(2)]
              gF = cv.take([128, D], F32)
              ot = [cv.take([128, D], F32) for _ in range(2)]
              P.dma("sp", I("dma_start", out=gF[:, :], in_=gF_d[:, :]), "gF", writes=[R("gF")])
              def final_norm(i):
                  src = XR[:, i, :]
                  ssc = 8 + 2 * (i % 4)
                  ss, rs = stat[:, ssc:ssc + 1], stat[:, ssc + 1:ssc + 2]
                  ss_r, rs_r = R("stat", ssc), R("stat", ssc + 1)
                  o_, o_r = ot[i % 2], R("ot", i % 2)
                  P.op("act", I("activation", out=junk[:, :], in_=src, func=AF.Square, accum_out=ss),
                       reads=[R("XR", i)], writes=[R("junk"), ss_r])
                  P.op("act", I("activation", out=rs, in_=ss, func=AF.Sqrt, scale=1.0 / D, bias=1e-6),
                       reads=[ss_r], writes=[rs_r])
                  P.op("dve", I("reciprocal", out=rs, in_=rs), reads=[rs_r], writes=[rs_r])
                  P.op("dve", I("scalar_tensor_tensor",
                      out=o_[:, :], in0=src, scalar=rs, in1=gF[:, :], op0=ALU.mult, op1=ALU.mult),
                      reads=[R("XR", i), rs_r, R("gF")], writes=[o_r])
                  out_toks.append(P.dma("sp", I("dma_start", out=out_d[b, i * 128:(i + 1) * 128, :], in_=o_[:, :]),
                                        f"ot{i % 2}", reads=[o_r]))

              upb = Rot([PB[0], PB[1], PB[2]])
              dnb = Rot([PB[3], PB[4], PB[5], PB[6]])
              cnt = 0
              for fg in range(8):
                  wu, wu_r = wU[fg % 2], R("wU", fg % 2)
                  wd_, wd_r = wD[fg % 2], R("wD", fg % 2)
                  def _ldmlp(fj):
                      load_w(wU[fj % 2][:, :, :], wup_d[:, fj * 512:(fj + 1) * 512].rearrange("(c p) n -> p c n", p=128),
                             f"wU{fj % 2}", R("wU", fj % 2))
                      load_w(wD[fj % 2][:, :, :], wdn_d[fj * 512:(fj + 1) * 512, :].rearrange("(c p) n -> p c n", p=128),
                             f"wD{fj % 2}", R("wD", fj % 2))
                  if fg == 0:
                      _ldmlp(0)
                  if fg + 1 < 8:
                      _ldmlp(fg + 1)
                  for tt in range(4):
                      tsl = slice(tt * 512, (tt + 1) * 512)
                      cnt += 1
                      hd, hd_r = hid[cnt % 2], R("hid", cnt % 2)
                      for fc in range(4):
                          bk, bk_r = upb.next()
                          P.op("pe", [(I("matmul",
                              bk[:, :], lhsT=wu[:, c, fc * 128:(fc + 1) * 128], rhs=HT[:, c, tsl],
                              start=(c == 0), stop=(c == 7))) for c in range(8)],
                              reads=[wu_r, R("HT", tt)], writes=[bk_r])
                          r_, r_r = rl[fc % 2], R("rl", fc % 2)
                          P.op("act", I("activation", out=r_[:, :], in_=bk[:, :], func=AF.Relu),
                               reads=[bk_r], writes=[r_r])
                          P.op("pool", I("tensor_tensor", out=hd[:, fc, :], in0=r_[:, :], in1=r_[:, :],
                                                                                         op=ALU.mult),
                               reads=[r_r], writes=[hd_r])
                      for t4 in range(4):
                          tb = tt * 4 + t4
                          for half in range(2):
                              bk, bk_r = dnb.next()
                              P.op("pe", [(I("matmul",
                                  bk[:, :], lhsT=hd[:, fc, t4 * 128:(t4 + 1) * 128], rhs=wd_[:, fc, half * 512:(half + 1) * 512],
                                  start=(fc == 0), stop=(fc == 3))) for fc in range(4)],
                                  reads=[hd_r, wd_r], writes=[bk_r])
                              P.op("dve", I("tensor_tensor",
                                  out=XR[:, tb, half * 512:(half + 1) * 512], in0=bk[:, :], in1=XR[:, tb, half * 512:(half + 1) * 512],
                                  op=ALU.add), reads=[bk_r, R("XR", tb)], writes=[R("XR", tb)])
                          if fg == 7:
                              final_norm(tb)
              P.barrier()

        except _Stop:
            P.barrier()
        P.wait_tokens("sp", out_toks)
        with nc.Block() as block:
            P.emit(block)
    nc._mk_stats = (P.nops, P.nwaits)
    return nc


_NC_CACHE = {}


def kernel(x, mem, norm_mix_g, w_in, kv_norm_g, w_uk, w_uv, w_out, norm_cross_g, norm_mem_g,
           w_q_cross, w_kv_cross, w_o_cross, norm_mlp_g, w_up, w_down, norm_final_g):
    f = lambda a: np.ascontiguousarray(np.asarray(a, dtype=np.float32))
    x, mem = f(x), f(mem)
    win_cols, _ = _win_plan()
    win = f(f(w_in)[0][:, win_cols])
    wukT = f(np.transpose(f(w_uk)[0], (2, 0, 1)).reshape(128, 512))
    wuv = f(np.transpose(f(w_uv)[0], (1, 0, 2)).reshape(128, 512))
    gcol = lambda g: f(g).reshape(8, 128).T
    gall = f(np.concatenate([gcol(norm_mix_g), gcol(norm_cross_g), gcol(norm_mem_g), gcol(norm_mlp_g),
                             f(kv_norm_g).reshape(128, 1)], axis=1))
    gF = f(np.broadcast_to(f(norm_final_g).reshape(1, D), (128, D)))
    consts = _consts()
    if "nc" not in _NC_CACHE:
        _NC_CACHE["nc"] = build_nc(NB)
    nc = _NC_CACHE["nc"]
    shared = dict(win=win, wukT=wukT, wuv=wuv, wout=f(w_out)[0], wq=f(w_q_cross)[0], wkv=f(w_kv_cross)[0],
                  wo=f(w_o_cross)[0], wup=f(w_up)[0], wdn=f(w_down)[0], gall=gall, gF=gF, **consts)
    in_maps = []
    for c in range(NCORES):
        m = dict(shared)
        m["x"] = x[c * NB:(c + 1) * NB]
        m["mem"] = mem[c * NB:(c + 1) * NB]
        in_maps.append(m)
    res = run_bass_kernel_spmd(nc, in_maps, core_ids=list(range(NCORES)))
    return np.concatenate([res.results[c]["out"] for c in range(NCORES)], axis=0).astype(np.float32)
```
